# Optimizing a Trainium2 kernel written in Bass

```python
import math
import jax, jax.numpy as jnp
from jax import lax
import numpy as np

D_MODEL = 1024
BATCH = 16
SEQ = 2048
DEPTH = 1

HG_HEADS = 4
HG_KDIM = 128
HG_VDIM = 128
HG_F = HG_HEADS * HG_KDIM
HG_V = HG_HEADS * HG_VDIM
HG_CHUNK = 64
MLA_HEADS = 8
MLA_NOPE = 64
MLA_ROPE = 32
MLA_VDIM = 64
MLA_QK = MLA_NOPE + MLA_ROPE
MLA_Q_RANK = 256
MLA_KV_RANK = 128
ATTN_BLOCK = 128
ROPE_THETA = 10000.0
N_GROUPS = 8
EXPERTS_PER_GROUP = 8
N_EXPERTS = N_GROUPS * EXPERTS_PER_GROUP
TOP_K = 2
D_EXPERT = 256
MOE_BLOCK = 128
PLE_DIM = 256
EPS = 1e-6
IN_WIDTHS = (HG_F, HG_F, HG_F, HG_V, HG_V, MLA_Q_RANK, MLA_KV_RANK, MLA_ROPE, D_MODEL, D_MODEL)
D_IN = HG_F * 3 + HG_V * 2 + MLA_Q_RANK + MLA_KV_RANK + MLA_ROPE + 2 * D_MODEL

kernel_name = "hybrid_hgrn2_mla_hmoe_block"


def rmsnorm(x, g):
    xf = x.astype(jnp.float32)
    y = xf * lax.rsqrt(jnp.mean(xf * xf, axis=-1, keepdims=True) + EPS)
    return (y * g.astype(jnp.float32)).astype(x.dtype)


def rope(x, positions):
    half = MLA_ROPE // 2
    inv_freq = ROPE_THETA ** (-jnp.arange(half, dtype=jnp.float32) / half)
    ang = positions.astype(jnp.float32)[..., None] * inv_freq
    cos = jnp.cos(ang)[:, :, None, :]
    sin = jnp.sin(ang)[:, :, None, :]
    x1 = x[..., :half].astype(jnp.float32)
    x2 = x[..., half:].astype(jnp.float32)
    out = jnp.concatenate([x1 * cos - x2 * sin, x2 * cos + x1 * sin], axis=-1)
    return out.astype(x.dtype)


def gla_chunkwise(q, k, v, log_f):
    B, H, S, K = q.shape
    V = v.shape[-1]
    n = S // HG_CHUNK

    def to_chunks(a):
        return jnp.moveaxis(a.reshape(B, H, n, HG_CHUNK, a.shape[-1]), 2, 0)

    qc, kc, vc, gc = to_chunks(q), to_chunks(k), to_chunks(v), to_chunks(log_f)
    lower = jnp.tril(jnp.ones((HG_CHUNK, HG_CHUNK), dtype=bool))

    def step(state, inp):
        qb, kb, vb, gb = inp
        g = jnp.cumsum(gb, axis=2)
        o_inter = jnp.einsum('bhtk,bhkv->bhtv', qb * jnp.exp(g), state)
        diff = g[:, :, :, None, :] - g[:, :, None, :, :]
        decay = jnp.exp(jnp.where(lower[:, :, None], diff, -jnp.inf))
        scores = jnp.einsum('bhtk,bhsk,bhtsk->bhts', qb, kb, decay)
        o_intra = jnp.einsum('bhts,bhsv->bhtv', scores, vb)
        g_last = g[:, :, -1]
        k_dec = kb * jnp.exp(g_last[:, :, None, :] - g)
        new_state = jnp.exp(g_last)[..., None] * state + jnp.einsum('bhck,bhcv->bhkv', k_dec, vb)
        return new_state, o_inter + o_intra

    s0 = jnp.zeros((B, H, K, V), jnp.float32)
    _, o = lax.scan(step, s0, (qc, kc, vc, gc))
    return jnp.moveaxis(o, 0, 2).reshape(B, H, S, V)


def block_dense_attention(q, k, v):
    B, H, S, Dq = q.shape
    nb = S // ATTN_BLOCK
    qb = jnp.moveaxis(q.reshape(B, H, nb, ATTN_BLOCK, Dq), 2, 0)
    scale = MLA_QK ** -0.5

    def one_block(qblk):
        s = jnp.einsum('bhqd,bhkd->bhqk', qblk, k).astype(jnp.float32) * scale
        pr = jax.nn.softmax(s, axis=-1).astype(v.dtype)
        return jnp.einsum('bhqk,bhkd->bhqd', pr, v)

    o = lax.map(one_block, qb)
    return jnp.moveaxis(o, 0, 2).reshape(B, H, S, v.shape[-1])


def hierarchical_moe(h, w_rg, b_rg, w_re, b_re, w1, w3, w2):
    T, D = h.shape
    g_logits = (h @ w_rg).astype(jnp.float32) + b_rg.astype(jnp.float32)
    g_prob = jax.nn.softmax(g_logits, axis=-1)
    g_sel = jnp.argmax(g_prob, axis=-1).astype(jnp.int32)
    p_group = jnp.max(g_prob, axis=-1)
    e_logits = ((h @ w_re).astype(jnp.float32) + b_re.astype(jnp.float32)).reshape(T, N_GROUPS, EXPERTS_PER_GROUP)
    e_in_group = jnp.take_along_axis(e_logits, g_sel[:, None, None], axis=1)[:, 0]
    top_v, top_i = lax.top_k(e_in_group, TOP_K)
    w_local = jax.nn.softmax(top_v, axis=-1)
    expert = (g_sel[:, None] * EXPERTS_PER_GROUP + top_i.astype(jnp.int32)).reshape(-1)
    weight = (p_group[:, None] * w_local).reshape(-1)
    token = jnp.repeat(jnp.arange(T, dtype=jnp.int32), TOP_K)
    A = T * TOP_K

    counts = jnp.zeros((N_EXPERTS,), jnp.int32).at[expert].add(1)
    starts = jnp.cumsum(counts) - counts
    pcounts = (counts + MOE_BLOCK - 1) // MOE_BLOCK * MOE_BLOCK
    pends = jnp.cumsum(pcounts)
    pstarts = pends - pcounts
    order = jnp.argsort(expert)
    e_sorted = expert[order]
    rank = jnp.arange(A, dtype=jnp.int32) - starts[e_sorted]
    dest = pstarts[e_sorted] + rank
    n_blocks = (A + N_EXPERTS * (MOE_BLOCK - 1) + MOE_BLOCK - 1) // MOE_BLOCK
    P = n_blocks * MOE_BLOCK
    buf_tok = jnp.full((P,), T, jnp.int32).at[dest].set(token[order])
    buf_w = jnp.zeros((P,), h.dtype).at[dest].set(weight[order].astype(h.dtype))
    block_expert = jnp.minimum(
        jnp.searchsorted(pends, jnp.arange(n_blocks, dtype=jnp.int32) * MOE_BLOCK, side='right'),
        N_EXPERTS - 1)
    h_pad = jnp.concatenate([h, jnp.zeros((1, D), h.dtype)], axis=0)
    xb = h_pad[buf_tok].reshape(n_blocks, MOE_BLOCK, D)

    def expert_block(args):
        xblk, e = args
        return (jax.nn.silu(xblk @ w1[e]) * (xblk @ w3[e])) @ w2[e]

    yb = lax.map(expert_block, (xb, block_expert)).reshape(P, D)
    out = jax.ops.segment_sum(yb * buf_w[:, None], buf_tok, num_segments=T + 1)
    return out[:T]


def setup_inputs(seed: int = 0) -> dict:
    key = jax.random.key(seed)
    ks = jax.random.split(key, 32)
    f32 = jnp.float32

    def w(k, shape, fan_in):
        return jax.random.normal(k, shape, f32) * (fan_in ** -0.5)

    def gain(k, shape):
        return 1.0 + 0.1 * jax.random.normal(k, shape, f32)

    L = DEPTH
    offsets = jax.random.randint(ks[2], (BATCH, 1), 0, SEQ, dtype=jnp.int32)
    positions = jnp.arange(SEQ, dtype=jnp.int32)[None, :] + offsets
    return {
        "x": jax.random.normal(ks[0], (BATCH, SEQ, D_MODEL), f32),
        "p": jax.random.normal(ks[1], (DEPTH, BATCH, SEQ, PLE_DIM), f32),
        "positions": positions,
        "ln_mix": gain(ks[3], (L, D_MODEL)),
        "w_in": w(ks[4], (L, D_MODEL, D_IN), D_MODEL),
        "hg_lb": 0.5 * jax.random.normal(ks[5], (2, DEPTH + 1, HG_F), f32),
        "hg_onorm": gain(ks[6], (L, HG_VDIM)),
        "w_oA": w(ks[7], (L, HG_V, D_MODEL), HG_V),
        "mla_qa_norm": gain(ks[8], (L, MLA_Q_RANK)),
        "mla_kva_norm": gain(ks[9], (L, MLA_KV_RANK)),
        "w_uq": w(ks[10], (L, MLA_Q_RANK, MLA_HEADS * MLA_QK), MLA_Q_RANK),
        "w_ukv": w(ks[11], (L, MLA_KV_RANK, MLA_HEADS * (MLA_NOPE + MLA_VDIM)), MLA_KV_RANK),
        "q_norm": gain(ks[12], (L, MLA_QK)),
        "k_norm": gain(ks[13], (L, MLA_QK)),
        "w_oB": w(ks[14], (L, MLA_HEADS * MLA_VDIM, D_MODEL), MLA_HEADS * MLA_VDIM),
        "w_out": w(ks[15], (L, D_MODEL, D_MODEL), D_MODEL),
        "ln_moe": gain(ks[16], (L, D_MODEL)),
        "w_rg": w(ks[17], (L, D_MODEL, N_GROUPS), D_MODEL),
        "b_rg": 0.01 * jax.random.normal(ks[18], (L, N_GROUPS), f32),
        "w_re": w(ks[19], (L, D_MODEL, N_EXPERTS), D_MODEL),
        "b_re": 0.01 * jax.random.normal(ks[20], (L, N_EXPERTS), f32),
        "w1": w(ks[21], (L, N_EXPERTS, D_MODEL, D_EXPERT), D_MODEL),
        "w3": w(ks[22], (L, N_EXPERTS, D_MODEL, D_EXPERT), D_MODEL),
        "w2": w(ks[23], (L, N_EXPERTS, D_EXPERT, D_MODEL), D_EXPERT),
        "ln_ple": gain(ks[24], (L, D_MODEL)),
        "w_ple_gate": w(ks[25], (L, D_MODEL, D_MODEL), D_MODEL),
        "w_ple_proj": w(ks[26], (L, PLE_DIM, D_MODEL), PLE_DIM),
    }


def reference(x, p, positions, ln_mix, w_in, hg_lb, hg_onorm, w_oA,
              mla_qa_norm, mla_kva_norm, w_uq, w_ukv, q_norm, k_norm, w_oB,
              w_out, ln_moe, w_rg, b_rg, w_re, b_re, w1, w3, w2,
              ln_ple, w_ple_gate, w_ple_proj):
    B, S, D = x.shape
    f32 = jnp.float32
    split_points = [int(v) for v in np.cumsum(IN_WIDTHS)[:-1]]
    lb_all = jnp.cumsum(jax.nn.softmax(hg_lb.astype(f32), axis=1), axis=1)

    def heads(a, hd):
        return a.reshape(B, S, -1, hd).transpose(0, 2, 1, 3)

    for layer in range(DEPTH):
        h = rmsnorm(x, ln_mix[layer])
        z = h @ w_in[layer]
        q_a, f_fw, f_bw, i_a, og_a, c_q, c_kv, k_rope, gate_a, gate_b = jnp.split(z, split_points, axis=-1)

        qh = heads(jax.nn.silu(q_a.astype(f32)), HG_KDIM)
        vh = heads(i_a.astype(f32), HG_VDIM)

        def hgrn_direction(f_logit, lb, reverse):
            f = lb + (1.0 - lb) * jax.nn.sigmoid(f_logit.astype(f32))
            log_f = heads(jnp.log(f), HG_KDIM)
            kh = heads(1.0 - f, HG_KDIM)
            if reverse:
                o = gla_chunkwise(jnp.flip(qh, 2), jnp.flip(kh, 2), jnp.flip(vh, 2), jnp.flip(log_f, 2))
                return jnp.flip(o, 2)
            return gla_chunkwise(qh, kh, vh, log_f)

        o_hg = hgrn_direction(f_fw, lb_all[0, layer], False) + hgrn_direction(f_bw, lb_all[1, layer], True)
        o_hg = o_hg.transpose(0, 2, 1, 3)
        o_hg = rmsnorm(o_hg, hg_onorm[layer]) * jax.nn.silu(og_a.astype(f32).reshape(B, S, HG_HEADS, HG_VDIM))
        y_a = o_hg.reshape(B, S, HG_V).astype(x.dtype) @ w_oA[layer]

        cq = rmsnorm(c_q, mla_qa_norm[layer])
        qm = (cq @ w_uq[layer]).reshape(B, S, MLA_HEADS, MLA_QK)
        ckv = rmsnorm(c_kv, mla_kva_norm[layer])
        kv = (ckv @ w_ukv[layer]).reshape(B, S, MLA_HEADS, MLA_NOPE + MLA_VDIM)
        k_nope, vm = kv[..., :MLA_NOPE], kv[..., MLA_NOPE:]
        km = jnp.concatenate(
            [k_nope, jnp.broadcast_to(k_rope[:, :, None, :], (B, S, MLA_HEADS, MLA_ROPE))], axis=-1)
        qm = rmsnorm(qm, q_norm[layer])
        km = rmsnorm(km, k_norm[layer])
        qm = jnp.concatenate([qm[..., :MLA_NOPE], rope(qm[..., MLA_NOPE:], positions)], axis=-1)
        km = jnp.concatenate([km[..., :MLA_NOPE], rope(km[..., MLA_NOPE:], positions)], axis=-1)
        o_mla = block_dense_attention(qm.transpose(0, 2, 1, 3), km.transpose(0, 2, 1, 3), vm.transpose(0, 2, 1, 3))
        y_b = o_mla.transpose(0, 2, 1, 3).reshape(B, S, MLA_HEADS * MLA_VDIM) @ w_oB[layer]

        merged = jax.nn.sigmoid(gate_a) * y_a + jax.nn.sigmoid(gate_b) * y_b
        x = x + merged @ w_out[layer]

        hm = rmsnorm(x, ln_moe[layer]).reshape(B * S, D)
        x = x + hierarchical_moe(hm, w_rg[layer], b_rg[layer], w_re[layer], b_re[layer],
                                 w1[layer], w3[layer], w2[layer]).reshape(B, S, D)

        ple_gate = jax.nn.sigmoid(rmsnorm(x, ln_ple[layer]) @ w_ple_gate[layer])
        x = x + (p[layer] @ w_ple_proj[layer]) * ple_gate
    return x
```

```python
import numpy as np
from contextlib import ExitStack
import concourse.bass as bass
import concourse.mybir as mybir
from concourse.alu_op_type import AluOpType as ALU
from concourse.bass_utils import run_bass_kernel_spmd

F32 = mybir.dt.float32
BF16 = mybir.dt.bfloat16
I32 = mybir.dt.int32
AF = mybir.ActivationFunctionType
AX = mybir.AxisListType

D = 1024
DIN = 5024
NEXP = 64
EPS = 1e-6
CH = 64


class Buf:
    __slots__ = ("w", "r")

    def __init__(self):
        self.w = {}
        self.r = {}


class T:
    def __init__(self, t):
        self.t = t
        self.b = Buf()

    def __getitem__(self, k):
        return self.t[k]


class Eng:
    def __init__(self, name, eng, sem):
        self.name = name
        self.eng = eng
        self.sem = sem
        self.cnt = 0
        self.seen = {}
        self.pr = []
        self.pw = []


class Ring:
    def __init__(self, nc, q, P):
        self.P = P
        self.sems = [nc.alloc_semaphore("dq_%s_%d" % (q, i)) for i in range(P)]
        self.cnt = [0] * P
        self.last = [None] * P
        self.n = 0


class KB:
    def __init__(self, nc):
        self.nc = nc
        self.engs = {}
        for name, e in (("pe", nc.tensor), ("act", nc.scalar), ("dve", nc.vector),
                        ("pool", nc.gpsimd), ("sp", nc.sync)):
            self.engs[name] = Eng(name, e, nc.alloc_semaphore("s_" + name))
        self.rings = {"sp": Ring(nc, "sp", 24), "act": Ring(nc, "act", 12), "pool": Ring(nc, "pool", 12)}
        self.dbufs = {}

    def db(self, *key):
        b = self.dbufs.get(key)
        if b is None:
            b = Buf()
            self.dbufs[key] = b
        return b

    def wait(self, en, tok):
        sem, val = tok
        E = self.engs[en]
        k = id(sem)
        if E.seen.get(k, 0) >= val:
            return
        E.eng.wait_ge(sem, val)
        E.seen[k] = val

    def _deps(self, en, reads, writes):
        for b in reads:
            for t in b.w.values():
                self.wait(en, t)
        for b in writes:
            for t in b.w.values():
                self.wait(en, t)
            for t in b.r.values():
                self.wait(en, t)

    @staticmethod
    def _rec(tok, reads, writes, partial):
        k = id(tok[0])
        for b in reads:
            b.r[k] = tok
        for b in writes:
            if partial:
                b.w[k] = tok
            else:
                b.w = {k: tok}
            b.r = {}

    def op(self, en, emit, reads=(), writes=(), signal=True, partial=False):
        E = self.engs[en]
        reads = [x.b if isinstance(x, T) else x for x in reads]
        writes = [x.b if isinstance(x, T) else x for x in writes]
        self._deps(en, reads, writes)
        ins = emit(E.eng)
        if signal:
            E.cnt += 1
            ins.then_inc(E.sem, 1)
            tok = (E.sem, E.cnt)
            self._rec(tok, E.pr + reads, [], False)
            self._rec(tok, [], E.pw + writes, partial)
            E.pr = []
            E.pw = []
        else:
            E.pr += reads
            E.pw += writes
        return ins

    def dma(self, q, out, in_, reads=(), writes=(), partial=False, indirect=None, **kw):
        E = self.engs[q]
        R = self.rings[q]
        reads = [x.b if isinstance(x, T) else x for x in reads]
        writes = [x.b if isinstance(x, T) else x for x in writes]
        self._deps(q, reads, writes)
        slot = R.n % R.P
        if R.last[slot] is not None:
            self.wait(q, R.last[slot])
        R.cnt[slot] += 16
        tok = (R.sems[slot], R.cnt[slot])
        if indirect is None:
            ins = E.eng.dma_start(out=out, in_=in_, **kw)
        else:
            ins = E.eng.indirect_dma_start(out=out, in_=in_, **indirect)
        ins.then_inc(R.sems[slot], 16)
        R.last[slot] = tok
        R.n += 1
        self._rec(tok, reads, writes, partial)
        return tok

    def barrier(self):
        toks = [(E.sem, E.cnt) for E in self.engs.values() if E.cnt > 0]
        for R in self.rings.values():
            toks += [t for t in R.last if t is not None]
        for en in self.engs:
            for t in toks:
                self.wait(en, t)


def build(NS=2, S=2048, CAP=256, debug=False, upto="F"):
    nc = bass.Bass("TRN2", target_bir_lowering=False)
    TT = NS * S
    NT = TT // 128
    NTS = S // 128
    NCH = S // CH
    NSLOT = NEXP * CAP
    assert S % 512 == 0

    def din(name, shape, dt=F32):
        return nc.dram_tensor(name, list(shape), dt, kind="ExternalInput").ap()

    def dscr(name, shape, dt):
        return nc.dram_tensor(name, list(shape), dt, kind="ExternalOutput" if debug else "Internal").ap()

    x_d = din("x", [NS, S, D])
    p_d = din("p", [NS, S, 256])
    pos_d = din("positions", [NS, S], I32)
    ln_mix_d = din("ln_mix", [D])
    w_in_d = din("w_in", [D, DIN])
    hg_lb_d = din("hg_lb", [2, 2, 512])
    hg_onorm_d = din("hg_onorm", [128])
    w_oA_d = din("w_oA", [512, D])
    qa_norm_d = din("mla_qa_norm", [256])
    kva_norm_d = din("mla_kva_norm", [128])
    w_uq_d = din("w_uq", [256, 768])
    w_ukv_d = din("w_ukv", [128, 1024])
    q_norm_d = din("q_norm", [96])
    k_norm_d = din("k_norm", [96])
    w_oB_d = din("w_oB", [512, D])
    w_out_d = din("w_out", [D, D])
    ln_moe_d = din("ln_moe", [D])
    w_rg_d = din("w_rg", [D, 8])
    b_rg_d = din("b_rg", [8])
    w_re_d = din("w_re", [D, 64])
    b_re_d = din("b_re", [64])
    w1_d = din("w1", [NEXP, D, 256])
    w3_d = din("w3", [NEXP, D, 256])
    w2_d = din("w2", [NEXP, 256, D])
    ln_ple_d = din("ln_ple", [D])
    w_pg_d = din("w_ple_gate", [D, D])
    w_pp_d = din("w_ple_proj", [256, D])
    out_d = nc.dram_tensor("out", [NS, S, D], F32, kind="ExternalOutput").ap()

    zq_d = dscr("zq", [NS, 4, 128, S], BF16)
    zf_d = dscr("zf", [NS, 2, 4, 128, S], F32)
    zv_d = dscr("zv", [TT, 512], BF16)
    zog_d = dscr("zog", [TT, 512], BF16)
    zg_d = dscr("zg", [TT, 2048], BF16)
    qT_d = dscr("qT", [NS, 96, 8, S], BF16)
    kT_d = dscr("kT", [NS, 96, 8, S], BF16)
    vm_d = dscr("vm", [TT, 8, 64], BF16)
    x1_d = dscr("x1", [TT, D], F32)
    hmb_d = dscr("hmb", [TT, D], BF16)
    xs_d = dscr("xs", [NSLOT, D], BF16)
    ys_d = dscr("ys", [NSLOT, D], F32)

    K = KB(nc)

    def pipeline(makers, W):
        active = []
        it = iter(makers)
        more = True
        while True:
            while len(active) < W and more:
                try:
                    active.append(next(it)())
                except StopIteration:
                    more = False
            if not active:
                break
            for g_ in list(active):
                try:
                    next(g_)
                except StopIteration:
                    active.remove(g_)
    op = K.op
    dma = K.dma
    root = ExitStack()
    right_stacks = []
    uid = [0]

    def sb(st, name, shape, dt, side="left"):
        if st is root or st in right_stacks:
            side = "right"
        uid[0] += 1
        return T(st.enter_context(nc.sbuf_tensor("sb%d_%s" % (uid[0], name), list(shape), dt, side=side)))

    ps_all = root.enter_context(nc.psum_tensor("ps_all", [128, 4096], F32))
    banks = [Buf() for _ in range(8)]
    bank_i = [0]

    reserved = set()

    def bank():
        while True:
            i = bank_i[0] % 8
            bank_i[0] += 1
            if i not in reserved:
                break
        return ps_all[:, i * 512:(i + 1) * 512], banks[i]

    def fixed_bank(i):
        return ps_all[:, i * 512:(i + 1) * 512], banks[i]

    dif_i = sb(root, "dif_i", [128, 128], I32)
    dif = sb(root, "dif", [128, 128], F32)
    ident_b = sb(root, "ident_b", [128, 128], BF16)
    ident_f = sb(root, "ident_f", [128, 128], F32)
    maskf = sb(root, "maskf", [128, 128], F32)
    maskb = sb(root, "maskb", [128, 128], F32)
    lstrict = sb(root, "lstrict", [128, 128], BF16)
    ones_b = sb(root, "ones_b", [128, 128], BF16)
    sel = sb(root, "sel", [128, 64], F32)
    op("pool", lambda e: e.iota(dif_i[:], pattern=[[1, 128]], base=0, channel_multiplier=-1), writes=[dif_i])
    op("dve", lambda e: e.tensor_copy(out=dif[:], in_=dif_i[:]), reads=[dif_i], writes=[dif])
    for dst, cmp_ in ((ident_b, ALU.is_equal), (ident_f, ALU.is_equal), (maskf, ALU.is_ge),
                      (maskb, ALU.is_le), (lstrict, ALU.is_gt)):
        op("dve", lambda e, dst=dst, cmp_=cmp_: e.tensor_single_scalar(out=dst[:], in_=dif[:], scalar=0.0, op=cmp_),
           reads=[dif], writes=[dst])
    op("dve", lambda e: e.memset(ones_b[:], 1.0), writes=[ones_b])
    op("dve", lambda e: e.memset(sel[:], 0.0), writes=[sel])
    op("dve", lambda e: e.memset(sel[64:65, :], 1.0), writes=[sel])

    def bc_load(st, name, src1d, n):
        t = sb(st, name, [128, n], F32)
        dma("sp", t[:], src1d.partition_broadcast(128), writes=[t])
        return t

    def rstd_from_ssq(ssq_ap, tmp, out_ap, dim, bufs):
        P_, n_ = ssq_ap.shape[0], ssq_ap.shape[1]
        op("dve", lambda e: e.tensor_scalar(out=tmp[0:P_, 0:n_], in0=ssq_ap, scalar1=1.0 / dim, scalar2=EPS,
                                            op0=ALU.mult, op1=ALU.add), reads=bufs, writes=[tmp])
        op("act", lambda e: e.activation(out=tmp[0:P_, 0:n_], in_=tmp[0:P_, 0:n_], func=AF.Sqrt),
           reads=[tmp], writes=[tmp])
        op("dve", lambda e: e.reciprocal(out=out_ap, in_=tmp[0:P_, 0:n_]), reads=[tmp], writes=bufs)

    def tmp_ap(tmp, like):
        n = like.shape[-1] if len(like.shape) == 2 else None
        return tmp[0:like.shape[0], 0:like.shape[1]]

    phA = ExitStack()
    Win = sb(phA, "Win", [128, 8, DIN], BF16)
    wstage = [sb(phA, "wstage%d" % i, [128, 1024], F32) for i in range(3)]
    cast_engs = ["dve", "pool", "act"]
    cast_i = [0]
    wst_cur = [wstage]

    def load_cast(dst_ap, dst_T, src_ap, ncols, npart=128):
        i = cast_i[0]
        cast_i[0] += 1
        stg = wst_cur[0][i % 3]
        if npart != 128:
            dma("sp", stg[0:npart, 0:ncols], src_ap, writes=[stg])
            op("dve", lambda e: e.tensor_copy(out=dst_ap, in_=stg[0:npart, 0:ncols]), reads=[stg], writes=[dst_T],
               partial=True)
            return
        dma("sp", stg[:, 0:ncols], src_ap, writes=[stg])
        en = cast_engs[i % 3]
        if en == "act":
            op("act", lambda e: e.copy(out=dst_ap, in_=stg[:, 0:ncols]), reads=[stg], writes=[dst_T], partial=True)
        else:
            op(en, lambda e: e.tensor_copy(out=dst_ap, in_=stg[:, 0:ncols]), reads=[stg], writes=[dst_T], partial=True)

    for kc in range(8):
        for c0 in range(0, DIN, 1024):
            c1 = min(DIN, c0 + 1024)
            load_cast(Win[:, kc, c0:c1], Win, w_in_d[kc * 128:(kc + 1) * 128, c0:c1], c1 - c0)

    mixW = phA
    w_uq = sb(mixW, "w_uq", [128, 2, 768], BF16)
    w_ukv = sb(mixW, "w_ukv", [128, 1024], BF16)
    for kc in range(2):
        load_cast(w_uq[:, kc, :], w_uq, w_uq_d[kc * 128:(kc + 1) * 128, :], 768)
    load_cast(w_ukv[:, :], w_ukv, w_ukv_d[:, :], 1024)
    g_mix = bc_load(mixW, "g_mix", ln_mix_d, D)
    g_qa = bc_load(mixW, "g_qa", qa_norm_d, 256)
    g_kva = bc_load(mixW, "g_kva", kva_norm_d, 128)
    g_qn = bc_load(mixW, "g_qn", q_norm_d, 96)
    g_kn = bc_load(mixW, "g_kn", k_norm_d, 96)
    g_on = bc_load(root, "g_on", hg_onorm_d, 128)

    lbraw = sb(root, "lbraw", [128, 16], F32)
    lb = sb(root, "lb", [128, 8], F32)
    oml = sb(root, "oml", [128, 8], F32)
    with nc.allow_non_contiguous_dma(reason="tiny param load"):
        dma("sp", lbraw[:, :], hg_lb_d.rearrange("d l (h k) -> k (d l h)", k=128), writes=[lbraw])
    lbv = lbraw[:, :].rearrange("p (d l h) -> p d l h", d=2, l=2)
    op("dve", lambda e: e.tensor_tensor(out=lb[:, :].rearrange("p (d h) -> p d h", d=2), in0=lbv[:, :, 0, :],
                                        in1=lbv[:, :, 1, :], op=ALU.subtract), reads=[lbraw], writes=[lb])
    op("act", lambda e: e.activation(out=lb[:, :], in_=lb[:, :], func=AF.Sigmoid), reads=[lb], writes=[lb])
    op("dve", lambda e: e.tensor_scalar(out=oml[:, :], in0=lb[:, :], scalar1=-1.0, scalar2=1.0, op0=ALU.mult,
                                        op1=ALU.add), reads=[lb], writes=[oml])

    pos_r = sb(mixW, "pos_r", [NT, 128], I32)
    dma("sp", pos_r[:, :], pos_d.rearrange("s (n p) -> (s n) p", p=128), writes=[pos_r])
    posr_f = sb(mixW, "posr_f", [NT, 128], F32)
    op("dve", lambda e: e.tensor_copy(out=posr_f[:, :], in_=pos_r[:, :]), reads=[pos_r], writes=[posr_f])
    posf = sb(mixW, "posf", [128, NT], F32)
    pa_, pb_ = bank()
    op("pe", lambda e: e.transpose(out=pa_[:, 0:NT], in_=posr_f[:, :], identity=ident_f[0:NT, 0:NT]),
       reads=[posr_f, ident_f], writes=[pb_])
    op("dve", lambda e: e.tensor_copy(out=posf[:, :], in_=pa_[:, 0:NT]), reads=[pb_], writes=[posf])
    invf = sb(mixW, "invf", [128, 16], F32)
    for i in range(16):
        v = float(10000.0 ** (-(i / 16.0)) / (2.0 * np.pi))
        op("dve", lambda e, i=i, v=v: e.memset(invf[:, i:i + 1], v), writes=[invf], partial=True)
    rope_cs = sb(mixW, "rope_cs", [128, NT, 32], F32)
    with ExitStack() as st0:
        turns = sb(st0, "turns", [128, NT, 32], F32)
        ti = sb(st0, "turns_i", [128, NT, 32], I32)
        tf = sb(st0, "turns_f", [128, NT, 32], F32)
        adj = sb(st0, "adj", [128, NT, 32], F32)
        op("dve", lambda e: e.tensor_tensor(out=turns[:, :, 16:32], in0=posf[:, :].unsqueeze(2).broadcast_to([128, NT, 16]),
                                            in1=invf[:, :].unsqueeze(1).broadcast_to([128, NT, 16]), op=ALU.mult),
           reads=[posf, invf], writes=[turns])
        op("dve", lambda e: e.tensor_scalar(out=turns[:, :, 0:16], in0=turns[:, :, 16:32], scalar1=0.25, scalar2=None,
                                            op0=ALU.add), reads=[turns], writes=[turns])
        op("dve", lambda e: e.tensor_copy(out=ti[:], in_=turns[:]), reads=[turns], writes=[ti])
        op("dve", lambda e: e.tensor_copy(out=tf[:], in_=ti[:]), reads=[ti], writes=[tf])
        op("dve", lambda e: e.tensor_tensor(out=turns[:], in0=turns[:], in1=tf[:], op=ALU.subtract), reads=[turns, tf],
           writes=[turns])
        op("dve", lambda e: e.tensor_single_scalar(out=adj[:], in_=turns[:], scalar=0.5, op=ALU.is_gt), reads=[turns],
           writes=[adj])
        op("dve", lambda e: e.tensor_tensor(out=turns[:], in0=turns[:], in1=adj[:], op=ALU.subtract), reads=[turns, adj],
           writes=[turns])
        op("dve", lambda e: e.tensor_single_scalar(out=adj[:], in_=turns[:], scalar=-0.5, op=ALU.is_lt), reads=[turns],
           writes=[adj])
        op("dve", lambda e: e.tensor_tensor(out=turns[:], in0=turns[:], in1=adj[:], op=ALU.add), reads=[turns, adj],
           writes=[turns])
        op("dve", lambda e: e.tensor_scalar(out=turns[:], in0=turns[:], scalar1=0.4999999, scalar2=-0.4999999,
                                            op0=ALU.min, op1=ALU.max), reads=[turns], writes=[turns])
        op("act", lambda e: e.activation(out=rope_cs[:], in_=turns[:], func=AF.Sin, scale=float(2.0 * np.pi)),
           reads=[turns], writes=[rope_cs])
        K.barrier()

    zt = sb(root, "zt", [128, D], BF16)
    op("pool", lambda e: e.memset(zt[:, :], 0.0), writes=[zt])
    xs_v = xs_d.rearrange("(a p) d -> p a d", p=128)
    NA = NSLOT // 128
    for a0 in range(0, NA, 16):
        a1 = min(NA, a0 + 16)
        dma("sp", xs_v[:, a0:a1, :], zt[:, :].unsqueeze(1).broadcast_to([128, a1 - a0, D]), reads=[zt],
            writes=[K.db("xs")], partial=True)

    stg_i = [0]
    with ExitStack() as st:
        xt = [sb(st, "xt%d" % i, [128, D], F32) for i in range(2)]
        sq = sb(st, "sq", [128, D], BF16)
        ssq = sb(st, "ssq", [128, 4], F32)
        rt = sb(st, "rt", [128, 16], F32)
        rs = sb(st, "rs", [128, 4], F32)
        hn = [sb(st, "hn%d" % i, [128, D], BF16) for i in range(2)]
        hT4 = [sb(st, "hT4_%d" % i, [128, 8, 512], BF16) for i in range(2)]
        stg = [sb(st, "stg%d" % i, [128, 512], F32) for i in range(6)]
        stgb = [sb(st, "stgb%d" % i, [128, 512], BF16) for i in range(6)]
        cz = [sb(st, "cz%d" % i, [128, 416], F32) for i in range(2)]
        czs = sb(st, "czs", [128, 384], BF16)
        cn = sb(st, "cn", [128, 384], BF16)
        cT = sb(st, "cT", [128, 3, 128], BF16)
        qk = sb(st, "qk", [128, 2, 8, 96], F32)
        qk2 = sb(st, "qk2", [128, 2, 8, 96], BF16)
        qss = sb(st, "qss", [128, 16], F32)
        qrs = sb(st, "qrs", [128, 16], F32)
        qkn = sb(st, "qkn", [128, 2, 8, 96], F32)
        qkb = sb(st, "qkb", [128, 2, 8, 96], BF16)
        rtmp = [sb(st, "rtmp%d" % i, [128, 2, 8, 16], F32) for i in range(4)]
        qkT = [sb(st, "qkT%d" % i, [96, 8, 128], BF16) for i in range(2)]
        vmb = sb(st, "vmb", [128, 8, 64], BF16)

        def next_stg(bf):
            i = stg_i[0]
            stg_i[0] += 1
            return (stgb if bf else stg)[i % 6]

        NG = TT // 512
        for g in range(NG):
            seq = (g * 512) // S
            t0 = (g * 512) % S
            h4 = hT4[g % 2]
            for j in range(4):
                i = g * 4 + j
                x_ = xt[i % 2]
                h_ = hn[i % 2]
                dma("sp", x_[:, :], x_d[seq, t0 + j * 128:t0 + (j + 1) * 128, :], writes=[x_])
                op("act", lambda e: e.activation(out=sq[:, :], in_=x_[:, :], func=AF.Square), reads=[x_], writes=[sq])
                op("dve", lambda e: e.tensor_reduce(out=ssq[:, 0:1], in_=sq[:, :], axis=AX.X, op=ALU.add), reads=[sq],
                   writes=[ssq])
                rstd_from_ssq(ssq[:, 0:1], rt, rs[:, 0:1], D, [ssq, rs])
                op("dve", lambda e: e.scalar_tensor_tensor(out=h_[:, :], in0=x_[:, :], scalar=rs[:, 0:1], in1=g_mix[:, :],
                                                           op0=ALU.mult, op1=ALU.mult), reads=[x_, rs, g_mix], writes=[h_])
                pa, pb = bank()
                pbf = pa.bitcast(BF16)
                for kc in range(8):
                    op("pe", lambda e, kc=kc: e.transpose(out=pbf[:, kc * 128:(kc + 1) * 128],
                                                          in_=h_[:, kc * 128:(kc + 1) * 128], identity=ident_b[:, :]),
                       reads=[h_, ident_b], writes=[pb], signal=(kc == 7))
                op("act", lambda e: e.copy(out=h4[:, :, j * 128:(j + 1) * 128],
                                           in_=pbf.rearrange("p (k t) -> p k t", k=8)), reads=[pb], writes=[h4],
                   partial=True)

            for c in range(12):
                pa, pb = bank()
                for kc in range(8):
                    op("pe", lambda e, kc=kc: e.matmul(pa, lhsT=Win[:, kc, c * 128:(c + 1) * 128], rhs=h4[:, kc, :],
                                                       start=(kc == 0), stop=(kc == 7)),
                       reads=[Win, h4], writes=[pb], signal=(kc == 7))
                if c < 4:
                    s_ = next_stg(True)
                    op("act", lambda e: e.activation(out=s_[:, :], in_=pa, func=AF.Silu), reads=[pb], writes=[s_])
                    dma("act", zq_d[seq, c, :, t0:t0 + 512], s_[:, :], reads=[s_], writes=[K.db("zq", seq, c, g)])
                else:
                    d_ = (c - 4) // 4
                    h_i = (c - 4) % 4
                    s_ = next_stg(False)
                    op("dve", lambda e: e.tensor_copy(out=s_[:, :], in_=pa), reads=[pb], writes=[s_])
                    dma("sp", zf_d[seq, d_, h_i, :, t0:t0 + 512], s_[:, :], reads=[s_],
                        writes=[K.db("zf", seq, d_, h_i, g)])

            for j in range(4):
                i = g * 4 + j
                tok0 = i * 128
                lts = h4[:, :, j * 128:(j + 1) * 128]
                groups = [(1536, 2048, "v"), (2048, 2560, "og"), (2560, 2976, "c"), (2976, 3488, "g0"),
                          (3488, 4000, "g1"), (4000, 4512, "g2"), (4512, 5024, "g3")]
                for (c0, c1, kind) in groups:
                    pa, pb = bank()
                    n = c1 - c0
                    for kc in range(8):
                        op("pe", lambda e, kc=kc: e.matmul(pa[:, 0:n], lhsT=lts[:, kc, :], rhs=Win[:, kc, c0:c1],
                                                           start=(kc == 0), stop=(kc == 7)),
                           reads=[Win, h4], writes=[pb], signal=(kc == 7))
                    if kind == "v":
                        s_ = next_stg(True)
                        op("dve", lambda e: e.tensor_copy(out=s_[:, :], in_=pa), reads=[pb], writes=[s_])
                        dma("sp", zv_d[tok0:tok0 + 128, :], s_[:, :], reads=[s_], writes=[K.db("zv", i)])
                    elif kind == "og":
                        s_ = next_stg(True)
                        op("act", lambda e: e.activation(out=s_[:, :], in_=pa, func=AF.Silu), reads=[pb], writes=[s_])
                        dma("act", zog_d[tok0:tok0 + 128, :], s_[:, :], reads=[s_], writes=[K.db("zog", i)])
                    elif kind[0] == "g":
                        gi = int(kind[1])
                        s_ = next_stg(True)
                        op("act", lambda e: e.activation(out=s_[:, :], in_=pa, func=AF.Sigmoid), reads=[pb], writes=[s_])
                        dma("act", zg_d[tok0:tok0 + 128, gi * 512:(gi + 1) * 512], s_[:, :], reads=[s_],
                            writes=[K.db("zg", i, gi)])
                    else:
                        c_ = cz[i % 2]
                        op("dve", lambda e: e.tensor_copy(out=c_[:, :], in_=pa[:, 0:416]), reads=[pb], writes=[c_])
                        op("act", lambda e: e.activation(out=czs[:, :], in_=c_[:, 0:384], func=AF.Square), reads=[c_],
                           writes=[czs])
                        op("dve", lambda e: e.tensor_reduce(out=ssq[:, 1:2], in_=czs[:, 0:256], axis=AX.X, op=ALU.add),
                           reads=[czs], writes=[ssq])
                        op("dve", lambda e: e.tensor_reduce(out=ssq[:, 2:3], in_=czs[:, 256:384], axis=AX.X, op=ALU.add),
                           reads=[czs], writes=[ssq])
                        rstd_from_ssq(ssq[:, 1:2], rt, rs[:, 1:2], 256, [ssq, rs])
                        rstd_from_ssq(ssq[:, 2:3], rt, rs[:, 2:3], 128, [ssq, rs])
                        op("dve", lambda e: e.scalar_tensor_tensor(out=cn[:, 0:256], in0=c_[:, 0:256], scalar=rs[:, 1:2],
                                                                   in1=g_qa[:, :], op0=ALU.mult, op1=ALU.mult),
                           reads=[c_, rs, g_qa], writes=[cn])
                        op("dve", lambda e: e.scalar_tensor_tensor(out=cn[:, 256:384], in0=c_[:, 256:384], scalar=rs[:, 2:3],
                                                                   in1=g_kva[:, :], op0=ALU.mult, op1=ALU.mult),
                           reads=[c_, rs, g_kva], writes=[cn])
                        pa2, pb2 = bank()
                        pbf2 = pa2.bitcast(BF16)
                        for kc in range(3):
                            op("pe", lambda e, kc=kc: e.transpose(out=pbf2[:, kc * 128:(kc + 1) * 128],
                                                                  in_=cn[:, kc * 128:(kc + 1) * 128], identity=ident_b[:, :]),
                               reads=[cn, ident_b], writes=[pb2], signal=(kc == 2))
                        op("act", lambda e: e.copy(out=cT[:, :, :], in_=pbf2[:, 0:384].rearrange("p (k t) -> p k t", k=3)),
                           reads=[pb2], writes=[cT])
                        pq0, pqb0 = bank()
                        pq1, pqb1 = bank()
                        for kc in range(2):
                            op("pe", lambda e, kc=kc: e.matmul(pq0, lhsT=cT[:, kc, :], rhs=w_uq[:, kc, 0:512],
                                                               start=(kc == 0), stop=(kc == 1)),
                               reads=[cT, w_uq], writes=[pqb0], signal=(kc == 1))
                        for kc in range(2):
                            op("pe", lambda e, kc=kc: e.matmul(pq1[:, 0:256], lhsT=cT[:, kc, :], rhs=w_uq[:, kc, 512:768],
                                                               start=(kc == 0), stop=(kc == 1)),
                               reads=[cT, w_uq], writes=[pqb1], signal=(kc == 1))
                        qflat = qk[:, 0, :, :].rearrange("p h d -> p (h d)")
                        op("act", lambda e: e.copy(out=qflat[:, 0:512], in_=pq0), reads=[pqb0], writes=[qk], partial=True)
                        op("act", lambda e: e.copy(out=qflat[:, 512:768], in_=pq1[:, 0:256]), reads=[pqb1], writes=[qk],
                           partial=True)
                        pk0, pkb0 = bank()
                        pk1, pkb1 = bank()
                        op("pe", lambda e: e.matmul(pk0, lhsT=cT[:, 2, :], rhs=w_ukv[:, 0:512], start=True, stop=True),
                           reads=[cT, w_ukv], writes=[pkb0])
                        op("pe", lambda e: e.matmul(pk1, lhsT=cT[:, 2, :], rhs=w_ukv[:, 512:1024], start=True, stop=True),
                           reads=[cT, w_ukv], writes=[pkb1])
                        for hh, (pk, pkb) in enumerate(((pk0, pkb0), (pk1, pkb1))):
                            pkv = pk.rearrange("p (h d) -> p h d", h=4)
                            op("dve", lambda e: e.tensor_copy(out=qk[:, 1, hh * 4:(hh + 1) * 4, 0:64], in_=pkv[:, :, 0:64]),
                               reads=[pkb], writes=[qk], partial=True)
                            op("act", lambda e: e.copy(out=vmb[:, hh * 4:(hh + 1) * 4, :], in_=pkv[:, :, 64:128]),
                               reads=[pkb], writes=[vmb], partial=True)
                        op("dve", lambda e: e.tensor_copy(out=qk[:, 1, :, 64:96],
                                                          in_=c_[:, 384:416].unsqueeze(1).broadcast_to([128, 8, 32])),
                           reads=[c_], writes=[qk], partial=True)
                        dma("act", vm_d[tok0:tok0 + 128, :, :], vmb[:, :, :], reads=[vmb], writes=[K.db("vm", i)])
                        op("act", lambda e: e.activation(out=qk2[:], in_=qk[:], func=AF.Square), reads=[qk], writes=[qk2])
                        op("dve", lambda e: e.tensor_reduce(out=qss[:, :], in_=qk2[:].rearrange("p a h d -> p (a h) d"),
                                                            axis=AX.X, op=ALU.add), reads=[qk2], writes=[qss])
                        rstd_from_ssq(qss[:, :], rt, qrs[:, :], 96, [qss, qrs])
                        op("dve", lambda e: e.tensor_tensor(out=qkn[:].rearrange("p a h d -> p (a h) d"),
                                                            in0=qk[:].rearrange("p a h d -> p (a h) d"),
                                                            in1=qrs[:, :].unsqueeze(2).broadcast_to([128, 16, 96]),
                                                            op=ALU.mult), reads=[qk, qrs], writes=[qkn])
                        for a_, gg in ((0, g_qn), (1, g_kn)):
                            op("dve", lambda e, a_=a_, gg=gg: e.tensor_tensor(
                                out=qkn[:, a_, :, :], in0=qkn[:, a_, :, :],
                                in1=gg[:, :].unsqueeze(1).broadcast_to([128, 8, 96]), op=ALU.mult),
                               reads=[qkn, gg], writes=[qkn])
                        cosb = rope_cs[:, i, 0:16].unsqueeze(1).unsqueeze(1).broadcast_to([128, 2, 8, 16])
                        sinb = rope_cs[:, i, 16:32].unsqueeze(1).unsqueeze(1).broadcast_to([128, 2, 8, 16])
                        x1_ = qkn[:, :, :, 64:80]
                        x2_ = qkn[:, :, :, 80:96]
                        op("dve", lambda e: e.tensor_tensor(out=rtmp[0][:], in0=x1_, in1=cosb, op=ALU.mult),
                           reads=[qkn, rope_cs], writes=[rtmp[0]])
                        op("pool", lambda e: e.tensor_tensor(out=rtmp[1][:], in0=x2_, in1=sinb, op=ALU.mult),
                           reads=[qkn, rope_cs], writes=[rtmp[1]])
                        op("dve", lambda e: e.tensor_tensor(out=rtmp[2][:], in0=x2_, in1=cosb, op=ALU.mult),
                           reads=[qkn, rope_cs], writes=[rtmp[2]])
                        op("pool", lambda e: e.tensor_tensor(out=rtmp[3][:], in0=x1_, in1=sinb, op=ALU.mult),
                           reads=[qkn, rope_cs], writes=[rtmp[3]])
                        op("act", lambda e: e.copy(out=qkb[:, :, :, 0:64], in_=qkn[:, :, :, 0:64]), reads=[qkn],
                           writes=[qkb], partial=True)
                        op("dve", lambda e: e.tensor_tensor(out=qkb[:, :, :, 64:80], in0=rtmp[0][:], in1=rtmp[1][:],
                                                            op=ALU.subtract), reads=[rtmp[0], rtmp[1]], writes=[qkb],
                           partial=True)
                        op("dve", lambda e: e.tensor_tensor(out=qkb[:, :, :, 80:96], in0=rtmp[2][:], in1=rtmp[3][:],
                                                            op=ALU.add), reads=[rtmp[2], rtmp[3]], writes=[qkb],
                           partial=True)
                        for a_, dst in ((0, qT_d), (1, kT_d)):
                            pa3, pb3 = bank()
                            pbf3 = pa3.bitcast(BF16)
                            for h in range(8):
                                op("pe", lambda e, h=h, a_=a_: e.transpose(out=pbf3[0:96, h * 128:(h + 1) * 128],
                                                                           in_=qkb[:, a_, h, :], identity=ident_b[:, :]),
                                   reads=[qkb, ident_b], writes=[pb3], signal=(h == 7))
                            qt_ = qkT[a_]
                            op("act" if a_ == 0 else "dve",
                               (lambda e: e.copy(out=qt_[:, :, :], in_=pbf3[0:96, :].rearrange("p (h t) -> p h t", h=8)))
                               if a_ == 0 else
                               (lambda e: e.tensor_copy(out=qt_[:, :, :], in_=pbf3[0:96, :].rearrange("p (h t) -> p h t", h=8))),
                               reads=[pb3], writes=[qt_])
                            tloc = t0 + j * 128
                            dma("sp", dst[seq, :, :, tloc:tloc + 128], qt_[:, :, :], reads=[qt_],
                                writes=[K.db("qkT", a_, i)])
        K.barrier()
    phA.close()
    if upto == "A":
        root.close()
        return nc

    moeR = root
    Moh = sb(moeR, "Moh", [128, NT, 2, 64], BF16)
    wts = sb(moeR, "wts", [128, NT, 2], F32)
    dest_i = sb(root, "dest_i", [128, NT, 2], I32)

    for seq in range(NS):
        seqst = ExitStack()
        oaT = sb(seqst, "oaT", [128, 4, S], BF16)
        obT = sb(seqst, "obT", [64, 8, S], BF16)

        with ExitStack() as st:
            qTs = sb(st, "qTs", [128, S], BF16)
            vtk = sb(st, "vtk", [CH, NCH, 128], BF16)
            ogt = sb(st, "ogt", [CH, NCH, 128], BF16)
            qt = [sb(st, "qt%d" % d, [128, S], BF16) for d in range(2)]
            kt = [sb(st, "kt%d" % d, [128, S], BF16) for d in range(2)]
            kdt = [sb(st, "kdt%d" % d, [CH, NCH, 128], BF16) for d in range(2)]
            dec = [sb(st, "dec%d" % d, [128, NCH], F32) for d in range(2)]
            S32 = [sb(st, "S32_%d" % d, [128, 128], F32) for d in range(2)]
            Sbf = [sb(st, "Sbf_%d" % d, [128, 128], BF16) for d in range(2)]
            PT = [[sb(st, "PT%d_%d" % (d, i), [CH, CH], BF16) for i in range(2)] for d in range(2)]
            for h in range(4):
                pst = ExitStack()
                smask = sb(pst, "smask", [128, S], F32)
                op("dve", lambda e: e.memset(smask[:, :], 1.0), writes=[smask])
                op("dve", lambda e: e.memset(smask[:, :].rearrange("p (c j) -> p c j", j=CH)[:, :, 0:1], 0.0), writes=[smask])
                lg = sb(pst, "lg", [128, S], F32)
                fA = sb(pst, "fA", [128, S], F32)
                lf = sb(pst, "lf", [128, S], F32)
                kk = sb(pst, "kk", [128, S], F32)
                gA = sb(pst, "gA", [128, S], F32)
                eA = sb(pst, "eA", [128, S], F32)
                kdT = sb(pst, "kdT", [128, S], BF16)
                for g in range(S // 512):
                    dma("sp", qTs[:, g * 512:(g + 1) * 512], zq_d[seq, h, :, g * 512:(g + 1) * 512],
                        reads=[K.db("zq", seq, h, seq * (S // 512) + g)], writes=[qTs], partial=True)
                dma("sp", vtk[:, :, :], zv_d[seq * S:(seq + 1) * S, h * 128:(h + 1) * 128].rearrange("(c p) v -> p c v", p=CH),
                    reads=[K.db("zv", i) for i in range(seq * NTS, (seq + 1) * NTS)], writes=[vtk])
                dma("sp", ogt[:, :, :], zog_d[seq * S:(seq + 1) * S, h * 128:(h + 1) * 128].rearrange("(c p) v -> p c v", p=CH),
                    reads=[K.db("zog", i) for i in range(seq * NTS, (seq + 1) * NTS)], writes=[ogt])
                for d in range(2):
                    for g in range(S // 512):
                        dma("sp", lg[:, g * 512:(g + 1) * 512], zf_d[seq, d, h, :, g * 512:(g + 1) * 512],
                            reads=[K.db("zf", seq, d, h, seq * (S // 512) + g)], writes=[lg], partial=True)
                    col = d * 4 + h
                    op("act", lambda e: e.activation(out=fA[:, :], in_=lg[:, :], func=AF.Sigmoid), reads=[lg], writes=[fA])
                    op("dve", lambda e: e.tensor_scalar(out=fA[:, :], in0=fA[:, :], scalar1=oml[:, col:col + 1],
                                                        scalar2=lb[:, col:col + 1], op0=ALU.mult, op1=ALU.add),
                       reads=[fA, oml, lb], writes=[fA])
                    op("act", lambda e: e.activation(out=lf[:, :], in_=fA[:, :], func=AF.Ln), reads=[fA], writes=[lf])
                    op("pool", lambda e: e.tensor_scalar(out=kk[:, :], in0=fA[:, :], scalar1=-1.0, scalar2=1.0,
                                                         op0=ALU.mult, op1=ALU.add), reads=[fA], writes=[kk])
                    op("dve", lambda e: e.tensor_tensor_scan(out=gA[:, :], data0=smask[:, :], data1=lf[:, :], initial=0.0,
                                                             op0=ALU.mult, op1=ALU.add), reads=[smask, lf], writes=[gA])
                    g3 = gA[:, :].rearrange("p (c j) -> p c j", j=CH)
                    if d == 0:
                        glast = g3[:, :, CH - 1:CH]
                        G_ = gA
                    else:
                        op("dve", lambda e: e.tensor_tensor(out=lg[:, :], in0=lf[:, :], in1=gA[:, :], op=ALU.subtract),
                           reads=[lf, gA], writes=[lg])
                        op("dve", lambda e: e.tensor_tensor(out=lg[:, :].rearrange("p (c j) -> p c j", j=CH),
                                                            in0=lg[:, :].rearrange("p (c j) -> p c j", j=CH),
                                                            in1=g3[:, :, CH - 1:CH].broadcast_to([128, NCH, CH]), op=ALU.add),
                           reads=[lg, gA], writes=[lg])
                        G_ = lg
                        glast = g3[:, :, CH - 1:CH]
                    G3 = G_[:, :].rearrange("p (c j) -> p c j", j=CH)
                    op("act", lambda e: e.activation(out=dec[d][:, :], in_=glast.rearrange("p c o -> p (c o)"), func=AF.Exp),
                       reads=[gA], writes=[dec[d]])
                    op("act", lambda e: e.activation(out=eA[:, :], in_=G_[:, :], func=AF.Exp), reads=[G_], writes=[eA])
                    op("dve", lambda e: e.tensor_tensor(out=qt[d][:, :], in0=qTs[:, :], in1=eA[:, :], op=ALU.mult),
                       reads=[qTs, eA], writes=[qt[d]])
                    op("act", lambda e: e.activation(out=eA[:, :], in_=G_[:, :], func=AF.Exp, scale=-1.0), reads=[G_],
                       writes=[eA])
                    op("pool", lambda e: e.tensor_tensor(out=kt[d][:, :], in0=kk[:, :], in1=eA[:, :], op=ALU.mult),
                       reads=[kk, eA], writes=[kt[d]])
                    op("dve", lambda e: e.tensor_tensor(out=fA[:, :].rearrange("p (c j) -> p c j", j=CH),
                                                        in0=glast.broadcast_to([128, NCH, CH]), in1=G3, op=ALU.subtract),
                       reads=[gA, G_], writes=[fA])
                    op("act", lambda e: e.activation(out=fA[:, :], in_=fA[:, :], func=AF.Exp), reads=[fA], writes=[fA])
                    op("dve", lambda e: e.tensor_tensor(out=kdT[:, :], in0=kk[:, :], in1=fA[:, :], op=ALU.mult),
                       reads=[kk, fA], writes=[kdT])
                    for c8 in range(0, NCH, 8):
                        pa, pb = bank()
                        pbf = pa.bitcast(BF16)
                        for cc in range(8):
                            c = c8 + cc
                            op("pe", lambda e, c=c, cc=cc: e.transpose(out=pbf[0:CH, cc * 128:(cc + 1) * 128],
                                                                       in_=kdT[:, c * CH:(c + 1) * CH], identity=ident_b[:, :]),
                               reads=[kdT, ident_b], writes=[pb], signal=(cc == 7))
                        op("act", lambda e: e.copy(out=kdt[d][:, c8:c8 + 8, :],
                                                   in_=pbf[0:CH, :].rearrange("p (c k) -> p c k", c=8)),
                           reads=[pb], writes=[kdt[d]], partial=True)
                K.barrier()
                pst.close()
                rst = ExitStack()
                oacc = [sb(rst, "oacc%d" % d, [CH, NCH, 128], F32) for d in range(2)]
                osq = sb(rst, "osq", [CH, NCH, 128], BF16)
                oss = sb(rst, "oss", [CH, NCH], F32)
                ors = sb(rst, "ors", [CH, NCH], F32)
                ort = sb(rst, "ort", [CH, NCH], F32)
                ohg = sb(rst, "ohg", [CH, NCH, 128], BF16)
                for d in range(2):
                    op("dve", lambda e, d=d: e.memset(S32[d][:, :], 0.0), writes=[S32[d]])
                    op("pool", lambda e, d=d: e.memset(Sbf[d][:, :], 0.0), writes=[Sbf[d]])
                for step in range(NCH):
                    for d in range(2):
                        c = step if d == 0 else NCH - 1 - step
                        cs_ = slice(c * CH, (c + 1) * CH)
                        pt_ = PT[d][step % 2]
                        pa, pb = bank()
                        op("pe", lambda e: e.matmul(pa[0:CH, 0:CH], lhsT=kt[d][:, cs_], rhs=qt[d][:, cs_], start=True, stop=True),
                           reads=[kt[d], qt[d]], writes=[pb])
                        mk = maskf if d == 0 else maskb
                        op("dve", lambda e: e.tensor_tensor(out=pt_[:, :], in0=pa[0:CH, 0:CH], in1=mk[0:CH, 0:CH], op=ALU.mult),
                           reads=[pb, mk], writes=[pt_])
                        pa2, pb2 = bank()
                        op("pe", lambda e: e.matmul(pa2[0:CH, 0:128], lhsT=qt[d][:, cs_], rhs=Sbf[d][:, :], start=True, stop=False),
                           reads=[qt[d], Sbf[d]], writes=[pb2], signal=False)
                        op("pe", lambda e: e.matmul(pa2[0:CH, 0:128], lhsT=pt_[:, :], rhs=vtk[:, c, :], start=False, stop=True),
                           reads=[pt_, vtk], writes=[pb2])
                        op("act", lambda e: e.copy(out=oacc[d][:, c, :], in_=pa2[0:CH, 0:128]), reads=[pb2], writes=[oacc[d]],
                           partial=True)
                        pa3, pb3 = bank()
                        op("pe", lambda e: e.matmul(pa3[:, 0:128], lhsT=kdt[d][:, c, :], rhs=vtk[:, c, :], start=True, stop=True),
                           reads=[kdt[d], vtk], writes=[pb3])
                        op("dve", lambda e: e.scalar_tensor_tensor(out=S32[d][:, :], in0=S32[d][:, :], scalar=dec[d][:, c:c + 1],
                                                                   in1=pa3[:, 0:128], op0=ALU.mult, op1=ALU.add),
                           reads=[S32[d], dec[d], pb3], writes=[S32[d]])
                        op("pool", lambda e: e.tensor_copy(out=Sbf[d][:, :], in_=S32[d][:, :]), reads=[S32[d]], writes=[Sbf[d]])
                op("dve", lambda e: e.tensor_tensor(out=oacc[0][:], in0=oacc[0][:], in1=oacc[1][:], op=ALU.add),
                   reads=[oacc[0], oacc[1]], writes=[oacc[0]])
                op("act", lambda e: e.activation(out=osq[:], in_=oacc[0][:], func=AF.Square), reads=[oacc[0]], writes=[osq])
                op("dve", lambda e: e.tensor_reduce(out=oss[:, :], in_=osq[:], axis=AX.X, op=ALU.add), reads=[osq], writes=[oss])
                rstd_from_ssq(oss[:, :], ort, ors[:, :], 128, [oss, ors])
                op("dve", lambda e: e.tensor_tensor(out=oacc[0][:], in0=oacc[0][:],
                                                    in1=ors[:, :].unsqueeze(2).broadcast_to([CH, NCH, 128]), op=ALU.mult),
                   reads=[oacc[0], ors], writes=[oacc[0]])
                op("pool", lambda e: e.tensor_tensor(out=oacc[0][:], in0=oacc[0][:],
                                                     in1=g_on[0:CH, :].unsqueeze(1).broadcast_to([CH, NCH, 128]), op=ALU.mult),
                   reads=[oacc[0], g_on], writes=[oacc[0]])
                op("dve", lambda e: e.tensor_tensor(out=ohg[:], in0=oacc[0][:], in1=ogt[:], op=ALU.mult),
                   reads=[oacc[0], ogt], writes=[ohg])
                for c8 in range(0, NCH, 8):
                    pa, pb = bank()
                    pbf = pa.bitcast(BF16)
                    for cc in range(8):
                        c = c8 + cc
                        op("pe", lambda e, c=c, cc=cc: e.transpose(out=pbf[:, cc * CH:(cc + 1) * CH], in_=ohg[:, c, :],
                                                                   identity=ident_b[0:CH, 0:CH]),
                           reads=[ohg, ident_b], writes=[pb], signal=(cc == 7))
                    op("act", lambda e: e.copy(out=oaT[:, h, c8 * CH:(c8 + 8) * CH], in_=pbf[:, 0:8 * CH]), reads=[pb],
                       writes=[oaT], partial=True)
                K.barrier()
                rst.close()

        if upto == "B":
            seqst.close()
            root.close()
            return nc
        with ExitStack() as st:
            QT = [sb(st, "QT%d" % i, [96, S], BF16) for i in range(2)]
            KT = [sb(st, "KT%d" % i, [96, S], BF16) for i in range(2)]
            VV = [sb(st, "VV%d" % i, [128, NTS, 65], BF16) for i in range(2)]
            for i in range(2):
                op("dve", lambda e, i=i: e.memset(VV[i][:, :, 64:65], 1.0), writes=[VV[i]], partial=True)
            G_ = 4
            NPT = 2 * G_ + 1
            PTs = [sb(st, "PTs%d" % i, [128, 512], BF16) for i in range(NPT)]
            Osb = [sb(st, "Osb%d" % i, [65, 512], F32) for i in range(2)]
            rden = [sb(st, "rden%d" % i, [64, 512], F32) for i in range(2)]
            scale = float(96 ** -0.5)
            reserved.update((0, 1))
            tiles = list(range(seq * NTS, (seq + 1) * NTS))
            NQG = S // 512
            items = [(h, qg, kt_) for h in range(8) for qg in range(NQG) for kt_ in range(NTS)]
            LA = G_

            def c_load(h):
                Q_, K_, V_ = QT[h % 2], KT[h % 2], VV[h % 2]
                dma("sp", Q_[:, :], qT_d[seq, :, h, :], reads=[K.db("qkT", 0, i) for i in tiles], writes=[Q_])
                dma("sp", K_[:, :], kT_d[seq, :, h, :], reads=[K.db("qkT", 1, i) for i in tiles], writes=[K_])
                dma("sp", V_[:, :, 0:64], vm_d[seq * S:(seq + 1) * S, h, :].rearrange("(n p) d -> p n d", p=128),
                    reads=[K.db("vm", i) for i in tiles], writes=[V_], partial=True)

            def c_qk(ii):
                h, qg, kt_ = items[ii]
                Q_, K_ = QT[h % 2], KT[h % 2]
                pa, pb = bank()
                op("pe", lambda e: e.matmul(pa, lhsT=K_[:, kt_ * 128:(kt_ + 1) * 128], rhs=Q_[:, qg * 512:(qg + 1) * 512],
                                            start=True, stop=True), reads=[K_, Q_], writes=[pb])
                p_ = PTs[ii % NPT]
                op("act", lambda e: e.activation(out=p_[:, :], in_=pa, func=AF.Exp, scale=scale), reads=[pb], writes=[p_])

            epi = []

            def c_pv(ii):
                h, qg, kt_ = items[ii]
                V_ = VV[h % 2]
                gi = h * NQG + qg
                po, pob = fixed_bank(gi % 2)
                p_ = PTs[ii % NPT]
                op("pe", lambda e: e.matmul(po[0:65, :], lhsT=V_[:, kt_, :], rhs=p_[:, :], start=(kt_ == 0),
                                            stop=(kt_ == NTS - 1)), reads=[V_, p_], writes=[pob], signal=(kt_ == NTS - 1))
                if kt_ == NTS - 1:
                    o_ = Osb[gi % 2]
                    op("dve", lambda e: e.tensor_copy(out=o_[:, :], in_=po[0:65, :]), reads=[pob], writes=[o_])
                    epi.append((ii, h, qg, gi))

            def c_epi(h, qg, gi):
                o_ = Osb[gi % 2]
                r_ = rden[gi % 2]
                pd, pdb = bank()
                op("pe", lambda e: e.matmul(pd[0:64, :], lhsT=sel[0:65, :], rhs=o_[:, :], start=True, stop=True),
                   reads=[sel, o_], writes=[pdb])
                op("dve", lambda e: e.reciprocal(out=r_[:, :], in_=pd[0:64, :]), reads=[pdb], writes=[r_])
                op("dve", lambda e: e.tensor_tensor(out=obT[:, h, qg * 512:(qg + 1) * 512], in0=o_[0:64, :], in1=r_[:, :],
                                                    op=ALU.mult), reads=[o_, r_], writes=[obT], partial=True)

            n_it = len(items)
            assert n_it % G_ == 0
            c_load(0)
            c_load(1)
            ngrp = n_it // G_
            for g in range(ngrp + 2):
                if g < ngrp:
                    for ii in range(g * G_, (g + 1) * G_):
                        c_qk(ii)
                while epi and epi[0][0] < (g - 1) * G_:
                    _, h_, qg_, gi_ = epi.pop(0)
                    c_epi(h_, qg_, gi_)
                if 1 <= g <= ngrp:
                    for ii in range((g - 1) * G_, g * G_):
                        c_pv(ii)
                    hl, qgl, ktl = items[g * G_ - 1]
                    if qgl == NQG - 1 and ktl == NTS - 1 and hl + 2 < 8:
                        c_load(hl + 2)
            assert not epi
            reserved.clear()
            K.barrier()
        if upto == "C":
            seqst.close()
            root.close()
            return nc

        with ExitStack() as st:
            w_oA = sb(st, "w_oA", [128, 4, D], BF16)
            w_oB = sb(st, "w_oB", [64, 8, D], BF16)
            w_out = sb(st, "w_out", [128, 8, D], BF16)
            w_rt = sb(st, "w_rt", [128, 8, 72], F32)
            wst_cur[0] = [sb(st, "wstD%d" % i, [128, 1024], F32) for i in range(3)]
            for kc in range(4):
                load_cast(w_oA[:, kc, :], w_oA, w_oA_d[kc * 128:(kc + 1) * 128, :], 1024)
            for h in range(8):
                load_cast(w_oB[:, h, :], w_oB, w_oB_d[h * 64:(h + 1) * 64, :], 1024, npart=64)
            for kc in range(8):
                load_cast(w_out[:, kc, :], w_out, w_out_d[kc * 128:(kc + 1) * 128, :], 1024)
            dma("sp", w_rt[:, :, 0:8], w_rg_d.rearrange("(k p) g -> p k g", p=128), writes=[w_rt], partial=True)
            dma("sp", w_rt[:, :, 8:72], w_re_d.rearrange("(k p) g -> p k g", p=128), writes=[w_rt], partial=True)
            g_moe = bc_load(st, "g_moe", ln_moe_d, D)
            b_rt = sb(st, "b_rt", [128, 72], F32)
            dma("sp", b_rt[:, 0:8], b_rg_d.partition_broadcast(128), writes=[b_rt], partial=True)
            dma("sp", b_rt[:, 8:72], b_re_d.partition_broadcast(128), writes=[b_rt], partial=True)
            hmbt = [sb(st, "hmbt%d" % i, [128, D], BF16) for i in range(2)]
            sg = [sb(st, "sg%d" % i, [128, 2048], BF16) for i in range(2)]
            xin = [sb(st, "xin%d" % i, [128, D], F32) for i in range(2)]
            ta_l = [sb(st, "ta%d" % _i, [128, D], F32) for _i in range(2)]
            tb_l = [sb(st, "tb%d" % _i, [128, D], F32) for _i in range(2)]
            mg_l = [sb(st, "mg%d" % _i, [128, D], BF16) for _i in range(2)]
            mT_l = [sb(st, "mT%d" % _i, [128, 8, 128], BF16) for _i in range(2)]
            x1t = [sb(st, "x1t%d" % i, [128, D], F32) for i in range(2)]
            sq_l = [sb(st, "sqD%d" % _i, [128, D], BF16) for _i in range(2)]
            ssq_l = [sb(st, "ssqD%d" % _i, [128, 4], F32) for _i in range(2)]
            rt_l = [sb(st, "rtD%d" % _i, [128, 4], F32) for _i in range(2)]
            rs_l = [sb(st, "rsD%d" % _i, [128, 4], F32) for _i in range(2)]
            hmf_l = [sb(st, "hmf%d" % _i, [128, D], F32) for _i in range(2)]
            hmT_l = [sb(st, "hmT%d" % _i, [128, 8, 128], F32) for _i in range(2)]
            lgt_l = [sb(st, "lgt%d" % _i, [128, 72], F32) for _i in range(2)]
            r8_l = [sb(st, "r8%d" % _i, [128, 8], F32) for _i in range(2)]
            gmx_l = [sb(st, "gmx%d" % _i, [128, 8], F32) for _i in range(2)]
            goh_l = [sb(st, "goh%d" % _i, [128, 8], F32) for _i in range(2)]
            gex_l = [sb(st, "gex%d" % _i, [128, 8], F32) for _i in range(2)]
            gsum_l = [sb(st, "gsum%d" % _i, [128, 2], F32) for _i in range(2)]
            pgrp_l = [sb(st, "pgrp%d" % _i, [128, 2], F32) for _i in range(2)]
            eml_l = [sb(st, "eml%d" % _i, [128, 64], F32) for _i in range(2)]
            pen_l = [sb(st, "pen%d" % _i, [128, 8], F32) for _i in range(2)]
            top8_l = [sb(st, "top8%d" % _i, [128, 8], F32) for _i in range(2)]
            dv_l = [sb(st, "dv%d" % _i, [128, 2], F32) for _i in range(2)]
            def d_tile(jt):
                ta = ta_l[(seq * NTS + jt) % 2]
                tb = tb_l[(seq * NTS + jt) % 2]
                mg = mg_l[(seq * NTS + jt) % 2]
                mT = mT_l[(seq * NTS + jt) % 2]
                sq = sq_l[(seq * NTS + jt) % 2]
                ssq = ssq_l[(seq * NTS + jt) % 2]
                rt = rt_l[(seq * NTS + jt) % 2]
                rs = rs_l[(seq * NTS + jt) % 2]
                hmf = hmf_l[(seq * NTS + jt) % 2]
                hmT = hmT_l[(seq * NTS + jt) % 2]
                lgt = lgt_l[(seq * NTS + jt) % 2]
                r8 = r8_l[(seq * NTS + jt) % 2]
                gmx = gmx_l[(seq * NTS + jt) % 2]
                goh = goh_l[(seq * NTS + jt) % 2]
                gex = gex_l[(seq * NTS + jt) % 2]
                gsum = gsum_l[(seq * NTS + jt) % 2]
                pgrp = pgrp_l[(seq * NTS + jt) % 2]
                eml = eml_l[(seq * NTS + jt) % 2]
                pen = pen_l[(seq * NTS + jt) % 2]
                top8 = top8_l[(seq * NTS + jt) % 2]
                dv = dv_l[(seq * NTS + jt) % 2]
                i = seq * NTS + jt
                tsl = slice(jt * 128, (jt + 1) * 128)
                s_ = sg[i % 2]
                x_ = xin[i % 2]
                x1_ = x1t[i % 2]
                dma("sp", s_[:, :], zg_d[i * 128:(i + 1) * 128, :], reads=[K.db("zg", i, gi) for gi in range(4)], writes=[s_])
                dma("sp", x_[:, :], x_d[seq, tsl, :], writes=[x_])
                yield
                ya = [bank(), bank()]
                for hf in range(2):
                    for hh in range(4):
                        op("pe", lambda e: e.matmul(ya[hf][0], lhsT=oaT[:, hh, tsl], rhs=w_oA[:, hh, hf * 512:(hf + 1) * 512],
                                                    start=(hh == 0), stop=(hh == 3)), reads=[oaT, w_oA], writes=[ya[hf][1]],
                           signal=(hh == 3))
                    op("dve", lambda e: e.tensor_tensor(out=ta[:, hf * 512:(hf + 1) * 512], in0=ya[hf][0],
                                                        in1=s_[:, hf * 512:(hf + 1) * 512], op=ALU.mult),
                       reads=[ya[hf][1], s_], writes=[ta], partial=True)
                yb = [bank(), bank()]
                for hf in range(2):
                    for hh in range(8):
                        op("pe", lambda e: e.matmul(yb[hf][0], lhsT=obT[:, hh, tsl], rhs=w_oB[:, hh, hf * 512:(hf + 1) * 512],
                                                    start=(hh == 0), stop=(hh == 7)), reads=[obT, w_oB], writes=[yb[hf][1]],
                           signal=(hh == 7))
                    op("dve", lambda e: e.tensor_tensor(out=tb[:, hf * 512:(hf + 1) * 512], in0=yb[hf][0],
                                                        in1=s_[:, 1024 + hf * 512:1024 + (hf + 1) * 512], op=ALU.mult),
                       reads=[yb[hf][1], s_], writes=[tb], partial=True)
                yield
                op("pool", lambda e: e.tensor_tensor(out=mg[:, :], in0=ta[:, :], in1=tb[:, :], op=ALU.add), reads=[ta, tb],
                   writes=[mg])
                pa, pb = bank()
                pbf = pa.bitcast(BF16)
                for kc in range(8):
                    op("pe", lambda e, kc=kc: e.transpose(out=pbf[:, kc * 128:(kc + 1) * 128], in_=mg[:, kc * 128:(kc + 1) * 128],
                                                          identity=ident_b[:, :]), reads=[mg, ident_b], writes=[pb],
                       signal=(kc == 7))
                op("act", lambda e: e.copy(out=mT[:, :, :], in_=pbf.rearrange("p (k t) -> p k t", k=8)), reads=[pb], writes=[mT])
                yield
                for hf in range(2):
                    pa, pb = bank()
                    for kc in range(8):
                        op("pe", lambda e, kc=kc: e.matmul(pa, lhsT=mT[:, kc, :], rhs=w_out[:, kc, hf * 512:(hf + 1) * 512],
                                                           start=(kc == 0), stop=(kc == 7)), reads=[mT, w_out], writes=[pb],
                           signal=(kc == 7))
                    op("dve", lambda e: e.tensor_tensor(out=x1_[:, hf * 512:(hf + 1) * 512], in0=pa,
                                                        in1=x_[:, hf * 512:(hf + 1) * 512], op=ALU.add), reads=[pb, x_],
                       writes=[x1_], partial=True)
                dma("sp", x1_d[i * 128:(i + 1) * 128, :], x1_[:, :], reads=[x1_], writes=[K.db("x1", i)])
                yield
                op("act", lambda e: e.activation(out=sq[:, :], in_=x1_[:, :], func=AF.Square), reads=[x1_], writes=[sq])
                op("dve", lambda e: e.tensor_reduce(out=ssq[:, 0:1], in_=sq[:, :], axis=AX.X, op=ALU.add), reads=[sq], writes=[ssq])
                rstd_from_ssq(ssq[:, 0:1], rt, rs[:, 0:1], D, [ssq, rs])
                op("dve", lambda e: e.scalar_tensor_tensor(out=hmf[:, :], in0=x1_[:, :], scalar=rs[:, 0:1], in1=g_moe[:, :],
                                                           op0=ALU.mult, op1=ALU.mult), reads=[x1_, rs, g_moe], writes=[hmf])
                hb_ = hmbt[i % 2]
                op("pool", lambda e: e.tensor_copy(out=hb_[:, :], in_=hmf[:, :]), reads=[hmf], writes=[hb_])
                dma("sp", hmb_d[i * 128:(i + 1) * 128, :], hb_[:, :], reads=[hb_], writes=[K.db("hmb", i)])
                yield
                for half in range(2):
                    pa, pb = bank()
                    for kc4 in range(4):
                        kc = half * 4 + kc4
                        op("pe", lambda e, kc=kc, kc4=kc4: e.transpose(out=pa[:, kc4 * 128:(kc4 + 1) * 128],
                                                                       in_=hmf[:, kc * 128:(kc + 1) * 128], identity=ident_f[:, :]),
                           reads=[hmf, ident_f], writes=[pb], signal=(kc4 == 3))
                    op("act", lambda e: e.copy(out=hmT[:, half * 4:(half + 1) * 4, :],
                                               in_=pa.rearrange("p (k t) -> p k t", k=4)), reads=[pb], writes=[hmT], partial=True)
                yield
                pa, pb = bank()
                for kc in range(8):
                    op("pe", lambda e, kc=kc: e.matmul(pa[:, 0:72], lhsT=hmT[:, kc, :], rhs=w_rt[:, kc, :], start=(kc == 0),
                                                       stop=(kc == 7)), reads=[hmT, w_rt], writes=[pb], signal=(kc == 7))
                op("dve", lambda e: e.tensor_tensor(out=lgt[:, :], in0=pa[:, 0:72], in1=b_rt[:, :], op=ALU.add), reads=[pb, b_rt],
                   writes=[lgt])
                op("dve", lambda e: e.max(out=gmx[:, :], in_=lgt[:, 0:8]), reads=[lgt], writes=[gmx])
                op("dve", lambda e: e.tensor_scalar(out=goh[:, :], in0=lgt[:, 0:8], scalar1=gmx[:, 0:1], scalar2=None,
                                                    op0=ALU.is_equal), reads=[lgt, gmx], writes=[goh])
                op("dve", lambda e: e.tensor_scalar(out=gex[:, :], in0=lgt[:, 0:8], scalar1=gmx[:, 0:1], scalar2=None,
                                                    op0=ALU.subtract), reads=[lgt, gmx], writes=[gex])
                op("act", lambda e: e.activation(out=gex[:, :], in_=gex[:, :], func=AF.Exp), reads=[gex], writes=[gex])
                op("dve", lambda e: e.tensor_reduce(out=gsum[:, 0:1], in_=gex[:, :], axis=AX.X, op=ALU.add), reads=[gex],
                   writes=[gsum])
                op("dve", lambda e: e.reciprocal(out=pgrp[:, 0:1], in_=gsum[:, 0:1]), reads=[gsum], writes=[pgrp])
                yield
                op("dve", lambda e: e.tensor_scalar(out=pen[:, :], in0=goh[:, :], scalar1=1.0e30, scalar2=-1.0e30, op0=ALU.mult,
                                                    op1=ALU.add), reads=[goh], writes=[pen])
                op("dve", lambda e: e.tensor_tensor(out=eml[:, :].rearrange("p (g j) -> p g j", g=8),
                                                    in0=lgt[:, 8:72].rearrange("p (g j) -> p g j", g=8),
                                                    in1=pen[:, :].unsqueeze(2).broadcast_to([128, 8, 8]), op=ALU.add),
                   reads=[lgt, pen], writes=[eml])
                op("dve", lambda e: e.max(out=top8[:, :], in_=eml[:, :]), reads=[eml], writes=[top8])
                for j2 in range(2):
                    op("dve", lambda e, j2=j2: e.tensor_scalar(out=Moh[:, i, j2, :], in0=eml[:, :], scalar1=top8[:, j2:j2 + 1],
                                                               scalar2=None, op0=ALU.is_equal), reads=[eml, top8],
                       writes=[Moh], partial=True)
                op("dve", lambda e: e.tensor_tensor(out=dv[:, 0:1], in0=top8[:, 0:1], in1=top8[:, 1:2], op=ALU.subtract),
                   reads=[top8], writes=[dv])
                op("dve", lambda e: e.tensor_tensor(out=dv[:, 1:2], in0=top8[:, 1:2], in1=top8[:, 0:1], op=ALU.subtract),
                   reads=[top8], writes=[dv])
                op("act", lambda e: e.activation(out=dv[:, :], in_=dv[:, :], func=AF.Sigmoid), reads=[dv], writes=[dv])
                op("dve", lambda e: e.tensor_scalar(out=wts[:, i, :], in0=dv[:, :], scalar1=pgrp[:, 0:1], scalar2=None,
                                                    op0=ALU.mult), reads=[dv, pgrp], writes=[wts], partial=True)
                yield
            pipeline([(lambda jt=jt: d_tile(jt)) for jt in range(NTS)], 2)
            K.barrier()
        seqst.close()
    if upto == "D":
        root.close()
        return nc

    with ExitStack() as st:
        Msum = sb(st, "Msum", [128, NT, 64], BF16)
        eoff_i = sb(st, "eoff_i", [128, 64], I32)
        eoff = sb(st, "eoff", [128, 64], F32)
        crk = sb(st, "crk", [128, 64], F32)
        junk = sb(st, "junk", [128, 64], F32)
        dest_f = sb(st, "dest_f", [128, NT, 2], F32)
        op("pool", lambda e: e.iota(eoff_i[:], pattern=[[CAP, 64]], base=0, channel_multiplier=0), writes=[eoff_i])
        op("dve", lambda e: e.tensor_copy(out=eoff[:], in_=eoff_i[:]), reads=[eoff_i], writes=[eoff])
        op("dve", lambda e: e.tensor_tensor(out=Msum[:], in0=Moh[:, :, 0, :], in1=Moh[:, :, 1, :], op=ALU.add), reads=[Moh],
           writes=[Msum])
        for i in range(NT):
            pa, pb = bank()
            for i2 in range(i + 1):
                lt = lstrict if i2 == i else ones_b
                op("pe", lambda e, i2=i2, lt=lt: e.matmul(pa[:, 0:64], lhsT=lt[:, :], rhs=Msum[:, i2, :], start=(i2 == 0),
                                                          stop=(i2 == i)), reads=[lt, Msum], writes=[pb], signal=(i2 == i))
            op("dve", lambda e: e.tensor_scalar(out=crk[:, :], in0=pa[:, 0:64], scalar1=float(CAP - 1), scalar2=None,
                                                op0=ALU.min), reads=[pb], writes=[crk])
            op("dve", lambda e: e.tensor_tensor(out=crk[:, :], in0=crk[:, :], in1=eoff[:, :], op=ALU.add), reads=[crk, eoff],
               writes=[crk])
            for j2 in range(2):
                op("dve", lambda e, j2=j2: e.tensor_tensor(out=junk[:, :], in0=crk[:, :], in1=Moh[:, i, j2, :], op=ALU.mult),
                   reads=[crk, Moh], writes=[junk])
                op("dve", lambda e, j2=j2: e.tensor_reduce(out=dest_f[:, i, j2:j2 + 1], in_=junk[:, :], axis=AX.X, op=ALU.add),
                   reads=[junk], writes=[dest_f], partial=True)
        op("dve", lambda e: e.tensor_copy(out=dest_i[:], in_=dest_f[:]), reads=[dest_f], writes=[dest_i])
        hst = [sb(st, "hst%d" % i, [128, D], BF16) for i in range(3)]
        for i in range(NT):
            hmb = hst[i % 3]
            dma("sp", hmb[:, :], hmb_d[i * 128:(i + 1) * 128, :], reads=[K.db("hmb", i)], writes=[hmb])
            for j2 in range(2):
                dma("pool", xs_d[:, :], hmb[:, :], reads=[hmb, dest_i], writes=[K.db("xs")], partial=True,
                    indirect=dict(out_offset=bass.IndirectOffsetOnAxis(ap=dest_i[:, i, j2:j2 + 1], axis=0), in_offset=None))
        K.barrier()

    with ExitStack() as st:
        NB = CAP // 128
        ws13 = [sb(st, "ws13_%d" % i, [128, 8, 512], F32) for i in range(3)]
        ws2 = [sb(st, "ws2_%d" % i, [128, 2, D], F32) for i in range(3)]
        wb13 = [sb(st, "wb13_%d" % i, [128, 8, 512], BF16) for i in range(2)]
        wb2 = [sb(st, "wb2_%d" % i, [128, 2, D], BF16) for i in range(2)]
        xsb = [sb(st, "xsb%d" % i, [128, NB, D], BF16) for i in range(3)]
        xsT = [sb(st, "xsT%d" % i, [128, 8, CAP], BF16) for i in range(2)]
        sl = [sb(st, "sl%d" % i, [128, 2, CAP], F32) for i in range(2)]
        hh_ = [sb(st, "hh%d" % i, [128, 2, CAP], BF16) for i in range(2)]
        ysb = [sb(st, "ysb%d" % i, [128, D], F32) for i in range(4)]
        yi = [0]

        def e_load(ex):
            a13, a2 = ws13[ex % 3], ws2[ex % 3]
            dma("sp", a13[:, :, 0:256], w1_d[ex].rearrange("(p k) f -> p k f", k=8), writes=[a13], partial=True)
            dma("sp", a13[:, :, 256:512], w3_d[ex].rearrange("(p k) f -> p k f", k=8), writes=[a13], partial=True)
            dma("sp", a2[:, :, :], w2_d[ex].rearrange("(c p) d -> p c d", p=128), writes=[a2])
            xb = xsb[ex % 3]
            dma("sp", xb[:, :, :], xs_d[ex * CAP:(ex + 1) * CAP, :].rearrange("(b p) d -> p b d", p=128),
                reads=[K.db("xs")], writes=[xb])

        def e_cast(ex):
            a13, a2, b13, b2 = ws13[ex % 3], ws2[ex % 3], wb13[ex % 2], wb2[ex % 2]
            op("dve", lambda e: e.tensor_copy(out=b13[:, 0:3, :], in_=a13[:, 0:3, :]), reads=[a13], writes=[b13], partial=True)
            op("act", lambda e: e.copy(out=b13[:, 3:6, :], in_=a13[:, 3:6, :]), reads=[a13], writes=[b13], partial=True)
            op("pool", lambda e: e.tensor_copy(out=b13[:, 6:8, :], in_=a13[:, 6:8, :]), reads=[a13], writes=[b13], partial=True)
            op("dve", lambda e: e.tensor_copy(out=b2[:, 0:1, :], in_=a2[:, 0:1, :]), reads=[a2], writes=[b2], partial=True)
            op("act", lambda e: e.copy(out=b2[:, 1:2, :], in_=a2[:, 1:2, :]), reads=[a2], writes=[b2], partial=True)

        def e_compute(ex):
            b13, b2 = wb13[ex % 2], wb2[ex % 2]
            xb, xT, hb, sl_ = xsb[ex % 3], xsT[ex % 2], hh_[ex % 2], sl[ex % 2]
            for b_ in range(NB):
                pa, pb = bank()
                pbf = pa.bitcast(BF16)
                xv = xb[:, b_, :].rearrange("p (q k) -> p k q", k=8)
                for kc in range(8):
                    op("pe", lambda e, kc=kc: e.transpose(out=pbf[:, kc * 128:(kc + 1) * 128], in_=xv[:, kc, :],
                                                          identity=ident_b[:, :]),
                       reads=[xb, ident_b], writes=[pb], signal=(kc == 7))
                if b_ % 2 == 0:
                    op("act", lambda e: e.copy(out=xT[:, :, b_ * 128:(b_ + 1) * 128], in_=pbf.rearrange("p (k t) -> p k t", k=8)),
                       reads=[pb], writes=[xT], partial=True)
                else:
                    op("dve", lambda e: e.tensor_copy(out=xT[:, :, b_ * 128:(b_ + 1) * 128],
                                                      in_=pbf.rearrange("p (k t) -> p k t", k=8)),
                       reads=[pb], writes=[xT], partial=True)
            ups = []
            for u in range(4):
                pa, pb = bank()
                for kc in range(8):
                    op("pe", lambda e, kc=kc: e.matmul(pa[:, 0:CAP], lhsT=b13[:, kc, u * 128:(u + 1) * 128], rhs=xT[:, kc, :],
                                                       start=(kc == 0), stop=(kc == 7)), reads=[b13, xT], writes=[pb],
                       signal=(kc == 7))
                ups.append((pa, pb))
            for c in range(2):
                op("act", lambda e: e.activation(out=sl_[:, c, :], in_=ups[c][0][:, 0:CAP], func=AF.Silu), reads=[ups[c][1]],
                   writes=[sl_], partial=True)
                op("dve", lambda e: e.tensor_tensor(out=hb[:, c, :], in0=ups[2 + c][0][:, 0:CAP], in1=sl_[:, c, :], op=ALU.mult),
                   reads=[ups[2 + c][1], sl_], writes=[hb], partial=True)
            for b_ in range(NB):
                y_ = ysb[yi[0] % 4]
                yi[0] += 1
                for hf in range(2):
                    pa, pb = bank()
                    for c in range(2):
                        op("pe", lambda e, c=c: e.matmul(pa, lhsT=hb[:, c, b_ * 128:(b_ + 1) * 128],
                                                         rhs=b2[:, c, hf * 512:(hf + 1) * 512], start=(c == 0), stop=(c == 1)),
                           reads=[hb, b2], writes=[pb], signal=(c == 1))
                    if hf == 0:
                        op("act", lambda e: e.copy(out=y_[:, 0:512], in_=pa), reads=[pb], writes=[y_], partial=True)
                    else:
                        op("dve", lambda e: e.tensor_copy(out=y_[:, 512:1024], in_=pa), reads=[pb], writes=[y_], partial=True)
                s0 = ex * CAP + b_ * 128
                dma("pool", ys_d[s0:s0 + 128, :], y_[:, :], reads=[y_], writes=[K.db("ys")], partial=True)

        e_load(0)
        e_load(1)
        e_cast(0)
        for ex in range(NEXP):
            if ex + 2 < NEXP:
                e_load(ex + 2)
            if ex + 1 < NEXP:
                e_cast(ex + 1)
            e_compute(ex)
        K.barrier()
    if upto == "E":
        root.close()
        return nc
    with ExitStack() as st:
        wstage2 = [sb(st, "wstF%d" % i, [128, 1024], F32) for i in range(2)]
        w_pg = sb(st, "w_pg", [128, 8, D], BF16)
        w_pp = sb(st, "w_pp", [128, 2, D], BF16)
        g_ple = bc_load(st, "g_ple", ln_ple_d, D)
        for kc in range(10):
            stg = wstage2[kc % 2]
            src = w_pg_d[kc * 128:(kc + 1) * 128, :] if kc < 8 else w_pp_d[(kc - 8) * 128:(kc - 7) * 128, :]
            dstT = w_pg if kc < 8 else w_pp
            dst = w_pg[:, kc, :] if kc < 8 else w_pp[:, kc - 8, :]
            dma("sp", stg[:, :], src, writes=[stg])
            op("dve" if kc % 2 == 0 else "pool", lambda e, dst=dst, stg=stg: e.tensor_copy(out=dst, in_=stg[:, :]), reads=[stg],
               writes=[dstT], partial=True)
        x1t = [sb(st, "x1F%d" % i, [128, D], F32) for i in range(2)]
        y1 = [sb(st, "y1F%d" % i, [128, D], F32) for i in range(2)]
        y2 = [sb(st, "y2F%d" % i, [128, D], F32) for i in range(2)]
        pt_ = [sb(st, "ptF%d" % i, [128, 256], F32) for i in range(2)]
        ptb_l = [sb(st, "ptb%d" % _i, [128, 256], BF16) for _i in range(2)]
        pT_l = [sb(st, "pT%d" % _i, [128, 2, 128], BF16) for _i in range(2)]
        sq_l = [sb(st, "sqF%d" % _i, [128, D], BF16) for _i in range(2)]
        ssq_l = [sb(st, "ssqF%d" % _i, [128, 4], F32) for _i in range(2)]
        rt_l = [sb(st, "rtF%d" % _i, [128, 4], F32) for _i in range(2)]
        rs_l = [sb(st, "rsF%d" % _i, [128, 4], F32) for _i in range(2)]
        hnb_l = [sb(st, "hnb%d" % _i, [128, D], BF16) for _i in range(2)]
        hnT_l = [sb(st, "hnT%d" % _i, [128, 8, 128], BF16) for _i in range(2)]
        gsb_l = [sb(st, "gsb%d" % _i, [128, D], F32) for _i in range(2)]
        ot = [sb(st, "otF%d" % i, [128, D], F32) for i in range(2)]
        def f_tile(i):
            ptb = ptb_l[i % 2]
            pT = pT_l[i % 2]
            sq = sq_l[i % 2]
            ssq = ssq_l[i % 2]
            rt = rt_l[i % 2]
            rs = rs_l[i % 2]
            hnb = hnb_l[i % 2]
            hnT = hnT_l[i % 2]
            gsb = gsb_l[i % 2]
            seq = i // NTS
            jt = i % NTS
            x_ = x1t[i % 2]
            o_ = ot[i % 2]
            dma("sp", x_[:, :], x1_d[i * 128:(i + 1) * 128, :], reads=[K.db("x1", i)], writes=[x_])
            dma("sp", pt_[i % 2][:, :], p_d[seq, jt * 128:(jt + 1) * 128, :], writes=[pt_[i % 2]])
            ys_ = [y1[i % 2], y2[i % 2]]
            for j2 in range(2):
                dma("pool", ys_[j2][:, :], ys_d[:, :], reads=[K.db("ys"), dest_i], writes=[ys_[j2]],
                    indirect=dict(out_offset=None, in_offset=bass.IndirectOffsetOnAxis(ap=dest_i[:, i, j2:j2 + 1], axis=0)))
            yield
            for j2 in range(2):
                op("dve", lambda e, j2=j2: e.scalar_tensor_tensor(out=x_[:, :], in0=ys_[j2][:, :], scalar=wts[:, i, j2:j2 + 1],
                                                                  in1=x_[:, :], op0=ALU.mult, op1=ALU.add),
                   reads=[ys_[j2], wts, x_], writes=[x_])
            yield
            op("act", lambda e: e.activation(out=sq[:, :], in_=x_[:, :], func=AF.Square), reads=[x_], writes=[sq])
            op("dve", lambda e: e.tensor_reduce(out=ssq[:, 0:1], in_=sq[:, :], axis=AX.X, op=ALU.add), reads=[sq], writes=[ssq])
            rstd_from_ssq(ssq[:, 0:1], rt, rs[:, 0:1], D, [ssq, rs])
            op("dve", lambda e: e.scalar_tensor_tensor(out=hnb[:, :], in0=x_[:, :], scalar=rs[:, 0:1], in1=g_ple[:, :],
                                                       op0=ALU.mult, op1=ALU.mult), reads=[x_, rs, g_ple], writes=[hnb])
            yield
            pa, pb = bank()
            pbf = pa.bitcast(BF16)
            for kc in range(8):
                op("pe", lambda e, kc=kc: e.transpose(out=pbf[:, kc * 128:(kc + 1) * 128], in_=hnb[:, kc * 128:(kc + 1) * 128],
                                                      identity=ident_b[:, :]), reads=[hnb, ident_b], writes=[pb], signal=(kc == 7))
            op("act", lambda e: e.copy(out=hnT[:, :, :], in_=pbf.rearrange("p (k t) -> p k t", k=8)), reads=[pb], writes=[hnT])
            yield
            op("pool", lambda e: e.tensor_copy(out=ptb[:, :], in_=pt_[i % 2][:, :]), reads=[pt_[i % 2]], writes=[ptb])
            pa, pb = bank()
            pbf = pa.bitcast(BF16)
            for kc in range(2):
                op("pe", lambda e, kc=kc: e.transpose(out=pbf[:, kc * 128:(kc + 1) * 128], in_=ptb[:, kc * 128:(kc + 1) * 128],
                                                      identity=ident_b[:, :]), reads=[ptb, ident_b], writes=[pb], signal=(kc == 1))
            op("act", lambda e: e.copy(out=pT[:, :, :], in_=pbf[:, 0:256].rearrange("p (k t) -> p k t", k=2)), reads=[pb],
               writes=[pT])
            yield
            for hf in range(2):
                pg, pgb = bank()
                for kc in range(8):
                    op("pe", lambda e, kc=kc: e.matmul(pg, lhsT=hnT[:, kc, :], rhs=w_pg[:, kc, hf * 512:(hf + 1) * 512],
                                                       start=(kc == 0), stop=(kc == 7)), reads=[hnT, w_pg], writes=[pgb],
                       signal=(kc == 7))
                op("act", lambda e: e.activation(out=gsb[:, hf * 512:(hf + 1) * 512], in_=pg, func=AF.Sigmoid), reads=[pgb],
                   writes=[gsb], partial=True)
                pp, ppb = bank()
                for kc in range(2):
                    op("pe", lambda e, kc=kc: e.matmul(pp, lhsT=pT[:, kc, :], rhs=w_pp[:, kc, hf * 512:(hf + 1) * 512],
                                                       start=(kc == 0), stop=(kc == 1)), reads=[pT, w_pp], writes=[ppb],
                       signal=(kc == 1))
                op("dve", lambda e: e.tensor_tensor(out=o_[:, hf * 512:(hf + 1) * 512], in0=pp,
                                                    in1=gsb[:, hf * 512:(hf + 1) * 512], op=ALU.mult), reads=[ppb, gsb],
                   writes=[o_], partial=True)
            yield
            op("pool", lambda e: e.tensor_tensor(out=o_[:, :], in0=o_[:, :], in1=x_[:, :], op=ALU.add), reads=[o_, x_],
               writes=[o_])
            dma("sp", out_d[seq, jt * 128:(jt + 1) * 128, :], o_[:, :], reads=[o_], writes=[K.db("out", i)])
            yield
        pipeline([(lambda i=i: f_tile(i)) for i in range(NT)], 2)
        K.barrier()
    root.close()
    return nc


WNAMES = ["ln_mix", "w_in", "hg_lb", "hg_onorm", "w_oA", "mla_qa_norm", "mla_kva_norm", "w_uq", "w_ukv", "q_norm",
          "k_norm", "w_oB", "w_out", "ln_moe", "w_rg", "b_rg", "w_re", "b_re", "w1", "w3", "w2", "ln_ple",
          "w_ple_gate", "w_ple_proj"]


def make_in_maps(inputs, n_cores, NS):
    shared = {}
    for n in WNAMES:
        a = np.ascontiguousarray(np.asarray(inputs[n], dtype=np.float32))
        if n == "hg_lb":
            shared[n] = a
        else:
            shared[n] = a.reshape(a.shape[1:]) if a.shape[0] == 1 else a
    for n in ("ln_mix", "hg_onorm", "mla_qa_norm", "mla_kva_norm", "q_norm", "k_norm", "ln_moe", "b_rg", "b_re", "ln_ple"):
        shared[n] = shared[n].reshape(-1)
    x = np.asarray(inputs["x"], dtype=np.float32)
    p = np.asarray(inputs["p"], dtype=np.float32)[0]
    pos = np.asarray(inputs["positions"], dtype=np.int32)
    maps = []
    for c in range(n_cores):
        m = dict(shared)
        m["x"] = np.ascontiguousarray(x[c * NS:(c + 1) * NS])
        m["p"] = np.ascontiguousarray(p[c * NS:(c + 1) * NS])
        m["positions"] = np.ascontiguousarray(pos[c * NS:(c + 1) * NS])
        maps.append(m)
    return maps


def kernel(**inputs):
    n = 8
    NS = 2
    nc = build(NS=NS, S=2048, CAP=256, debug=True)
    maps = make_in_maps(inputs, n, NS)
    res = run_bass_kernel_spmd(nc, maps, core_ids=list(range(n)))
    return np.concatenate([np.asarray(r["out"]) for r in res.results], axis=0).astype(np.float32)
```

```python
import numpy as np
from contextlib import ExitStack
import concourse.bass as bass
import concourse.mybir as mybir
from concourse.alu_op_type import AluOpType as ALU
from concourse.bass_utils import run_bass_kernel_spmd

F32 = mybir.dt.float32
BF16 = mybir.dt.bfloat16
I32 = mybir.dt.int32
AF = mybir.ActivationFunctionType
AX = mybir.AxisListType

D = 1024
DIN = 5024
NEXP = 64
EPS = 1e-6
CH = 64


class Buf:
    __slots__ = ("w", "r")

    def __init__(self):
        self.w = {}
        self.r = {}


class T:
    def __init__(self, t):
        self.t = t
        self.b = Buf()

    def __getitem__(self, k):
        return self.t[k]


class Eng:
    def __init__(self, name, eng, sem):
        self.name = name
        self.eng = eng
        self.sem = sem
        self.cnt = 0
        self.seen = {}
        self.pr = []
        self.pw = []


class Ring:
    def __init__(self, nc, q, P):
        self.P = P
        self.sems = [nc.alloc_semaphore("dq_%s_%d" % (q, i)) for i in range(P)]
        self.cnt = [0] * P
        self.last = [None] * P
        self.n = 0


class KB:
    def __init__(self, nc):
        self.nc = nc
        self.engs = {}
        for name, e in (("pe", nc.tensor), ("act", nc.scalar), ("dve", nc.vector),
                        ("pool", nc.gpsimd), ("sp", nc.sync)):
            self.engs[name] = Eng(name, e, nc.alloc_semaphore("s_" + name))
        self.rings = {"sp": Ring(nc, "sp", 24), "act": Ring(nc, "act", 12), "pool": Ring(nc, "pool", 12)}
        self.dbufs = {}

    def db(self, *key):
        b = self.dbufs.get(key)
        if b is None:
            b = Buf()
            self.dbufs[key] = b
        return b

    def wait(self, en, tok):
        sem, val = tok
        E = self.engs[en]
        k = id(sem)
        if E.seen.get(k, 0) >= val:
            return
        E.eng.wait_ge(sem, val)
        E.seen[k] = val

    def _deps(self, en, reads, writes):
        for b in reads:
            for t in b.w.values():
                self.wait(en, t)
        for b in writes:
            for t in b.w.values():
                self.wait(en, t)
            for t in b.r.values():
                self.wait(en, t)

    @staticmethod
    def _rec(tok, reads, writes, partial):
        k = id(tok[0])
        for b in reads:
            b.r[k] = tok
        for b in writes:
            if partial:
                b.w[k] = tok
            else:
                b.w = {k: tok}
            b.r = {}

    def op(self, en, emit, reads=(), writes=(), signal=True, partial=False):
        E = self.engs[en]
        reads = [x.b if isinstance(x, T) else x for x in reads]
        writes = [x.b if isinstance(x, T) else x for x in writes]
        self._deps(en, reads, writes)
        ins = emit(E.eng)
        if signal:
            E.cnt += 1
            ins.then_inc(E.sem, 1)
            tok = (E.sem, E.cnt)
            self._rec(tok, E.pr + reads, [], False)
            self._rec(tok, [], E.pw + writes, partial)
            E.pr = []
            E.pw = []
        else:
            E.pr += reads
            E.pw += writes
        return ins

    def dma(self, q, out, in_, reads=(), writes=(), partial=False, indirect=None, **kw):
        E = self.engs[q]
        R = self.rings[q]
        reads = [x.b if isinstance(x, T) else x for x in reads]
        writes = [x.b if isinstance(x, T) else x for x in writes]
        self._deps(q, reads, writes)
        slot = R.n % R.P
        if R.last[slot] is not None:
            self.wait(q, R.last[slot])
        R.cnt[slot] += 16
        tok = (R.sems[slot], R.cnt[slot])
        if indirect is None:
            ins = E.eng.dma_start(out=out, in_=in_, **kw)
        else:
            ins = E.eng.indirect_dma_start(out=out, in_=in_, **indirect)
        ins.then_inc(R.sems[slot], 16)
        R.last[slot] = tok
        R.n += 1
        self._rec(tok, reads, writes, partial)
        return tok

    def barrier(self):
        toks = [(E.sem, E.cnt) for E in self.engs.values() if E.cnt > 0]
        for R in self.rings.values():
            toks += [t for t in R.last if t is not None]
        for en in self.engs:
            for t in toks:
                self.wait(en, t)


def build(NS=2, S=2048, CAP=256, debug=False, upto="F"):
    nc = bass.Bass("TRN2", target_bir_lowering=False)
    TT = NS * S
    NT = TT // 128
    NTS = S // 128
    NCH = S // CH
    NSLOT = NEXP * CAP
    assert S % 512 == 0

    def din(name, shape, dt=F32):
        return nc.dram_tensor(name, list(shape), dt, kind="ExternalInput").ap()

    def dscr(name, shape, dt):
        return nc.dram_tensor(name, list(shape), dt, kind="ExternalOutput" if debug else "Internal").ap()

    x_d = din("x", [NS, S, D])
    p_d = din("p", [NS, S, 256])
    pos_d = din("positions", [NS, S], I32)
    ln_mix_d = din("ln_mix", [D])
    w_in_d = din("w_in", [D, DIN])
    hg_lb_d = din("hg_lb", [2, 2, 512])
    hg_onorm_d = din("hg_onorm", [128])
    w_oA_d = din("w_oA", [512, D])
    qa_norm_d = din("mla_qa_norm", [256])
    kva_norm_d = din("mla_kva_norm", [128])
    w_uq_d = din("w_uq", [256, 768])
    w_ukv_d = din("w_ukv", [128, 1024])
    q_norm_d = din("q_norm", [96])
    k_norm_d = din("k_norm", [96])
    w_oB_d = din("w_oB", [512, D])
    w_out_d = din("w_out", [D, D])
    ln_moe_d = din("ln_moe", [D])
    w_rg_d = din("w_rg", [D, 8])
    b_rg_d = din("b_rg", [8])
    w_re_d = din("w_re", [D, 64])
    b_re_d = din("b_re", [64])
    w1_d = din("w1", [NEXP, D, 256])
    w3_d = din("w3", [NEXP, D, 256])
    w2_d = din("w2", [NEXP, 256, D])
    ln_ple_d = din("ln_ple", [D])
    w_pg_d = din("w_ple_gate", [D, D])
    w_pp_d = din("w_ple_proj", [256, D])
    out_d = nc.dram_tensor("out", [NS, S, D], F32, kind="ExternalOutput").ap()

    zq_d = dscr("zq", [NS, 4, 128, S], BF16)
    zf_d = dscr("zf", [NS, 2, 4, 128, S], F32)
    zv_d = dscr("zv", [TT, 512], BF16)
    zog_d = dscr("zog", [TT, 512], BF16)
    zg_d = dscr("zg", [TT, 2048], BF16)
    qT_d = dscr("qT", [NS, 96, 8, S], BF16)
    kT_d = dscr("kT", [NS, 96, 8, S], BF16)
    vm_d = dscr("vm", [TT, 8, 64], BF16)
    x1_d = dscr("x1", [TT, D], F32)
    hmb_d = dscr("hmb", [TT, D], BF16)
    xs_d = dscr("xs", [NSLOT, D], BF16)
    ys_d = dscr("ys", [NSLOT, D], F32)

    K = KB(nc)

    def pipeline(makers, W):
        active = []
        it = iter(makers)
        more = True
        while True:
            while len(active) < W and more:
                try:
                    active.append(next(it)())
                except StopIteration:
                    more = False
            if not active:
                break
            for g_ in list(active):
                try:
                    next(g_)
                except StopIteration:
                    active.remove(g_)
    op = K.op
    dma = K.dma
    root = ExitStack()
    right_stacks = []
    uid = [0]

    def sb(st, name, shape, dt, side="left"):
        if st is root or st in right_stacks:
            side = "right"
        uid[0] += 1
        return T(st.enter_context(nc.sbuf_tensor("sb%d_%s" % (uid[0], name), list(shape), dt, side=side)))

    ps_all = root.enter_context(nc.psum_tensor("ps_all", [128, 4096], F32))
    banks = [Buf() for _ in range(8)]
    bank_i = [0]

    reserved = set()

    def bank():
        while True:
            i = bank_i[0] % 8
            bank_i[0] += 1
            if i not in reserved:
                break
        return ps_all[:, i * 512:(i + 1) * 512], banks[i]

    def fixed_bank(i):
        return ps_all[:, i * 512:(i + 1) * 512], banks[i]

    dif_i = sb(root, "dif_i", [128, 128], I32)
    dif = sb(root, "dif", [128, 128], F32)
    ident_b = sb(root, "ident_b", [128, 128], BF16)
    ident_f = sb(root, "ident_f", [128, 128], F32)
    maskf = sb(root, "maskf", [128, 128], F32)
    maskb = sb(root, "maskb", [128, 128], F32)
    lstrict = sb(root, "lstrict", [128, 128], BF16)
    ones_b = sb(root, "ones_b", [128, 128], BF16)
    sel = sb(root, "sel", [128, 64], F32)
    op("pool", lambda e: e.iota(dif_i[:], pattern=[[1, 128]], base=0, channel_multiplier=-1), writes=[dif_i])
    op("dve", lambda e: e.tensor_copy(out=dif[:], in_=dif_i[:]), reads=[dif_i], writes=[dif])
    for dst, cmp_ in ((ident_b, ALU.is_equal), (ident_f, ALU.is_equal), (maskf, ALU.is_ge),
                      (maskb, ALU.is_le), (lstrict, ALU.is_gt)):
        op("dve", lambda e, dst=dst, cmp_=cmp_: e.tensor_single_scalar(out=dst[:], in_=dif[:], scalar=0.0, op=cmp_),
           reads=[dif], writes=[dst])
    op("dve", lambda e: e.memset(ones_b[:], 1.0), writes=[ones_b])
    op("dve", lambda e: e.memset(sel[:], 0.0), writes=[sel])
    op("dve", lambda e: e.memset(sel[64:65, :], 1.0), writes=[sel])

    def bc_load(st, name, src1d, n):
        t = sb(st, name, [128, n], F32)
        dma("sp", t[:], src1d.partition_broadcast(128), writes=[t])
        return t

    def rstd_from_ssq(ssq_ap, tmp, out_ap, dim, bufs):
        P_, n_ = ssq_ap.shape[0], ssq_ap.shape[1]
        op("dve", lambda e: e.tensor_scalar(out=tmp[0:P_, 0:n_], in0=ssq_ap, scalar1=1.0 / dim, scalar2=EPS,
                                            op0=ALU.mult, op1=ALU.add), reads=bufs, writes=[tmp])
        op("act", lambda e: e.activation(out=tmp[0:P_, 0:n_], in_=tmp[0:P_, 0:n_], func=AF.Sqrt),
           reads=[tmp], writes=[tmp])
        op("dve", lambda e: e.reciprocal(out=out_ap, in_=tmp[0:P_, 0:n_]), reads=[tmp], writes=bufs)

    def tmp_ap(tmp, like):
        n = like.shape[-1] if len(like.shape) == 2 else None
        return tmp[0:like.shape[0], 0:like.shape[1]]

    phA = ExitStack()
    Win = sb(phA, "Win", [128, 8, DIN], BF16)
    wstage = [sb(phA, "wstage%d" % i, [128, 1024], F32) for i in range(3)]
    cast_engs = ["dve", "pool", "act"]
    cast_i = [0]
    wst_cur = [wstage]

    def load_cast(dst_ap, dst_T, src_ap, ncols, npart=128):
        i = cast_i[0]
        cast_i[0] += 1
        stg = wst_cur[0][i % 3]
        if npart != 128:
            dma("sp", stg[0:npart, 0:ncols], src_ap, writes=[stg])
            op("dve", lambda e: e.tensor_copy(out=dst_ap, in_=stg[0:npart, 0:ncols]), reads=[stg], writes=[dst_T],
               partial=True)
            return
        dma("sp", stg[:, 0:ncols], src_ap, writes=[stg])
        en = cast_engs[i % 3]
        if en == "act":
            op("act", lambda e: e.copy(out=dst_ap, in_=stg[:, 0:ncols]), reads=[stg], writes=[dst_T], partial=True)
        else:
            op(en, lambda e: e.tensor_copy(out=dst_ap, in_=stg[:, 0:ncols]), reads=[stg], writes=[dst_T], partial=True)

    for kc in range(8):
        for c0 in range(0, DIN, 1024):
            c1 = min(DIN, c0 + 1024)
            load_cast(Win[:, kc, c0:c1], Win, w_in_d[kc * 128:(kc + 1) * 128, c0:c1], c1 - c0)

    mixW = phA
    w_uq = sb(mixW, "w_uq", [128, 2, 768], BF16)
    w_ukv = sb(mixW, "w_ukv", [128, 1024], BF16)
    for kc in range(2):
        load_cast(w_uq[:, kc, :], w_uq, w_uq_d[kc * 128:(kc + 1) * 128, :], 768)
    load_cast(w_ukv[:, :], w_ukv, w_ukv_d[:, :], 1024)
    g_mix = bc_load(mixW, "g_mix", ln_mix_d, D)
    g_qa = bc_load(mixW, "g_qa", qa_norm_d, 256)
    g_kva = bc_load(mixW, "g_kva", kva_norm_d, 128)
    g_qn = bc_load(mixW, "g_qn", q_norm_d, 96)
    g_kn = bc_load(mixW, "g_kn", k_norm_d, 96)
    g_on = bc_load(root, "g_on", hg_onorm_d, 128)

    lbraw = sb(root, "lbraw", [128, 16], F32)
    lb = sb(root, "lb", [128, 8], F32)
    oml = sb(root, "oml", [128, 8], F32)
    with nc.allow_non_contiguous_dma(reason="tiny param load"):
        dma("sp", lbraw[:, :], hg_lb_d.rearrange("d l (h k) -> k (d l h)", k=128), writes=[lbraw])
    lbv = lbraw[:, :].rearrange("p (d l h) -> p d l h", d=2, l=2)
    op("dve", lambda e: e.tensor_tensor(out=lb[:, :].rearrange("p (d h) -> p d h", d=2), in0=lbv[:, :, 0, :],
                                        in1=lbv[:, :, 1, :], op=ALU.subtract), reads=[lbraw], writes=[lb])
    op("act", lambda e: e.activation(out=lb[:, :], in_=lb[:, :], func=AF.Sigmoid), reads=[lb], writes=[lb])
    op("dve", lambda e: e.tensor_scalar(out=oml[:, :], in0=lb[:, :], scalar1=-1.0, scalar2=1.0, op0=ALU.mult,
                                        op1=ALU.add), reads=[lb], writes=[oml])

    pos_r = sb(mixW, "pos_r", [NT, 128], I32)
    dma("sp", pos_r[:, :], pos_d.rearrange("s (n p) -> (s n) p", p=128), writes=[pos_r])
    posr_f = sb(mixW, "posr_f", [NT, 128], F32)
    op("dve", lambda e: e.tensor_copy(out=posr_f[:, :], in_=pos_r[:, :]), reads=[pos_r], writes=[posr_f])
    posf = sb(mixW, "posf", [128, NT], F32)
    pa_, pb_ = bank()
    op("pe", lambda e: e.transpose(out=pa_[:, 0:NT], in_=posr_f[:, :], identity=ident_f[0:NT, 0:NT]),
       reads=[posr_f, ident_f], writes=[pb_])
    op("dve", lambda e: e.tensor_copy(out=posf[:, :], in_=pa_[:, 0:NT]), reads=[pb_], writes=[posf])
    invf = sb(mixW, "invf", [128, 16], F32)
    for i in range(16):
        v = float(10000.0 ** (-(i / 16.0)) / (2.0 * np.pi))
        op("dve", lambda e, i=i, v=v: e.memset(invf[:, i:i + 1], v), writes=[invf], partial=True)
    rope_cs = sb(mixW, "rope_cs", [128, NT, 32], F32)
    with ExitStack() as st0:
        turns = sb(st0, "turns", [128, NT, 32], F32)
        ti = sb(st0, "turns_i", [128, NT, 32], I32)
        tf = sb(st0, "turns_f", [128, NT, 32], F32)
        adj = sb(st0, "adj", [128, NT, 32], F32)
        op("dve", lambda e: e.tensor_tensor(out=turns[:, :, 16:32], in0=posf[:, :].unsqueeze(2).broadcast_to([128, NT, 16]),
                                            in1=invf[:, :].unsqueeze(1).broadcast_to([128, NT, 16]), op=ALU.mult),
           reads=[posf, invf], writes=[turns])
        op("dve", lambda e: e.tensor_scalar(out=turns[:, :, 0:16], in0=turns[:, :, 16:32], scalar1=0.25, scalar2=None,
                                            op0=ALU.add), reads=[turns], writes=[turns])
        op("dve", lambda e: e.tensor_copy(out=ti[:], in_=turns[:]), reads=[turns], writes=[ti])
        op("dve", lambda e: e.tensor_copy(out=tf[:], in_=ti[:]), reads=[ti], writes=[tf])
        op("dve", lambda e: e.tensor_tensor(out=turns[:], in0=turns[:], in1=tf[:], op=ALU.subtract), reads=[turns, tf],
           writes=[turns])
        op("dve", lambda e: e.tensor_single_scalar(out=adj[:], in_=turns[:], scalar=0.5, op=ALU.is_gt), reads=[turns],
           writes=[adj])
        op("dve", lambda e: e.tensor_tensor(out=turns[:], in0=turns[:], in1=adj[:], op=ALU.subtract), reads=[turns, adj],
           writes=[turns])
        op("dve", lambda e: e.tensor_single_scalar(out=adj[:], in_=turns[:], scalar=-0.5, op=ALU.is_lt), reads=[turns],
           writes=[adj])
        op("dve", lambda e: e.tensor_tensor(out=turns[:], in0=turns[:], in1=adj[:], op=ALU.add), reads=[turns, adj],
           writes=[turns])
        op("dve", lambda e: e.tensor_scalar(out=turns[:], in0=turns[:], scalar1=0.4999999, scalar2=-0.4999999,
                                            op0=ALU.min, op1=ALU.max), reads=[turns], writes=[turns])
        op("act", lambda e: e.activation(out=rope_cs[:], in_=turns[:], func=AF.Sin, scale=float(2.0 * np.pi)),
           reads=[turns], writes=[rope_cs])
        K.barrier()

    zt = sb(root, "zt", [128, D], BF16)
    op("pool", lambda e: e.memset(zt[:, :], 0.0), writes=[zt])
    xs_v = xs_d.rearrange("(a p) d -> p a d", p=128)
    NA = NSLOT // 128
    for a0 in range(0, NA, 16):
        a1 = min(NA, a0 + 16)
        dma("sp", xs_v[:, a0:a1, :], zt[:, :].unsqueeze(1).broadcast_to([128, a1 - a0, D]), reads=[zt],
            writes=[K.db("xs")], partial=True)

    stg_i = [0]
    with ExitStack() as st:
        xt = [sb(st, "xt%d" % i, [128, D], F32) for i in range(2)]
        sq = sb(st, "sq", [128, D], BF16)
        ssq = sb(st, "ssq", [128, 4], F32)
        rt = sb(st, "rt", [128, 16], F32)
        rs = sb(st, "rs", [128, 4], F32)
        hn = [sb(st, "hn%d" % i, [128, D], BF16) for i in range(2)]
        hT4 = [sb(st, "hT4_%d" % i, [128, 8, 512], BF16) for i in range(2)]
        stg = [sb(st, "stg%d" % i, [128, 512], F32) for i in range(6)]
        stgb = [sb(st, "stgb%d" % i, [128, 512], BF16) for i in range(6)]
        cz = [sb(st, "cz%d" % i, [128, 416], F32) for i in range(2)]
        czs = sb(st, "czs", [128, 384], BF16)
        cn = sb(st, "cn", [128, 384], BF16)
        cT = sb(st, "cT", [128, 3, 128], BF16)
        qk = sb(st, "qk", [128, 2, 8, 96], F32)
        qk2 = sb(st, "qk2", [128, 2, 8, 96], BF16)
        qss = sb(st, "qss", [128, 16], F32)
        qrs = sb(st, "qrs", [128, 16], F32)
        qkn = sb(st, "qkn", [128, 2, 8, 96], F32)
        qkb = sb(st, "qkb", [128, 2, 8, 96], BF16)
        rtmp = [sb(st, "rtmp%d" % i, [128, 2, 8, 16], F32) for i in range(4)]
        qkT = [sb(st, "qkT%d" % i, [96, 8, 128], BF16) for i in range(2)]
        vmb = sb(st, "vmb", [128, 8, 64], BF16)

        def next_stg(bf):
            i = stg_i[0]
            stg_i[0] += 1
            return (stgb if bf else stg)[i % 6]

        NG = TT // 512
        for g in range(NG):
            seq = (g * 512) // S
            t0 = (g * 512) % S
            h4 = hT4[g % 2]
            for j in range(4):
                i = g * 4 + j
                x_ = xt[i % 2]
                h_ = hn[i % 2]
                dma("sp", x_[:, :], x_d[seq, t0 + j * 128:t0 + (j + 1) * 128, :], writes=[x_])
                op("act", lambda e: e.activation(out=sq[:, :], in_=x_[:, :], func=AF.Square), reads=[x_], writes=[sq])
                op("dve", lambda e: e.tensor_reduce(out=ssq[:, 0:1], in_=sq[:, :], axis=AX.X, op=ALU.add), reads=[sq],
                   writes=[ssq])
                rstd_from_ssq(ssq[:, 0:1], rt, rs[:, 0:1], D, [ssq, rs])
                op("dve", lambda e: e.scalar_tensor_tensor(out=h_[:, :], in0=x_[:, :], scalar=rs[:, 0:1], in1=g_mix[:, :],
                                                           op0=ALU.mult, op1=ALU.mult), reads=[x_, rs, g_mix], writes=[h_])
                pa, pb = bank()
                pbf = pa.bitcast(BF16)
                for kc in range(8):
                    op("pe", lambda e, kc=kc: e.transpose(out=pbf[:, kc * 128:(kc + 1) * 128],
                                                          in_=h_[:, kc * 128:(kc + 1) * 128], identity=ident_b[:, :]),
                       reads=[h_, ident_b], writes=[pb], signal=(kc == 7))
                op("act", lambda e: e.copy(out=h4[:, :, j * 128:(j + 1) * 128],
                                           in_=pbf.rearrange("p (k t) -> p k t", k=8)), reads=[pb], writes=[h4],
                   partial=True)

            for c in range(12):
                pa, pb = bank()
                for kc in range(8):
                    op("pe", lambda e, kc=kc: e.matmul(pa, lhsT=Win[:, kc, c * 128:(c + 1) * 128], rhs=h4[:, kc, :],
                                                       start=(kc == 0), stop=(kc == 7)),
                       reads=[Win, h4], writes=[pb], signal=(kc == 7))
                if c < 4:
                    s_ = next_stg(True)
                    op("act", lambda e: e.activation(out=s_[:, :], in_=pa, func=AF.Silu), reads=[pb], writes=[s_])
                    dma("act", zq_d[seq, c, :, t0:t0 + 512], s_[:, :], reads=[s_], writes=[K.db("zq", seq, c, g)])
                else:
                    d_ = (c - 4) // 4
                    h_i = (c - 4) % 4
                    s_ = next_stg(False)
                    op("dve", lambda e: e.tensor_copy(out=s_[:, :], in_=pa), reads=[pb], writes=[s_])
                    dma("sp", zf_d[seq, d_, h_i, :, t0:t0 + 512], s_[:, :], reads=[s_],
                        writes=[K.db("zf", seq, d_, h_i, g)])

            for j in range(4):
                i = g * 4 + j
                tok0 = i * 128
                lts = h4[:, :, j * 128:(j + 1) * 128]
                groups = [(1536, 2048, "v"), (2048, 2560, "og"), (2560, 2976, "c"), (2976, 3488, "g0"),
                          (3488, 4000, "g1"), (4000, 4512, "g2"), (4512, 5024, "g3")]
                for (c0, c1, kind) in groups:
                    pa, pb = bank()
                    n = c1 - c0
                    for kc in range(8):
                        op("pe", lambda e, kc=kc: e.matmul(pa[:, 0:n], lhsT=lts[:, kc, :], rhs=Win[:, kc, c0:c1],
                                                           start=(kc == 0), stop=(kc == 7)),
                           reads=[Win, h4], writes=[pb], signal=(kc == 7))
                    if kind == "v":
                        s_ = next_stg(True)
                        op("dve", lambda e: e.tensor_copy(out=s_[:, :], in_=pa), reads=[pb], writes=[s_])
                        dma("sp", zv_d[tok0:tok0 + 128, :], s_[:, :], reads=[s_], writes=[K.db("zv", i)])
                    elif kind == "og":
                        s_ = next_stg(True)
                        op("act", lambda e: e.activation(out=s_[:, :], in_=pa, func=AF.Silu), reads=[pb], writes=[s_])
                        dma("act", zog_d[tok0:tok0 + 128, :], s_[:, :], reads=[s_], writes=[K.db("zog", i)])
                    elif kind[0] == "g":
                        gi = int(kind[1])
                        s_ = next_stg(True)
                        op("act", lambda e: e.activation(out=s_[:, :], in_=pa, func=AF.Sigmoid), reads=[pb], writes=[s_])
                        dma("act", zg_d[tok0:tok0 + 128, gi * 512:(gi + 1) * 512], s_[:, :], reads=[s_],
                            writes=[K.db("zg", i, gi)])
                    else:
                        c_ = cz[i % 2]
                        op("dve", lambda e: e.tensor_copy(out=c_[:, :], in_=pa[:, 0:416]), reads=[pb], writes=[c_])
                        op("act", lambda e: e.activation(out=czs[:, :], in_=c_[:, 0:384], func=AF.Square), reads=[c_],
                           writes=[czs])
                        op("dve", lambda e: e.tensor_reduce(out=ssq[:, 1:2], in_=czs[:, 0:256], axis=AX.X, op=ALU.add),
                           reads=[czs], writes=[ssq])
                        op("dve", lambda e: e.tensor_reduce(out=ssq[:, 2:3], in_=czs[:, 256:384], axis=AX.X, op=ALU.add),
                           reads=[czs], writes=[ssq])
                        rstd_from_ssq(ssq[:, 1:2], rt, rs[:, 1:2], 256, [ssq, rs])
                        rstd_from_ssq(ssq[:, 2:3], rt, rs[:, 2:3], 128, [ssq, rs])
                        op("dve", lambda e: e.scalar_tensor_tensor(out=cn[:, 0:256], in0=c_[:, 0:256], scalar=rs[:, 1:2],
                                                                   in1=g_qa[:, :], op0=ALU.mult, op1=ALU.mult),
                           reads=[c_, rs, g_qa], writes=[cn])
                        op("dve", lambda e: e.scalar_tensor_tensor(out=cn[:, 256:384], in0=c_[:, 256:384], scalar=rs[:, 2:3],
                                                                   in1=g_kva[:, :], op0=ALU.mult, op1=ALU.mult),
                           reads=[c_, rs, g_kva], writes=[cn])
                        pa2, pb2 = bank()
                        pbf2 = pa2.bitcast(BF16)
                        for kc in range(3):
                            op("pe", lambda e, kc=kc: e.transpose(out=pbf2[:, kc * 128:(kc + 1) * 128],
                                                                  in_=cn[:, kc * 128:(kc + 1) * 128], identity=ident_b[:, :]),
                               reads=[cn, ident_b], writes=[pb2], signal=(kc == 2))
                        op("act", lambda e: e.copy(out=cT[:, :, :], in_=pbf2[:, 0:384].rearrange("p (k t) -> p k t", k=3)),
                           reads=[pb2], writes=[cT])
                        pq0, pqb0 = bank()
                        pq1, pqb1 = bank()
                        for kc in range(2):
                            op("pe", lambda e, kc=kc: e.matmul(pq0, lhsT=cT[:, kc, :], rhs=w_uq[:, kc, 0:512],
                                                               start=(kc == 0), stop=(kc == 1)),
                               reads=[cT, w_uq], writes=[pqb0], signal=(kc == 1))
                        for kc in range(2):
                            op("pe", lambda e, kc=kc: e.matmul(pq1[:, 0:256], lhsT=cT[:, kc, :], rhs=w_uq[:, kc, 512:768],
                                                               start=(kc == 0), stop=(kc == 1)),
                               reads=[cT, w_uq], writes=[pqb1], signal=(kc == 1))
                        qflat = qk[:, 0, :, :].rearrange("p h d -> p (h d)")
                        op("act", lambda e: e.copy(out=qflat[:, 0:512], in_=pq0), reads=[pqb0], writes=[qk], partial=True)
                        op("act", lambda e: e.copy(out=qflat[:, 512:768], in_=pq1[:, 0:256]), reads=[pqb1], writes=[qk],
                           partial=True)
                        pk0, pkb0 = bank()
                        pk1, pkb1 = bank()
                        op("pe", lambda e: e.matmul(pk0, lhsT=cT[:, 2, :], rhs=w_ukv[:, 0:512], start=True, stop=True),
                           reads=[cT, w_ukv], writes=[pkb0])
                        op("pe", lambda e: e.matmul(pk1, lhsT=cT[:, 2, :], rhs=w_ukv[:, 512:1024], start=True, stop=True),
                           reads=[cT, w_ukv], writes=[pkb1])
                        for hh, (pk, pkb) in enumerate(((pk0, pkb0), (pk1, pkb1))):
                            pkv = pk.rearrange("p (h d) -> p h d", h=4)
                            op("dve", lambda e: e.tensor_copy(out=qk[:, 1, hh * 4:(hh + 1) * 4, 0:64], in_=pkv[:, :, 0:64]),
                               reads=[pkb], writes=[qk], partial=True)
                            op("act", lambda e: e.copy(out=vmb[:, hh * 4:(hh + 1) * 4, :], in_=pkv[:, :, 64:128]),
                               reads=[pkb], writes=[vmb], partial=True)
                        op("dve", lambda e: e.tensor_copy(out=qk[:, 1, :, 64:96],
                                                          in_=c_[:, 384:416].unsqueeze(1).broadcast_to([128, 8, 32])),
                           reads=[c_], writes=[qk], partial=True)
                        dma("act", vm_d[tok0:tok0 + 128, :, :], vmb[:, :, :], reads=[vmb], writes=[K.db("vm", i)])
                        op("act", lambda e: e.activation(out=qk2[:], in_=qk[:], func=AF.Square), reads=[qk], writes=[qk2])
                        op("dve", lambda e: e.tensor_reduce(out=qss[:, :], in_=qk2[:].rearrange("p a h d -> p (a h) d"),
                                                            axis=AX.X, op=ALU.add), reads=[qk2], writes=[qss])
                        rstd_from_ssq(qss[:, :], rt, qrs[:, :], 96, [qss, qrs])
                        op("dve", lambda e: e.tensor_tensor(out=qkn[:].rearrange("p a h d -> p (a h) d"),
                                                            in0=qk[:].rearrange("p a h d -> p (a h) d"),
                                                            in1=qrs[:, :].unsqueeze(2).broadcast_to([128, 16, 96]),
                                                            op=ALU.mult), reads=[qk, qrs], writes=[qkn])
                        for a_, gg in ((0, g_qn), (1, g_kn)):
                            op("dve", lambda e, a_=a_, gg=gg: e.tensor_tensor(
                                out=qkn[:, a_, :, :], in0=qkn[:, a_, :, :],
                                in1=gg[:, :].unsqueeze(1).broadcast_to([128, 8, 96]), op=ALU.mult),
                               reads=[qkn, gg], writes=[qkn])
                        cosb = rope_cs[:, i, 0:16].unsqueeze(1).unsqueeze(1).broadcast_to([128, 2, 8, 16])
                        sinb = rope_cs[:, i, 16:32].unsqueeze(1).unsqueeze(1).broadcast_to([128, 2, 8, 16])
                        x1_ = qkn[:, :, :, 64:80]
                        x2_ = qkn[:, :, :, 80:96]
                        op("dve", lambda e: e.tensor_tensor(out=rtmp[0][:], in0=x1_, in1=cosb, op=ALU.mult),
                           reads=[qkn, rope_cs], writes=[rtmp[0]])
                        op("pool", lambda e: e.tensor_tensor(out=rtmp[1][:], in0=x2_, in1=sinb, op=ALU.mult),
                           reads=[qkn, rope_cs], writes=[rtmp[1]])
                        op("dve", lambda e: e.tensor_tensor(out=rtmp[2][:], in0=x2_, in1=cosb, op=ALU.mult),
                           reads=[qkn, rope_cs], writes=[rtmp[2]])
                        op("pool", lambda e: e.tensor_tensor(out=rtmp[3][:], in0=x1_, in1=sinb, op=ALU.mult),
                           reads=[qkn, rope_cs], writes=[rtmp[3]])
                        op("act", lambda e: e.copy(out=qkb[:, :, :, 0:64], in_=qkn[:, :, :, 0:64]), reads=[qkn],
                           writes=[qkb], partial=True)
                        op("dve", lambda e: e.tensor_tensor(out=qkb[:, :, :, 64:80], in0=rtmp[0][:], in1=rtmp[1][:],
                                                            op=ALU.subtract), reads=[rtmp[0], rtmp[1]], writes=[qkb],
                           partial=True)
                        op("dve", lambda e: e.tensor_tensor(out=qkb[:, :, :, 80:96], in0=rtmp[2][:], in1=rtmp[3][:],
                                                            op=ALU.add), reads=[rtmp[2], rtmp[3]], writes=[qkb],
                           partial=True)
                        for a_, dst in ((0, qT_d), (1, kT_d)):
                            pa3, pb3 = bank()
                            pbf3 = pa3.bitcast(BF16)
                            for h in range(8):
                                op("pe", lambda e, h=h, a_=a_: e.transpose(out=pbf3[0:96, h * 128:(h + 1) * 128],
                                                                           in_=qkb[:, a_, h, :], identity=ident_b[:, :]),
                                   reads=[qkb, ident_b], writes=[pb3], signal=(h == 7))
                            qt_ = qkT[a_]
                            op("act" if a_ == 0 else "dve",
                               (lambda e: e.copy(out=qt_[:, :, :], in_=pbf3[0:96, :].rearrange("p (h t) -> p h t", h=8)))
                               if a_ == 0 else
                               (lambda e: e.tensor_copy(out=qt_[:, :, :], in_=pbf3[0:96, :].rearrange("p (h t) -> p h t", h=8))),
                               reads=[pb3], writes=[qt_])
                            tloc = t0 + j * 128
                            dma("sp", dst[seq, :, :, tloc:tloc + 128], qt_[:, :, :], reads=[qt_],
                                writes=[K.db("qkT", a_, i)])
        K.barrier()
    phA.close()
    if upto == "A":
        root.close()
        return nc

    moeR = root
    Moh = sb(moeR, "Moh", [128, NT, 2, 64], BF16)
    wts = sb(moeR, "wts", [128, NT, 2], F32)
    dest_i = sb(root, "dest_i", [128, NT, 2], I32)

    for seq in range(NS):
        seqst = ExitStack()
        oaT = sb(seqst, "oaT", [128, 4, S], BF16)
        obT = sb(seqst, "obT", [64, 8, S], BF16)

        with ExitStack() as st:
            qTs = sb(st, "qTs", [128, S], BF16)
            vtk = sb(st, "vtk", [CH, NCH, 128], BF16)
            ogt = sb(st, "ogt", [CH, NCH, 128], BF16)
            qt = [sb(st, "qt%d" % d, [128, S], BF16) for d in range(2)]
            kt = [sb(st, "kt%d" % d, [128, S], BF16) for d in range(2)]
            kdt = [sb(st, "kdt%d" % d, [CH, NCH, 128], BF16) for d in range(2)]
            dec = [sb(st, "dec%d" % d, [128, NCH], F32) for d in range(2)]
            S32 = [sb(st, "S32_%d" % d, [128, 128], F32) for d in range(2)]
            Sbf = [sb(st, "Sbf_%d" % d, [128, 128], BF16) for d in range(2)]
            PT = [[sb(st, "PT%d_%d" % (d, i), [CH, CH], BF16) for i in range(2)] for d in range(2)]
            for h in range(4):
                pst = ExitStack()
                SH = S // 2
                NCH2 = NCH // 2
                smask = sb(pst, "smask", [128, SH], F32)
                op("dve", lambda e: e.memset(smask[:, :], 1.0), writes=[smask])
                op("dve", lambda e: e.memset(smask[:, :].rearrange("p (c j) -> p c j", j=CH)[:, :, 0:1], 0.0), writes=[smask])
                lgH = [sb(pst, "lg%d" % i, [128, SH], F32) for i in range(2)]
                fAH = [sb(pst, "fA%d" % i, [128, SH], F32) for i in range(2)]
                lfH = [sb(pst, "lf%d" % i, [128, SH], F32) for i in range(2)]
                kkH = [sb(pst, "kk%d" % i, [128, SH], F32) for i in range(2)]
                gAH = [sb(pst, "gA%d" % i, [128, SH], F32) for i in range(2)]
                eAH = [sb(pst, "eA%d" % i, [128, SH], F32) for i in range(2)]
                kdT = sb(pst, "kdT", [128, S], BF16)
                for g in range(S // 512):
                    dma("sp", qTs[:, g * 512:(g + 1) * 512], zq_d[seq, h, :, g * 512:(g + 1) * 512],
                        reads=[K.db("zq", seq, h, seq * (S // 512) + g)], writes=[qTs], partial=True)
                dma("sp", vtk[:, :, :], zv_d[seq * S:(seq + 1) * S, h * 128:(h + 1) * 128].rearrange("(c p) v -> p c v", p=CH),
                    reads=[K.db("zv", i) for i in range(seq * NTS, (seq + 1) * NTS)], writes=[vtk])
                dma("sp", ogt[:, :, :], zog_d[seq * S:(seq + 1) * S, h * 128:(h + 1) * 128].rearrange("(c p) v -> p c v", p=CH),
                    reads=[K.db("zog", i) for i in range(seq * NTS, (seq + 1) * NTS)], writes=[ogt])
                for d in range(2):
                    col = d * 4 + h
                    for hf in range(2):
                        for g in range(SH // 512):
                            gg = hf * (SH // 512) + g
                            dma("sp", lgH[hf][:, g * 512:(g + 1) * 512], zf_d[seq, d, h, :, gg * 512:(gg + 1) * 512],
                                reads=[K.db("zf", seq, d, h, seq * (S // 512) + gg)], writes=[lgH[hf]], partial=True)
                    HF = (0, 1)

                    def v3(t_):
                        return t_[:, :].rearrange("p (c j) -> p c j", j=CH)
                    for hf in HF:
                        op("act", lambda e: e.activation(out=fAH[hf][:, :], in_=lgH[hf][:, :], func=AF.Sigmoid), reads=[lgH[hf]],
                           writes=[fAH[hf]])
                    for hf in HF:
                        op("dve", lambda e: e.tensor_scalar(out=fAH[hf][:, :], in0=fAH[hf][:, :], scalar1=oml[:, col:col + 1],
                                                            scalar2=lb[:, col:col + 1], op0=ALU.mult, op1=ALU.add),
                           reads=[fAH[hf], oml, lb], writes=[fAH[hf]])
                    for hf in HF:
                        op("act", lambda e: e.activation(out=lfH[hf][:, :], in_=fAH[hf][:, :], func=AF.Ln), reads=[fAH[hf]],
                           writes=[lfH[hf]])
                        op("pool", lambda e: e.tensor_scalar(out=kkH[hf][:, :], in0=fAH[hf][:, :], scalar1=-1.0, scalar2=1.0,
                                                             op0=ALU.mult, op1=ALU.add), reads=[fAH[hf]], writes=[kkH[hf]])
                    for hf in HF:
                        op("dve", lambda e: e.tensor_tensor_scan(out=gAH[hf][:, :], data0=smask[:, :], data1=lfH[hf][:, :],
                                                                 initial=0.0, op0=ALU.mult, op1=ALU.add),
                           reads=[smask, lfH[hf]], writes=[gAH[hf]])
                    GH = []
                    for hf in HF:
                        if d == 0:
                            GH.append(gAH[hf])
                        else:
                            op("dve", lambda e: e.tensor_tensor(out=lgH[hf][:, :], in0=lfH[hf][:, :], in1=gAH[hf][:, :],
                                                                op=ALU.subtract), reads=[lfH[hf], gAH[hf]], writes=[lgH[hf]])
                            op("dve", lambda e: e.tensor_tensor(out=v3(lgH[hf]), in0=v3(lgH[hf]),
                                                                in1=v3(gAH[hf])[:, :, CH - 1:CH].broadcast_to([128, NCH2, CH]),
                                                                op=ALU.add), reads=[lgH[hf], gAH[hf]], writes=[lgH[hf]])
                            GH.append(lgH[hf])
                    for hf in HF:
                        glast = v3(gAH[hf])[:, :, CH - 1:CH]
                        op("act", lambda e: e.activation(out=dec[d][:, hf * NCH2:(hf + 1) * NCH2],
                                                         in_=glast.rearrange("p c o -> p (c o)"), func=AF.Exp),
                           reads=[gAH[hf]], writes=[dec[d]], partial=True)
                        op("act", lambda e: e.activation(out=eAH[hf][:, :], in_=GH[hf][:, :], func=AF.Exp), reads=[GH[hf]],
                           writes=[eAH[hf]])
                    for hf in HF:
                        op("dve", lambda e: e.tensor_tensor(out=qt[d][:, hf * SH:(hf + 1) * SH], in0=qTs[:, hf * SH:(hf + 1) * SH],
                                                            in1=eAH[hf][:, :], op=ALU.mult), reads=[qTs, eAH[hf]], writes=[qt[d]],
                           partial=True)
                    for hf in HF:
                        op("act", lambda e: e.activation(out=eAH[hf][:, :], in_=GH[hf][:, :], func=AF.Exp, scale=-1.0),
                           reads=[GH[hf]], writes=[eAH[hf]])
                    for hf in HF:
                        op("pool", lambda e: e.tensor_tensor(out=kt[d][:, hf * SH:(hf + 1) * SH], in0=kkH[hf][:, :],
                                                             in1=eAH[hf][:, :], op=ALU.mult), reads=[kkH[hf], eAH[hf]],
                           writes=[kt[d]], partial=True)
                        glast = v3(gAH[hf])[:, :, CH - 1:CH]
                        op("dve", lambda e: e.tensor_tensor(out=v3(fAH[hf]), in0=glast.broadcast_to([128, NCH2, CH]),
                                                            in1=v3(GH[hf]), op=ALU.subtract), reads=[gAH[hf], GH[hf]],
                           writes=[fAH[hf]])
                    for hf in HF:
                        op("act", lambda e: e.activation(out=fAH[hf][:, :], in_=fAH[hf][:, :], func=AF.Exp), reads=[fAH[hf]],
                           writes=[fAH[hf]])
                    for hf in HF:
                        op("dve", lambda e: e.tensor_tensor(out=kdT[:, hf * SH:(hf + 1) * SH], in0=kkH[hf][:, :], in1=fAH[hf][:, :],
                                                            op=ALU.mult), reads=[kkH[hf], fAH[hf]], writes=[kdT], partial=True)
                    for c8 in range(0, NCH, 8):
                        pa, pb = bank()
                        pbf = pa.bitcast(BF16)
                        for cc in range(8):
                            c = c8 + cc
                            op("pe", lambda e, c=c, cc=cc: e.transpose(out=pbf[0:CH, cc * 128:(cc + 1) * 128],
                                                                       in_=kdT[:, c * CH:(c + 1) * CH], identity=ident_b[:, :]),
                               reads=[kdT, ident_b], writes=[pb], signal=(cc == 7))
                        op("act", lambda e: e.copy(out=kdt[d][:, c8:c8 + 8, :],
                                                   in_=pbf[0:CH, :].rearrange("p (c k) -> p c k", c=8)),
                           reads=[pb], writes=[kdt[d]], partial=True)
                K.barrier()
                pst.close()
                rst = ExitStack()
                oacc = [sb(rst, "oacc%d" % d, [CH, NCH, 128], F32) for d in range(2)]
                osq = sb(rst, "osq", [CH, NCH, 128], BF16)
                oss = sb(rst, "oss", [CH, NCH], F32)
                ors = sb(rst, "ors", [CH, NCH], F32)
                ort = sb(rst, "ort", [CH, NCH], F32)
                ohg = sb(rst, "ohg", [CH, NCH, 128], BF16)
                for d in range(2):
                    op("dve", lambda e, d=d: e.memset(S32[d][:, :], 0.0), writes=[S32[d]])
                    op("pool", lambda e, d=d: e.memset(Sbf[d][:, :], 0.0), writes=[Sbf[d]])
                for step in range(NCH):
                    for d in range(2):
                        c = step if d == 0 else NCH - 1 - step
                        cs_ = slice(c * CH, (c + 1) * CH)
                        pt_ = PT[d][step % 2]
                        pa, pb = bank()
                        op("pe", lambda e: e.matmul(pa[0:CH, 0:CH], lhsT=kt[d][:, cs_], rhs=qt[d][:, cs_], start=True, stop=True),
                           reads=[kt[d], qt[d]], writes=[pb])
                        mk = maskf if d == 0 else maskb
                        op("dve", lambda e: e.tensor_tensor(out=pt_[:, :], in0=pa[0:CH, 0:CH], in1=mk[0:CH, 0:CH], op=ALU.mult),
                           reads=[pb, mk], writes=[pt_])
                        pa2, pb2 = bank()
                        op("pe", lambda e: e.matmul(pa2[0:CH, 0:128], lhsT=qt[d][:, cs_], rhs=Sbf[d][:, :], start=True, stop=False),
                           reads=[qt[d], Sbf[d]], writes=[pb2], signal=False)
                        op("pe", lambda e: e.matmul(pa2[0:CH, 0:128], lhsT=pt_[:, :], rhs=vtk[:, c, :], start=False, stop=True),
                           reads=[pt_, vtk], writes=[pb2])
                        op("act", lambda e: e.copy(out=oacc[d][:, c, :], in_=pa2[0:CH, 0:128]), reads=[pb2], writes=[oacc[d]],
                           partial=True)
                        pa3, pb3 = bank()
                        op("pe", lambda e: e.matmul(pa3[:, 0:128], lhsT=kdt[d][:, c, :], rhs=vtk[:, c, :], start=True, stop=True),
                           reads=[kdt[d], vtk], writes=[pb3])
                        op("dve", lambda e: e.scalar_tensor_tensor(out=S32[d][:, :], in0=S32[d][:, :], scalar=dec[d][:, c:c + 1],
                                                                   in1=pa3[:, 0:128], op0=ALU.mult, op1=ALU.add),
                           reads=[S32[d], dec[d], pb3], writes=[S32[d]])
                        op("pool", lambda e: e.tensor_copy(out=Sbf[d][:, :], in_=S32[d][:, :]), reads=[S32[d]], writes=[Sbf[d]])
                op("dve", lambda e: e.tensor_tensor(out=oacc[0][:], in0=oacc[0][:], in1=oacc[1][:], op=ALU.add),
                   reads=[oacc[0], oacc[1]], writes=[oacc[0]])
                op("act", lambda e: e.activation(out=osq[:], in_=oacc[0][:], func=AF.Square), reads=[oacc[0]], writes=[osq])
                op("dve", lambda e: e.tensor_reduce(out=oss[:, :], in_=osq[:], axis=AX.X, op=ALU.add), reads=[osq], writes=[oss])
                rstd_from_ssq(oss[:, :], ort, ors[:, :], 128, [oss, ors])
                op("dve", lambda e: e.tensor_tensor(out=oacc[0][:], in0=oacc[0][:],
                                                    in1=ors[:, :].unsqueeze(2).broadcast_to([CH, NCH, 128]), op=ALU.mult),
                   reads=[oacc[0], ors], writes=[oacc[0]])
                op("pool", lambda e: e.tensor_tensor(out=oacc[0][:], in0=oacc[0][:],
                                                     in1=g_on[0:CH, :].unsqueeze(1).broadcast_to([CH, NCH, 128]), op=ALU.mult),
                   reads=[oacc[0], g_on], writes=[oacc[0]])
                op("dve", lambda e: e.tensor_tensor(out=ohg[:], in0=oacc[0][:], in1=ogt[:], op=ALU.mult),
                   reads=[oacc[0], ogt], writes=[ohg])
                for c8 in range(0, NCH, 8):
                    pa, pb = bank()
                    pbf = pa.bitcast(BF16)
                    for cc in range(8):
                        c = c8 + cc
                        op("pe", lambda e, c=c, cc=cc: e.transpose(out=pbf[:, cc * CH:(cc + 1) * CH], in_=ohg[:, c, :],
                                                                   identity=ident_b[0:CH, 0:CH]),
                           reads=[ohg, ident_b], writes=[pb], signal=(cc == 7))
                    op("act", lambda e: e.copy(out=oaT[:, h, c8 * CH:(c8 + 8) * CH], in_=pbf[:, 0:8 * CH]), reads=[pb],
                       writes=[oaT], partial=True)
                K.barrier()
                rst.close()

        if upto == "B":
            seqst.close()
            root.close()
            return nc
        with ExitStack() as st:
            QT = [sb(st, "QT%d" % i, [96, S], BF16) for i in range(2)]
            KT = [sb(st, "KT%d" % i, [96, S], BF16) for i in range(2)]
            VV = [sb(st, "VV%d" % i, [128, NTS, 65], BF16) for i in range(2)]
            for i in range(2):
                op("dve", lambda e, i=i: e.memset(VV[i][:, :, 64:65], 1.0), writes=[VV[i]], partial=True)
            G_ = 4
            NPT = 2 * G_ + 1
            PTs = [sb(st, "PTs%d" % i, [128, 512], BF16) for i in range(NPT)]
            Osb = [sb(st, "Osb%d" % i, [65, 512], F32) for i in range(2)]
            rden = [sb(st, "rden%d" % i, [64, 512], F32) for i in range(2)]
            scale = float(96 ** -0.5)
            reserved.update((0, 1))
            tiles = list(range(seq * NTS, (seq + 1) * NTS))
            NQG = S // 512
            items = [(h, qg, kt_) for h in range(8) for qg in range(NQG) for kt_ in range(NTS)]
            LA = G_

            def c_load(h):
                Q_, K_, V_ = QT[h % 2], KT[h % 2], VV[h % 2]
                dma("sp", Q_[:, :], qT_d[seq, :, h, :], reads=[K.db("qkT", 0, i) for i in tiles], writes=[Q_])
                dma("sp", K_[:, :], kT_d[seq, :, h, :], reads=[K.db("qkT", 1, i) for i in tiles], writes=[K_])
                dma("sp", V_[:, :, 0:64], vm_d[seq * S:(seq + 1) * S, h, :].rearrange("(n p) d -> p n d", p=128),
                    reads=[K.db("vm", i) for i in tiles], writes=[V_], partial=True)

            def c_qk(ii):
                h, qg, kt_ = items[ii]
                Q_, K_ = QT[h % 2], KT[h % 2]
                pa, pb = bank()
                op("pe", lambda e: e.matmul(pa, lhsT=K_[:, kt_ * 128:(kt_ + 1) * 128], rhs=Q_[:, qg * 512:(qg + 1) * 512],
                                            start=True, stop=True), reads=[K_, Q_], writes=[pb])
                p_ = PTs[ii % NPT]
                op("act", lambda e: e.activation(out=p_[:, :], in_=pa, func=AF.Exp, scale=scale), reads=[pb], writes=[p_])

            epi = []

            def c_pv(ii):
                h, qg, kt_ = items[ii]
                V_ = VV[h % 2]
                gi = h * NQG + qg
                po, pob = fixed_bank(gi % 2)
                p_ = PTs[ii % NPT]
                op("pe", lambda e: e.matmul(po[0:65, :], lhsT=V_[:, kt_, :], rhs=p_[:, :], start=(kt_ == 0),
                                            stop=(kt_ == NTS - 1)), reads=[V_, p_], writes=[pob], signal=(kt_ == NTS - 1))
                if kt_ == NTS - 1:
                    o_ = Osb[gi % 2]
                    op("dve", lambda e: e.tensor_copy(out=o_[:, :], in_=po[0:65, :]), reads=[pob], writes=[o_])
                    epi.append((ii, h, qg, gi))

            def c_epi(h, qg, gi):
                o_ = Osb[gi % 2]
                r_ = rden[gi % 2]
                pd, pdb = bank()
                op("pe", lambda e: e.matmul(pd[0:64, :], lhsT=sel[0:65, :], rhs=o_[:, :], start=True, stop=True),
                   reads=[sel, o_], writes=[pdb])
                op("dve", lambda e: e.reciprocal(out=r_[:, :], in_=pd[0:64, :]), reads=[pdb], writes=[r_])
                op("dve", lambda e: e.tensor_tensor(out=obT[:, h, qg * 512:(qg + 1) * 512], in0=o_[0:64, :], in1=r_[:, :],
                                                    op=ALU.mult), reads=[o_, r_], writes=[obT], partial=True)

            n_it = len(items)
            assert n_it % G_ == 0
            c_load(0)
            c_load(1)
            ngrp = n_it // G_
            for g in range(ngrp + 2):
                if g < ngrp:
                    for ii in range(g * G_, (g + 1) * G_):
                        c_qk(ii)
                while epi and epi[0][0] < (g - 1) * G_:
                    _, h_, qg_, gi_ = epi.pop(0)
                    c_epi(h_, qg_, gi_)
                if 1 <= g <= ngrp:
                    for ii in range((g - 1) * G_, g * G_):
                        c_pv(ii)
                    hl, qgl, ktl = items[g * G_ - 1]
                    if qgl == NQG - 1 and ktl == NTS - 1 and hl + 2 < 8:
                        c_load(hl + 2)
            assert not epi
            reserved.clear()
            K.barrier()
        if upto == "C":
            seqst.close()
            root.close()
            return nc

        with ExitStack() as st:
            w_oA = sb(st, "w_oA", [128, 4, D], BF16)
            w_oB = sb(st, "w_oB", [64, 8, D], BF16)
            w_out = sb(st, "w_out", [128, 8, D], BF16)
            w_rt = sb(st, "w_rt", [128, 8, 72], F32)
            wst_cur[0] = [sb(st, "wstD%d" % i, [128, 1024], F32) for i in range(3)]
            for kc in range(4):
                load_cast(w_oA[:, kc, :], w_oA, w_oA_d[kc * 128:(kc + 1) * 128, :], 1024)
            for h in range(8):
                load_cast(w_oB[:, h, :], w_oB, w_oB_d[h * 64:(h + 1) * 64, :], 1024, npart=64)
            for kc in range(8):
                load_cast(w_out[:, kc, :], w_out, w_out_d[kc * 128:(kc + 1) * 128, :], 1024)
            dma("sp", w_rt[:, :, 0:8], w_rg_d.rearrange("(k p) g -> p k g", p=128), writes=[w_rt], partial=True)
            dma("sp", w_rt[:, :, 8:72], w_re_d.rearrange("(k p) g -> p k g", p=128), writes=[w_rt], partial=True)
            g_moe = bc_load(st, "g_moe", ln_moe_d, D)
            b_rt = sb(st, "b_rt", [128, 72], F32)
            dma("sp", b_rt[:, 0:8], b_rg_d.partition_broadcast(128), writes=[b_rt], partial=True)
            dma("sp", b_rt[:, 8:72], b_re_d.partition_broadcast(128), writes=[b_rt], partial=True)
            hmbt = [sb(st, "hmbt%d" % i, [128, D], BF16) for i in range(2)]
            sg = [sb(st, "sg%d" % i, [128, 2048], BF16) for i in range(2)]
            xin = [sb(st, "xin%d" % i, [128, D], F32) for i in range(2)]
            ta_l = [sb(st, "ta%d" % _i, [128, D], F32) for _i in range(2)]
            tb_l = [sb(st, "tb%d" % _i, [128, D], F32) for _i in range(2)]
            mg_l = [sb(st, "mg%d" % _i, [128, D], BF16) for _i in range(2)]
            mT_l = [sb(st, "mT%d" % _i, [128, 8, 128], BF16) for _i in range(2)]
            x1t = [sb(st, "x1t%d" % i, [128, D], F32) for i in range(2)]
            sq_l = [sb(st, "sqD%d" % _i, [128, D], BF16) for _i in range(2)]
            ssq_l = [sb(st, "ssqD%d" % _i, [128, 4], F32) for _i in range(2)]
            rt_l = [sb(st, "rtD%d" % _i, [128, 4], F32) for _i in range(2)]
            rs_l = [sb(st, "rsD%d" % _i, [128, 4], F32) for _i in range(2)]
            hmf_l = [sb(st, "hmf%d" % _i, [128, D], F32) for _i in range(2)]
            hmT_l = [sb(st, "hmT%d" % _i, [128, 8, 128], F32) for _i in range(2)]
            lgt_l = [sb(st, "lgt%d" % _i, [128, 72], F32) for _i in range(2)]
            r8_l = [sb(st, "r8%d" % _i, [128, 8], F32) for _i in range(2)]
            gmx_l = [sb(st, "gmx%d" % _i, [128, 8], F32) for _i in range(2)]
            goh_l = [sb(st, "goh%d" % _i, [128, 8], F32) for _i in range(2)]
            gex_l = [sb(st, "gex%d" % _i, [128, 8], F32) for _i in range(2)]
            gsum_l = [sb(st, "gsum%d" % _i, [128, 2], F32) for _i in range(2)]
            pgrp_l = [sb(st, "pgrp%d" % _i, [128, 2], F32) for _i in range(2)]
            eml_l = [sb(st, "eml%d" % _i, [128, 64], F32) for _i in range(2)]
            pen_l = [sb(st, "pen%d" % _i, [128, 8], F32) for _i in range(2)]
            top8_l = [sb(st, "top8%d" % _i, [128, 8], F32) for _i in range(2)]
            dv_l = [sb(st, "dv%d" % _i, [128, 2], F32) for _i in range(2)]
            def d_tile(jt):
                ta = ta_l[(seq * NTS + jt) % 2]
                tb = tb_l[(seq * NTS + jt) % 2]
                mg = mg_l[(seq * NTS + jt) % 2]
                mT = mT_l[(seq * NTS + jt) % 2]
                sq = sq_l[(seq * NTS + jt) % 2]
                ssq = ssq_l[(seq * NTS + jt) % 2]
                rt = rt_l[(seq * NTS + jt) % 2]
                rs = rs_l[(seq * NTS + jt) % 2]
                hmf = hmf_l[(seq * NTS + jt) % 2]
                hmT = hmT_l[(seq * NTS + jt) % 2]
                lgt = lgt_l[(seq * NTS + jt) % 2]
                r8 = r8_l[(seq * NTS + jt) % 2]
                gmx = gmx_l[(seq * NTS + jt) % 2]
                goh = goh_l[(seq * NTS + jt) % 2]
                gex = gex_l[(seq * NTS + jt) % 2]
                gsum = gsum_l[(seq * NTS + jt) % 2]
                pgrp = pgrp_l[(seq * NTS + jt) % 2]
                eml = eml_l[(seq * NTS + jt) % 2]
                pen = pen_l[(seq * NTS + jt) % 2]
                top8 = top8_l[(seq * NTS + jt) % 2]
                dv = dv_l[(seq * NTS + jt) % 2]
                i = seq * NTS + jt
                tsl = slice(jt * 128, (jt + 1) * 128)
                s_ = sg[i % 2]
                x_ = xin[i % 2]
                x1_ = x1t[i % 2]
                dma("sp", s_[:, :], zg_d[i * 128:(i + 1) * 128, :], reads=[K.db("zg", i, gi) for gi in range(4)], writes=[s_])
                dma("sp", x_[:, :], x_d[seq, tsl, :], writes=[x_])
                yield
                ya = [bank(), bank()]
                for hf in range(2):
                    for hh in range(4):
                        op("pe", lambda e: e.matmul(ya[hf][0], lhsT=oaT[:, hh, tsl], rhs=w_oA[:, hh, hf * 512:(hf + 1) * 512],
                                                    start=(hh == 0), stop=(hh == 3)), reads=[oaT, w_oA], writes=[ya[hf][1]],
                           signal=(hh == 3))
                    op("dve", lambda e: e.tensor_tensor(out=ta[:, hf * 512:(hf + 1) * 512], in0=ya[hf][0],
                                                        in1=s_[:, hf * 512:(hf + 1) * 512], op=ALU.mult),
                       reads=[ya[hf][1], s_], writes=[ta], partial=True)
                yb = [bank(), bank()]
                for hf in range(2):
                    for hh in range(8):
                        op("pe", lambda e: e.matmul(yb[hf][0], lhsT=obT[:, hh, tsl], rhs=w_oB[:, hh, hf * 512:(hf + 1) * 512],
                                                    start=(hh == 0), stop=(hh == 7)), reads=[obT, w_oB], writes=[yb[hf][1]],
                           signal=(hh == 7))
                    op("dve", lambda e: e.tensor_tensor(out=tb[:, hf * 512:(hf + 1) * 512], in0=yb[hf][0],
                                                        in1=s_[:, 1024 + hf * 512:1024 + (hf + 1) * 512], op=ALU.mult),
                       reads=[yb[hf][1], s_], writes=[tb], partial=True)
                yield
                op("pool", lambda e: e.tensor_tensor(out=mg[:, :], in0=ta[:, :], in1=tb[:, :], op=ALU.add), reads=[ta, tb],
                   writes=[mg])
                pa, pb = bank()
                pbf = pa.bitcast(BF16)
                for kc in range(8):
                    op("pe", lambda e, kc=kc: e.transpose(out=pbf[:, kc * 128:(kc + 1) * 128], in_=mg[:, kc * 128:(kc + 1) * 128],
                                                          identity=ident_b[:, :]), reads=[mg, ident_b], writes=[pb],
                       signal=(kc == 7))
                op("act", lambda e: e.copy(out=mT[:, :, :], in_=pbf.rearrange("p (k t) -> p k t", k=8)), reads=[pb], writes=[mT])
                yield
                for hf in range(2):
                    pa, pb = bank()
                    for kc in range(8):
                        op("pe", lambda e, kc=kc: e.matmul(pa, lhsT=mT[:, kc, :], rhs=w_out[:, kc, hf * 512:(hf + 1) * 512],
                                                           start=(kc == 0), stop=(kc == 7)), reads=[mT, w_out], writes=[pb],
                           signal=(kc == 7))
                    op("dve", lambda e: e.tensor_tensor(out=x1_[:, hf * 512:(hf + 1) * 512], in0=pa,
                                                        in1=x_[:, hf * 512:(hf + 1) * 512], op=ALU.add), reads=[pb, x_],
                       writes=[x1_], partial=True)
                dma("sp", x1_d[i * 128:(i + 1) * 128, :], x1_[:, :], reads=[x1_], writes=[K.db("x1", i)])
                yield
                op("act", lambda e: e.activation(out=sq[:, :], in_=x1_[:, :], func=AF.Square), reads=[x1_], writes=[sq])
                op("dve", lambda e: e.tensor_reduce(out=ssq[:, 0:1], in_=sq[:, :], axis=AX.X, op=ALU.add), reads=[sq], writes=[ssq])
                rstd_from_ssq(ssq[:, 0:1], rt, rs[:, 0:1], D, [ssq, rs])
                op("dve", lambda e: e.scalar_tensor_tensor(out=hmf[:, :], in0=x1_[:, :], scalar=rs[:, 0:1], in1=g_moe[:, :],
                                                           op0=ALU.mult, op1=ALU.mult), reads=[x1_, rs, g_moe], writes=[hmf])
                hb_ = hmbt[i % 2]
                op("pool", lambda e: e.tensor_copy(out=hb_[:, :], in_=hmf[:, :]), reads=[hmf], writes=[hb_])
                dma("sp", hmb_d[i * 128:(i + 1) * 128, :], hb_[:, :], reads=[hb_], writes=[K.db("hmb", i)])
                yield
                for half in range(2):
                    pa, pb = bank()
                    for kc4 in range(4):
                        kc = half * 4 + kc4
                        op("pe", lambda e, kc=kc, kc4=kc4: e.transpose(out=pa[:, kc4 * 128:(kc4 + 1) * 128],
                                                                       in_=hmf[:, kc * 128:(kc + 1) * 128], identity=ident_f[:, :]),
                           reads=[hmf, ident_f], writes=[pb], signal=(kc4 == 3))
                    op("act", lambda e: e.copy(out=hmT[:, half * 4:(half + 1) * 4, :],
                                               in_=pa.rearrange("p (k t) -> p k t", k=4)), reads=[pb], writes=[hmT], partial=True)
                yield
                pa, pb = bank()
                for kc in range(8):
                    op("pe", lambda e, kc=kc: e.matmul(pa[:, 0:72], lhsT=hmT[:, kc, :], rhs=w_rt[:, kc, :], start=(kc == 0),
                                                       stop=(kc == 7)), reads=[hmT, w_rt], writes=[pb], signal=(kc == 7))
                op("dve", lambda e: e.tensor_tensor(out=lgt[:, :], in0=pa[:, 0:72], in1=b_rt[:, :], op=ALU.add), reads=[pb, b_rt],
                   writes=[lgt])
                op("dve", lambda e: e.max(out=gmx[:, :], in_=lgt[:, 0:8]), reads=[lgt], writes=[gmx])
                op("dve", lambda e: e.tensor_scalar(out=goh[:, :], in0=lgt[:, 0:8], scalar1=gmx[:, 0:1], scalar2=None,
                                                    op0=ALU.is_equal), reads=[lgt, gmx], writes=[goh])
                op("dve", lambda e: e.tensor_scalar(out=gex[:, :], in0=lgt[:, 0:8], scalar1=gmx[:, 0:1], scalar2=None,
                                                    op0=ALU.subtract), reads=[lgt, gmx], writes=[gex])
                op("act", lambda e: e.activation(out=gex[:, :], in_=gex[:, :], func=AF.Exp), reads=[gex], writes=[gex])
                op("dve", lambda e: e.tensor_reduce(out=gsum[:, 0:1], in_=gex[:, :], axis=AX.X, op=ALU.add), reads=[gex],
                   writes=[gsum])
                op("dve", lambda e: e.reciprocal(out=pgrp[:, 0:1], in_=gsum[:, 0:1]), reads=[gsum], writes=[pgrp])
                yield
                op("dve", lambda e: e.tensor_scalar(out=pen[:, :], in0=goh[:, :], scalar1=1.0e30, scalar2=-1.0e30, op0=ALU.mult,
                                                    op1=ALU.add), reads=[goh], writes=[pen])
                op("dve", lambda e: e.tensor_tensor(out=eml[:, :].rearrange("p (g j) -> p g j", g=8),
                                                    in0=lgt[:, 8:72].rearrange("p (g j) -> p g j", g=8),
                                                    in1=pen[:, :].unsqueeze(2).broadcast_to([128, 8, 8]), op=ALU.add),
                   reads=[lgt, pen], writes=[eml])
                op("dve", lambda e: e.max(out=top8[:, :], in_=eml[:, :]), reads=[eml], writes=[top8])
                for j2 in range(2):
                    op("dve", lambda e, j2=j2: e.tensor_scalar(out=Moh[:, i, j2, :], in0=eml[:, :], scalar1=top8[:, j2:j2 + 1],
                                                               scalar2=None, op0=ALU.is_equal), reads=[eml, top8],
                       writes=[Moh], partial=True)
                op("dve", lambda e: e.tensor_tensor(out=dv[:, 0:1], in0=top8[:, 0:1], in1=top8[:, 1:2], op=ALU.subtract),
                   reads=[top8], writes=[dv])
                op("dve", lambda e: e.tensor_tensor(out=dv[:, 1:2], in0=top8[:, 1:2], in1=top8[:, 0:1], op=ALU.subtract),
                   reads=[top8], writes=[dv])
                op("act", lambda e: e.activation(out=dv[:, :], in_=dv[:, :], func=AF.Sigmoid), reads=[dv], writes=[dv])
                op("dve", lambda e: e.tensor_scalar(out=wts[:, i, :], in0=dv[:, :], scalar1=pgrp[:, 0:1], scalar2=None,
                                                    op0=ALU.mult), reads=[dv, pgrp], writes=[wts], partial=True)
                yield
            pipeline([(lambda jt=jt: d_tile(jt)) for jt in range(NTS)], 2)
            K.barrier()
        seqst.close()
    if upto == "D":
        root.close()
        return nc

    with ExitStack() as st:
        Msum = sb(st, "Msum", [128, NT, 64], BF16)
        eoff_i = sb(st, "eoff_i", [128, 64], I32)
        eoff = sb(st, "eoff", [128, 64], F32)
        crk = sb(st, "crk", [128, 64], F32)
        junk = sb(st, "junk", [128, 64], F32)
        dest_f = sb(st, "dest_f", [128, NT, 2], F32)
        op("pool", lambda e: e.iota(eoff_i[:], pattern=[[CAP, 64]], base=0, channel_multiplier=0), writes=[eoff_i])
        op("dve", lambda e: e.tensor_copy(out=eoff[:], in_=eoff_i[:]), reads=[eoff_i], writes=[eoff])
        op("dve", lambda e: e.tensor_tensor(out=Msum[:], in0=Moh[:, :, 0, :], in1=Moh[:, :, 1, :], op=ALU.add), reads=[Moh],
           writes=[Msum])
        for i in range(NT):
            pa, pb = bank()
            for i2 in range(i + 1):
                lt = lstrict if i2 == i else ones_b
                op("pe", lambda e, i2=i2, lt=lt: e.matmul(pa[:, 0:64], lhsT=lt[:, :], rhs=Msum[:, i2, :], start=(i2 == 0),
                                                          stop=(i2 == i)), reads=[lt, Msum], writes=[pb], signal=(i2 == i))
            op("dve", lambda e: e.tensor_scalar(out=crk[:, :], in0=pa[:, 0:64], scalar1=float(CAP - 1), scalar2=None,
                                                op0=ALU.min), reads=[pb], writes=[crk])
            op("dve", lambda e: e.tensor_tensor(out=crk[:, :], in0=crk[:, :], in1=eoff[:, :], op=ALU.add), reads=[crk, eoff],
               writes=[crk])
            for j2 in range(2):
                op("dve", lambda e, j2=j2: e.tensor_tensor(out=junk[:, :], in0=crk[:, :], in1=Moh[:, i, j2, :], op=ALU.mult),
                   reads=[crk, Moh], writes=[junk])
                op("dve", lambda e, j2=j2: e.tensor_reduce(out=dest_f[:, i, j2:j2 + 1], in_=junk[:, :], axis=AX.X, op=ALU.add),
                   reads=[junk], writes=[dest_f], partial=True)
        op("dve", lambda e: e.tensor_copy(out=dest_i[:], in_=dest_f[:]), reads=[dest_f], writes=[dest_i])
        hst = [sb(st, "hst%d" % i, [128, D], BF16) for i in range(3)]
        for i in range(NT):
            hmb = hst[i % 3]
            dma("sp", hmb[:, :], hmb_d[i * 128:(i + 1) * 128, :], reads=[K.db("hmb", i)], writes=[hmb])
            for j2 in range(2):
                dma("pool", xs_d[:, :], hmb[:, :], reads=[hmb, dest_i], writes=[K.db("xs")], partial=True,
                    indirect=dict(out_offset=bass.IndirectOffsetOnAxis(ap=dest_i[:, i, j2:j2 + 1], axis=0), in_offset=None))
        K.barrier()

    with ExitStack() as st:
        NB = CAP // 128
        ws13 = [sb(st, "ws13_%d" % i, [128, 8, 512], F32) for i in range(3)]
        ws2 = [sb(st, "ws2_%d" % i, [128, 2, D], F32) for i in range(3)]
        wb13 = [sb(st, "wb13_%d" % i, [128, 8, 512], BF16) for i in range(2)]
        wb2 = [sb(st, "wb2_%d" % i, [128, 2, D], BF16) for i in range(2)]
        xsb = [sb(st, "xsb%d" % i, [128, NB, D], BF16) for i in range(3)]
        xsT = [sb(st, "xsT%d" % i, [128, 8, CAP], BF16) for i in range(2)]
        sl = [sb(st, "sl%d" % i, [128, 2, CAP], F32) for i in range(2)]
        hh_ = [sb(st, "hh%d" % i, [128, 2, CAP], BF16) for i in range(2)]
        ysb = [sb(st, "ysb%d" % i, [128, D], F32) for i in range(4)]
        yi = [0]

        def e_load(ex):
            a13, a2 = ws13[ex % 3], ws2[ex % 3]
            dma("sp", a13[:, :, 0:256], w1_d[ex].rearrange("(p k) f -> p k f", k=8), writes=[a13], partial=True)
            dma("sp", a13[:, :, 256:512], w3_d[ex].rearrange("(p k) f -> p k f", k=8), writes=[a13], partial=True)
            dma("sp", a2[:, :, :], w2_d[ex].rearrange("(c p) d -> p c d", p=128), writes=[a2])
            xb = xsb[ex % 3]
            dma("sp", xb[:, :, :], xs_d[ex * CAP:(ex + 1) * CAP, :].rearrange("(b p) d -> p b d", p=128),
                reads=[K.db("xs")], writes=[xb])

        def e_cast(ex):
            a13, a2, b13, b2 = ws13[ex % 3], ws2[ex % 3], wb13[ex % 2], wb2[ex % 2]
            op("dve", lambda e: e.tensor_copy(out=b13[:, 0:3, :], in_=a13[:, 0:3, :]), reads=[a13], writes=[b13], partial=True)
            op("act", lambda e: e.copy(out=b13[:, 3:6, :], in_=a13[:, 3:6, :]), reads=[a13], writes=[b13], partial=True)
            op("pool", lambda e: e.tensor_copy(out=b13[:, 6:8, :], in_=a13[:, 6:8, :]), reads=[a13], writes=[b13], partial=True)
            op("dve", lambda e: e.tensor_copy(out=b2[:, 0:1, :], in_=a2[:, 0:1, :]), reads=[a2], writes=[b2], partial=True)
            op("act", lambda e: e.copy(out=b2[:, 1:2, :], in_=a2[:, 1:2, :]), reads=[a2], writes=[b2], partial=True)

        def e_compute(ex):
            b13, b2 = wb13[ex % 2], wb2[ex % 2]
            xb, xT, hb, sl_ = xsb[ex % 3], xsT[ex % 2], hh_[ex % 2], sl[ex % 2]
            for b_ in range(NB):
                pa, pb = bank()
                pbf = pa.bitcast(BF16)
                xv = xb[:, b_, :].rearrange("p (q k) -> p k q", k=8)
                for kc in range(8):
                    op("pe", lambda e, kc=kc: e.transpose(out=pbf[:, kc * 128:(kc + 1) * 128], in_=xv[:, kc, :],
                                                          identity=ident_b[:, :]),
                       reads=[xb, ident_b], writes=[pb], signal=(kc == 7))
                if b_ % 2 == 0:
                    op("act", lambda e: e.copy(out=xT[:, :, b_ * 128:(b_ + 1) * 128], in_=pbf.rearrange("p (k t) -> p k t", k=8)),
                       reads=[pb], writes=[xT], partial=True)
                else:
                    op("dve", lambda e: e.tensor_copy(out=xT[:, :, b_ * 128:(b_ + 1) * 128],
                                                      in_=pbf.rearrange("p (k t) -> p k t", k=8)),
                       reads=[pb], writes=[xT], partial=True)
            ups = []
            for u in range(4):
                pa, pb = bank()
                for kc in range(8):
                    op("pe", lambda e, kc=kc: e.matmul(pa[:, 0:CAP], lhsT=b13[:, kc, u * 128:(u + 1) * 128], rhs=xT[:, kc, :],
                                                       start=(kc == 0), stop=(kc == 7)), reads=[b13, xT], writes=[pb],
                       signal=(kc == 7))
                ups.append((pa, pb))
            for c in range(2):
                op("act", lambda e: e.activation(out=sl_[:, c, :], in_=ups[c][0][:, 0:CAP], func=AF.Silu), reads=[ups[c][1]],
                   writes=[sl_], partial=True)
                op("dve", lambda e: e.tensor_tensor(out=hb[:, c, :], in0=ups[2 + c][0][:, 0:CAP], in1=sl_[:, c, :], op=ALU.mult),
                   reads=[ups[2 + c][1], sl_], writes=[hb], partial=True)
            for b_ in range(NB):
                y_ = ysb[yi[0] % 4]
                yi[0] += 1
                for hf in range(2):
                    pa, pb = bank()
                    for c in range(2):
                        op("pe", lambda e, c=c: e.matmul(pa, lhsT=hb[:, c, b_ * 128:(b_ + 1) * 128],
                                                         rhs=b2[:, c, hf * 512:(hf + 1) * 512], start=(c == 0), stop=(c == 1)),
                           reads=[hb, b2], writes=[pb], signal=(c == 1))
                    if hf == 0:
                        op("act", lambda e: e.copy(out=y_[:, 0:512], in_=pa), reads=[pb], writes=[y_], partial=True)
                    else:
                        op("dve", lambda e: e.tensor_copy(out=y_[:, 512:1024], in_=pa), reads=[pb], writes=[y_], partial=True)
                s0 = ex * CAP + b_ * 128
                dma("pool", ys_d[s0:s0 + 128, :], y_[:, :], reads=[y_], writes=[K.db("ys")], partial=True)

        e_load(0)
        e_load(1)
        e_cast(0)
        for ex in range(NEXP):
            if ex + 2 < NEXP:
                e_load(ex + 2)
            if ex + 1 < NEXP:
                e_cast(ex + 1)
            e_compute(ex)
        K.barrier()
    if upto == "E":
        root.close()
        return nc
    with ExitStack() as st:
        wstage2 = [sb(st, "wstF%d" % i, [128, 1024], F32) for i in range(2)]
        w_pg = sb(st, "w_pg", [128, 8, D], BF16)
        w_pp = sb(st, "w_pp", [128, 2, D], BF16)
        g_ple = bc_load(st, "g_ple", ln_ple_d, D)
        for kc in range(10):
            stg = wstage2[kc % 2]
            src = w_pg_d[kc * 128:(kc + 1) * 128, :] if kc < 8 else w_pp_d[(kc - 8) * 128:(kc - 7) * 128, :]
            dstT = w_pg if kc < 8 else w_pp
            dst = w_pg[:, kc, :] if kc < 8 else w_pp[:, kc - 8, :]
            dma("sp", stg[:, :], src, writes=[stg])
            op("dve" if kc % 2 == 0 else "pool", lambda e, dst=dst, stg=stg: e.tensor_copy(out=dst, in_=stg[:, :]), reads=[stg],
               writes=[dstT], partial=True)
        x1t = [sb(st, "x1F%d" % i, [128, D], F32) for i in range(2)]
        y1 = [sb(st, "y1F%d" % i, [128, D], F32) for i in range(2)]
        y2 = [sb(st, "y2F%d" % i, [128, D], F32) for i in range(2)]
        pt_ = [sb(st, "ptF%d" % i, [128, 256], F32) for i in range(2)]
        ptb_l = [sb(st, "ptb%d" % _i, [128, 256], BF16) for _i in range(2)]
        pT_l = [sb(st, "pT%d" % _i, [128, 2, 128], BF16) for _i in range(2)]
        sq_l = [sb(st, "sqF%d" % _i, [128, D], BF16) for _i in range(2)]
        ssq_l = [sb(st, "ssqF%d" % _i, [128, 4], F32) for _i in range(2)]
        rt_l = [sb(st, "rtF%d" % _i, [128, 4], F32) for _i in range(2)]
        rs_l = [sb(st, "rsF%d" % _i, [128, 4], F32) for _i in range(2)]
        hnb_l = [sb(st, "hnb%d" % _i, [128, D], BF16) for _i in range(2)]
        hnT_l = [sb(st, "hnT%d" % _i, [128, 8, 128], BF16) for _i in range(2)]
        gsb_l = [sb(st, "gsb%d" % _i, [128, D], F32) for _i in range(2)]
        ot = [sb(st, "otF%d" % i, [128, D], F32) for i in range(2)]
        def f_tile(i):
            ptb = ptb_l[i % 2]
            pT = pT_l[i % 2]
            sq = sq_l[i % 2]
            ssq = ssq_l[i % 2]
            rt = rt_l[i % 2]
            rs = rs_l[i % 2]
            hnb = hnb_l[i % 2]
            hnT = hnT_l[i % 2]
            gsb = gsb_l[i % 2]
            seq = i // NTS
            jt = i % NTS
            x_ = x1t[i % 2]
            o_ = ot[i % 2]
            dma("sp", x_[:, :], x1_d[i * 128:(i + 1) * 128, :], reads=[K.db("x1", i)], writes=[x_])
            dma("sp", pt_[i % 2][:, :], p_d[seq, jt * 128:(jt + 1) * 128, :], writes=[pt_[i % 2]])
            ys_ = [y1[i % 2], y2[i % 2]]
            for j2 in range(2):
                dma("pool", ys_[j2][:, :], ys_d[:, :], reads=[K.db("ys"), dest_i], writes=[ys_[j2]],
                    indirect=dict(out_offset=None, in_offset=bass.IndirectOffsetOnAxis(ap=dest_i[:, i, j2:j2 + 1], axis=0)))
            yield
            for j2 in range(2):
                op("dve", lambda e, j2=j2: e.scalar_tensor_tensor(out=x_[:, :], in0=ys_[j2][:, :], scalar=wts[:, i, j2:j2 + 1],
                                                                  in1=x_[:, :], op0=ALU.mult, op1=ALU.add),
                   reads=[ys_[j2], wts, x_], writes=[x_])
            yield
            op("act", lambda e: e.activation(out=sq[:, :], in_=x_[:, :], func=AF.Square), reads=[x_], writes=[sq])
            op("dve", lambda e: e.tensor_reduce(out=ssq[:, 0:1], in_=sq[:, :], axis=AX.X, op=ALU.add), reads=[sq], writes=[ssq])
            rstd_from_ssq(ssq[:, 0:1], rt, rs[:, 0:1], D, [ssq, rs])
            op("dve", lambda e: e.scalar_tensor_tensor(out=hnb[:, :], in0=x_[:, :], scalar=rs[:, 0:1], in1=g_ple[:, :],
                                                       op0=ALU.mult, op1=ALU.mult), reads=[x_, rs, g_ple], writes=[hnb])
            yield
            pa, pb = bank()
            pbf = pa.bitcast(BF16)
            for kc in range(8):
                op("pe", lambda e, kc=kc: e.transpose(out=pbf[:, kc * 128:(kc + 1) * 128], in_=hnb[:, kc * 128:(kc + 1) * 128],
                                                      identity=ident_b[:, :]), reads=[hnb, ident_b], writes=[pb], signal=(kc == 7))
            op("act", lambda e: e.copy(out=hnT[:, :, :], in_=pbf.rearrange("p (k t) -> p k t", k=8)), reads=[pb], writes=[hnT])
            yield
            op("pool", lambda e: e.tensor_copy(out=ptb[:, :], in_=pt_[i % 2][:, :]), reads=[pt_[i % 2]], writes=[ptb])
            pa, pb = bank()
            pbf = pa.bitcast(BF16)
            for kc in range(2):
                op("pe", lambda e, kc=kc: e.transpose(out=pbf[:, kc * 128:(kc + 1) * 128], in_=ptb[:, kc * 128:(kc + 1) * 128],
                                                      identity=ident_b[:, :]), reads=[ptb, ident_b], writes=[pb], signal=(kc == 1))
            op("act", lambda e: e.copy(out=pT[:, :, :], in_=pbf[:, 0:256].rearrange("p (k t) -> p k t", k=2)), reads=[pb],
               writes=[pT])
            yield
            for hf in range(2):
                pg, pgb = bank()
                for kc in range(8):
                    op("pe", lambda e, kc=kc: e.matmul(pg, lhsT=hnT[:, kc, :], rhs=w_pg[:, kc, hf * 512:(hf + 1) * 512],
                                                       start=(kc == 0), stop=(kc == 7)), reads=[hnT, w_pg], writes=[pgb],
                       signal=(kc == 7))
                op("act", lambda e: e.activation(out=gsb[:, hf * 512:(hf + 1) * 512], in_=pg, func=AF.Sigmoid), reads=[pgb],
                   writes=[gsb], partial=True)
                pp, ppb = bank()
                for kc in range(2):
                    op("pe", lambda e, kc=kc: e.matmul(pp, lhsT=pT[:, kc, :], rhs=w_pp[:, kc, hf * 512:(hf + 1) * 512],
                                                       start=(kc == 0), stop=(kc == 1)), reads=[pT, w_pp], writes=[ppb],
                       signal=(kc == 1))
                op("dve", lambda e: e.tensor_tensor(out=o_[:, hf * 512:(hf + 1) * 512], in0=pp,
                                                    in1=gsb[:, hf * 512:(hf + 1) * 512], op=ALU.mult), reads=[ppb, gsb],
                   writes=[o_], partial=True)
            yield
            op("pool", lambda e: e.tensor_tensor(out=o_[:, :], in0=o_[:, :], in1=x_[:, :], op=ALU.add), reads=[o_, x_],
               writes=[o_])
            dma("sp", out_d[seq, jt * 128:(jt + 1) * 128, :], o_[:, :], reads=[o_], writes=[K.db("out", i)])
            yield
        pipeline([(lambda i=i: f_tile(i)) for i in range(NT)], 2)
        K.barrier()
    root.close()
    return nc


WNAMES = ["ln_mix", "w_in", "hg_lb", "hg_onorm", "w_oA", "mla_qa_norm", "mla_kva_norm", "w_uq", "w_ukv", "q_norm",
          "k_norm", "w_oB", "w_out", "ln_moe", "w_rg", "b_rg", "w_re", "b_re", "w1", "w3", "w2", "ln_ple",
          "w_ple_gate", "w_ple_proj"]


def make_in_maps(inputs, n_cores, NS):
    shared = {}
    for n in WNAMES:
        a = np.ascontiguousarray(np.asarray(inputs[n], dtype=np.float32))
        if n == "hg_lb":
            shared[n] = a
        else:
            shared[n] = a.reshape(a.shape[1:]) if a.shape[0] == 1 else a
    for n in ("ln_mix", "hg_onorm", "mla_qa_norm", "mla_kva_norm", "q_norm", "k_norm", "ln_moe", "b_rg", "b_re", "ln_ple"):
        shared[n] = shared[n].reshape(-1)
    x = np.asarray(inputs["x"], dtype=np.float32)
    p = np.asarray(inputs["p"], dtype=np.float32)[0]
    pos = np.asarray(inputs["positions"], dtype=np.int32)
    maps = []
    for c in range(n_cores):
        m = dict(shared)
        m["x"] = np.ascontiguousarray(x[c * NS:(c + 1) * NS])
        m["p"] = np.ascontiguousarray(p[c * NS:(c + 1) * NS])
        m["positions"] = np.ascontiguousarray(pos[c * NS:(c + 1) * NS])
        maps.append(m)
    return maps


def kernel(**inputs):
    n = 8
    NS = 2
    nc = build(NS=NS, S=2048, CAP=256, debug=True)
    maps = make_in_maps(inputs, n, NS)
    res = run_bass_kernel_spmd(nc, maps, core_ids=list(range(n)))
    return np.concatenate([np.asarray(r["out"]) for r in res.results], axis=0).astype(np.float32)
```

```python
import numpy as np
from contextlib import ExitStack
import concourse.bass as bass
import concourse.mybir as mybir
from concourse.alu_op_type import AluOpType as ALU
from concourse.bass_utils import run_bass_kernel_spmd

F32 = mybir.dt.float32
BF16 = mybir.dt.bfloat16
I32 = mybir.dt.int32
AF = mybir.ActivationFunctionType
AX = mybir.AxisListType

D = 1024
DIN = 5024
NEXP = 64
EPS = 1e-6
CH = 64


class Buf:
    __slots__ = ("w", "r")

    def __init__(self):
        self.w = {}
        self.r = {}


class T:
    def __init__(self, t):
        self.t = t
        self.b = Buf()

    def __getitem__(self, k):
        return self.t[k]


class Eng:
    def __init__(self, name, eng, sem):
        self.name = name
        self.eng = eng
        self.sem = sem
        self.cnt = 0
        self.seen = {}
        self.pr = []
        self.pw = []


class Ring:
    def __init__(self, nc, q, P):
        self.P = P
        self.sems = [nc.alloc_semaphore("dq_%s_%d" % (q, i)) for i in range(P)]
        self.cnt = [0] * P
        self.last = [None] * P
        self.n = 0


class KB:
    def __init__(self, nc):
        self.nc = nc
        self.engs = {}
        for name, e in (("pe", nc.tensor), ("act", nc.scalar), ("dve", nc.vector),
                        ("pool", nc.gpsimd), ("sp", nc.sync)):
            self.engs[name] = Eng(name, e, nc.alloc_semaphore("s_" + name))
        self.rings = {"sp": Ring(nc, "sp", 24), "act": Ring(nc, "act", 12), "pool": Ring(nc, "pool", 12)}
        self.dbufs = {}

    def db(self, *key):
        b = self.dbufs.get(key)
        if b is None:
            b = Buf()
            self.dbufs[key] = b
        return b

    def wait(self, en, tok):
        sem, val = tok
        E = self.engs[en]
        k = id(sem)
        if E.seen.get(k, 0) >= val:
            return
        E.eng.wait_ge(sem, val)
        E.seen[k] = val

    def _deps(self, en, reads, writes):
        for b in reads:
            for t in b.w.values():
                self.wait(en, t)
        for b in writes:
            for t in b.w.values():
                self.wait(en, t)
            for t in b.r.values():
                self.wait(en, t)

    @staticmethod
    def _rec(tok, reads, writes, partial):
        k = id(tok[0])
        for b in reads:
            b.r[k] = tok
        for b in writes:
            if partial:
                b.w[k] = tok
            else:
                b.w = {k: tok}
            b.r = {}

    def op(self, en, emit, reads=(), writes=(), signal=True, partial=False):
        E = self.engs[en]
        reads = [x.b if isinstance(x, T) else x for x in reads]
        writes = [x.b if isinstance(x, T) else x for x in writes]
        self._deps(en, reads, writes)
        ins = emit(E.eng)
        if signal:
            E.cnt += 1
            ins.then_inc(E.sem, 1)
            tok = (E.sem, E.cnt)
            self._rec(tok, E.pr + reads, [], False)
            self._rec(tok, [], E.pw + writes, partial)
            E.pr = []
            E.pw = []
        else:
            E.pr += reads
            E.pw += writes
        return ins

    def dma(self, q, out, in_, reads=(), writes=(), partial=False, indirect=None, **kw):
        E = self.engs[q]
        R = self.rings[q]
        reads = [x.b if isinstance(x, T) else x for x in reads]
        writes = [x.b if isinstance(x, T) else x for x in writes]
        self._deps(q, reads, writes)
        slot = R.n % R.P
        if R.last[slot] is not None:
            self.wait(q, R.last[slot])
        R.cnt[slot] += 16
        tok = (R.sems[slot], R.cnt[slot])
        if indirect is None:
            ins = E.eng.dma_start(out=out, in_=in_, **kw)
        else:
            ins = E.eng.indirect_dma_start(out=out, in_=in_, **indirect)
        ins.then_inc(R.sems[slot], 16)
        R.last[slot] = tok
        R.n += 1
        self._rec(tok, reads, writes, partial)
        return tok

    def barrier(self):
        toks = [(E.sem, E.cnt) for E in self.engs.values() if E.cnt > 0]
        for R in self.rings.values():
            toks += [t for t in R.last if t is not None]
        for en in self.engs:
            for t in toks:
                self.wait(en, t)


def build(NS=2, S=2048, CAP=256, debug=False, upto="F"):
    nc = bass.Bass("TRN2", target_bir_lowering=False)
    TT = NS * S
    NT = TT // 128
    NTS = S // 128
    NCH = S // CH
    NSLOT = NEXP * CAP
    assert S % 512 == 0

    def din(name, shape, dt=F32):
        return nc.dram_tensor(name, list(shape), dt, kind="ExternalInput").ap()

    def dscr(name, shape, dt):
        return nc.dram_tensor(name, list(shape), dt, kind="ExternalOutput" if debug else "Internal").ap()

    x_d = din("x", [NS, S, D])
    p_d = din("p", [NS, S, 256])
    pos_d = din("positions", [NS, S], I32)
    ln_mix_d = din("ln_mix", [D])
    w_in_d = din("w_in", [D, DIN])
    hg_lb_d = din("hg_lb", [2, 2, 512])
    hg_onorm_d = din("hg_onorm", [128])
    w_oA_d = din("w_oA", [512, D])
    qa_norm_d = din("mla_qa_norm", [256])
    kva_norm_d = din("mla_kva_norm", [128])
    w_uq_d = din("w_uq", [256, 768])
    w_ukv_d = din("w_ukv", [128, 1024])
    q_norm_d = din("q_norm", [96])
    k_norm_d = din("k_norm", [96])
    w_oB_d = din("w_oB", [512, D])
    w_out_d = din("w_out", [D, D])
    ln_moe_d = din("ln_moe", [D])
    w_rg_d = din("w_rg", [D, 8])
    b_rg_d = din("b_rg", [8])
    w_re_d = din("w_re", [D, 64])
    b_re_d = din("b_re", [64])
    w1_d = din("w1", [NEXP, D, 256])
    w3_d = din("w3", [NEXP, D, 256])
    w2_d = din("w2", [NEXP, 256, D])
    ln_ple_d = din("ln_ple", [D])
    w_pg_d = din("w_ple_gate", [D, D])
    w_pp_d = din("w_ple_proj", [256, D])
    out_d = nc.dram_tensor("out", [NS, S, D], F32, kind="ExternalOutput").ap()

    zq_d = dscr("zq", [NS, 4, 128, S], BF16)
    zf_d = dscr("zf", [NS, 2, 4, 128, S], F32)
    zv_d = dscr("zv", [TT, 512], BF16)
    zog_d = dscr("zog", [TT, 512], BF16)
    zg_d = dscr("zg", [TT, 2048], BF16)
    qT_d = dscr("qT", [NS, 96, 8, S], BF16)
    kT_d = dscr("kT", [NS, 96, 8, S], BF16)
    vm_d = dscr("vm", [TT, 8, 64], BF16)
    x1_d = dscr("x1", [TT, D], F32)
    hmb_d = dscr("hmb", [TT, D], BF16)
    xs_d = dscr("xs", [NSLOT, D], BF16)
    ys_d = dscr("ys", [NSLOT, D], F32)

    K = KB(nc)

    def pipeline(makers, W):
        active = []
        it = iter(makers)
        more = True
        while True:
            while len(active) < W and more:
                try:
                    active.append(next(it)())
                except StopIteration:
                    more = False
            if not active:
                break
            for g_ in list(active):
                try:
                    next(g_)
                except StopIteration:
                    active.remove(g_)
    op = K.op
    dma = K.dma
    root = ExitStack()
    right_stacks = []
    uid = [0]

    def sb(st, name, shape, dt, side="left"):
        if st is root or st in right_stacks:
            side = "right"
        uid[0] += 1
        return T(st.enter_context(nc.sbuf_tensor("sb%d_%s" % (uid[0], name), list(shape), dt, side=side)))

    ps_all = root.enter_context(nc.psum_tensor("ps_all", [128, 4096], F32))
    banks = [Buf() for _ in range(8)]
    bank_i = [0]

    reserved = set()

    def bank():
        while True:
            i = bank_i[0] % 8
            bank_i[0] += 1
            if i not in reserved:
                break
        return ps_all[:, i * 512:(i + 1) * 512], banks[i]

    def fixed_bank(i):
        return ps_all[:, i * 512:(i + 1) * 512], banks[i]

    dif_i = sb(root, "dif_i", [128, 128], I32)
    dif = sb(root, "dif", [128, 128], F32)
    ident_b = sb(root, "ident_b", [128, 128], BF16)
    ident_f = sb(root, "ident_f", [128, 128], F32)
    maskf = sb(root, "maskf", [128, 128], F32)
    maskb = sb(root, "maskb", [128, 128], F32)
    lstrict = sb(root, "lstrict", [128, 128], BF16)
    ones_b = sb(root, "ones_b", [128, 128], BF16)
    sel = sb(root, "sel", [128, 64], F32)
    op("pool", lambda e: e.iota(dif_i[:], pattern=[[1, 128]], base=0, channel_multiplier=-1), writes=[dif_i])
    op("dve", lambda e: e.tensor_copy(out=dif[:], in_=dif_i[:]), reads=[dif_i], writes=[dif])
    for dst, cmp_ in ((ident_b, ALU.is_equal), (ident_f, ALU.is_equal), (maskf, ALU.is_ge),
                      (maskb, ALU.is_le), (lstrict, ALU.is_gt)):
        op("dve", lambda e, dst=dst, cmp_=cmp_: e.tensor_single_scalar(out=dst[:], in_=dif[:], scalar=0.0, op=cmp_),
           reads=[dif], writes=[dst])
    op("dve", lambda e: e.memset(ones_b[:], 1.0), writes=[ones_b])
    op("dve", lambda e: e.memset(sel[:], 0.0), writes=[sel])
    op("dve", lambda e: e.memset(sel[64:65, :], 1.0), writes=[sel])

    def bc_load(st, name, src1d, n):
        t = sb(st, name, [128, n], F32)
        dma("sp", t[:], src1d.partition_broadcast(128), writes=[t])
        return t

    def rstd_from_ssq(ssq_ap, tmp, out_ap, dim, bufs):
        P_, n_ = ssq_ap.shape[0], ssq_ap.shape[1]
        op("dve", lambda e: e.tensor_scalar(out=tmp[0:P_, 0:n_], in0=ssq_ap, scalar1=1.0 / dim, scalar2=EPS,
                                            op0=ALU.mult, op1=ALU.add), reads=bufs, writes=[tmp])
        op("act", lambda e: e.activation(out=tmp[0:P_, 0:n_], in_=tmp[0:P_, 0:n_], func=AF.Sqrt),
           reads=[tmp], writes=[tmp])
        op("dve", lambda e: e.reciprocal(out=out_ap, in_=tmp[0:P_, 0:n_]), reads=[tmp], writes=bufs)

    def tmp_ap(tmp, like):
        n = like.shape[-1] if len(like.shape) == 2 else None
        return tmp[0:like.shape[0], 0:like.shape[1]]

    phA = ExitStack()
    Win = sb(phA, "Win", [128, 8, DIN], BF16)
    wstage = [sb(phA, "wstage%d" % i, [128, 1024], F32) for i in range(3)]
    cast_engs = ["dve", "pool", "act"]
    cast_i = [0]
    wst_cur = [wstage]

    def load_cast(dst_ap, dst_T, src_ap, ncols, npart=128):
        i = cast_i[0]
        cast_i[0] += 1
        stg = wst_cur[0][i % 3]
        if npart != 128:
            dma("sp", stg[0:npart, 0:ncols], src_ap, writes=[stg])
            op("dve", lambda e: e.tensor_copy(out=dst_ap, in_=stg[0:npart, 0:ncols]), reads=[stg], writes=[dst_T],
               partial=True)
            return
        dma("sp", stg[:, 0:ncols], src_ap, writes=[stg])
        en = cast_engs[i % 3]
        if en == "act":
            op("act", lambda e: e.copy(out=dst_ap, in_=stg[:, 0:ncols]), reads=[stg], writes=[dst_T], partial=True)
        else:
            op(en, lambda e: e.tensor_copy(out=dst_ap, in_=stg[:, 0:ncols]), reads=[stg], writes=[dst_T], partial=True)

    for kc in range(8):
        for c0 in range(0, DIN, 1024):
            c1 = min(DIN, c0 + 1024)
            load_cast(Win[:, kc, c0:c1], Win, w_in_d[kc * 128:(kc + 1) * 128, c0:c1], c1 - c0)

    mixW = phA
    w_uq = sb(mixW, "w_uq", [128, 2, 768], BF16)
    w_ukv = sb(mixW, "w_ukv", [128, 1024], BF16)
    for kc in range(2):
        load_cast(w_uq[:, kc, :], w_uq, w_uq_d[kc * 128:(kc + 1) * 128, :], 768)
    load_cast(w_ukv[:, :], w_ukv, w_ukv_d[:, :], 1024)
    g_mix = bc_load(mixW, "g_mix", ln_mix_d, D)
    g_qa = bc_load(mixW, "g_qa", qa_norm_d, 256)
    g_kva = bc_load(mixW, "g_kva", kva_norm_d, 128)
    g_qn = bc_load(mixW, "g_qn", q_norm_d, 96)
    g_kn = bc_load(mixW, "g_kn", k_norm_d, 96)
    g_on = bc_load(root, "g_on", hg_onorm_d, 128)

    lbraw = sb(root, "lbraw", [128, 16], F32)
    lb = sb(root, "lb", [128, 8], F32)
    oml = sb(root, "oml", [128, 8], F32)
    with nc.allow_non_contiguous_dma(reason="tiny param load"):
        dma("sp", lbraw[:, :], hg_lb_d.rearrange("d l (h k) -> k (d l h)", k=128), writes=[lbraw])
    lbv = lbraw[:, :].rearrange("p (d l h) -> p d l h", d=2, l=2)
    op("dve", lambda e: e.tensor_tensor(out=lb[:, :].rearrange("p (d h) -> p d h", d=2), in0=lbv[:, :, 0, :],
                                        in1=lbv[:, :, 1, :], op=ALU.subtract), reads=[lbraw], writes=[lb])
    op("act", lambda e: e.activation(out=lb[:, :], in_=lb[:, :], func=AF.Sigmoid), reads=[lb], writes=[lb])
    op("dve", lambda e: e.tensor_scalar(out=oml[:, :], in0=lb[:, :], scalar1=-1.0, scalar2=1.0, op0=ALU.mult,
                                        op1=ALU.add), reads=[lb], writes=[oml])

    pos_r = sb(mixW, "pos_r", [NT, 128], I32)
    dma("sp", pos_r[:, :], pos_d.rearrange("s (n p) -> (s n) p", p=128), writes=[pos_r])
    posr_f = sb(mixW, "posr_f", [NT, 128], F32)
    op("dve", lambda e: e.tensor_copy(out=posr_f[:, :], in_=pos_r[:, :]), reads=[pos_r], writes=[posr_f])
    posf = sb(mixW, "posf", [128, NT], F32)
    pa_, pb_ = bank()
    op("pe", lambda e: e.transpose(out=pa_[:, 0:NT], in_=posr_f[:, :], identity=ident_f[0:NT, 0:NT]),
       reads=[posr_f, ident_f], writes=[pb_])
    op("dve", lambda e: e.tensor_copy(out=posf[:, :], in_=pa_[:, 0:NT]), reads=[pb_], writes=[posf])
    invf = sb(mixW, "invf", [128, 16], F32)
    for i in range(16):
        v = float(10000.0 ** (-(i / 16.0)) / (2.0 * np.pi))
        op("dve", lambda e, i=i, v=v: e.memset(invf[:, i:i + 1], v), writes=[invf], partial=True)
    rope_cs = sb(mixW, "rope_cs", [128, NT, 32], F32)
    with ExitStack() as st0:
        turns = sb(st0, "turns", [128, NT, 32], F32)
        ti = sb(st0, "turns_i", [128, NT, 32], I32)
        tf = sb(st0, "turns_f", [128, NT, 32], F32)
        adj = sb(st0, "adj", [128, NT, 32], F32)
        op("dve", lambda e: e.tensor_tensor(out=turns[:, :, 16:32], in0=posf[:, :].unsqueeze(2).broadcast_to([128, NT, 16]),
                                            in1=invf[:, :].unsqueeze(1).broadcast_to([128, NT, 16]), op=ALU.mult),
           reads=[posf, invf], writes=[turns])
        op("dve", lambda e: e.tensor_scalar(out=turns[:, :, 0:16], in0=turns[:, :, 16:32], scalar1=0.25, scalar2=None,
                                            op0=ALU.add), reads=[turns], writes=[turns])
        op("dve", lambda e: e.tensor_copy(out=ti[:], in_=turns[:]), reads=[turns], writes=[ti])
        op("dve", lambda e: e.tensor_copy(out=tf[:], in_=ti[:]), reads=[ti], writes=[tf])
        op("dve", lambda e: e.tensor_tensor(out=turns[:], in0=turns[:], in1=tf[:], op=ALU.subtract), reads=[turns, tf],
           writes=[turns])
        op("dve", lambda e: e.tensor_single_scalar(out=adj[:], in_=turns[:], scalar=0.5, op=ALU.is_gt), reads=[turns],
           writes=[adj])
        op("dve", lambda e: e.tensor_tensor(out=turns[:], in0=turns[:], in1=adj[:], op=ALU.subtract), reads=[turns, adj],
           writes=[turns])
        op("dve", lambda e: e.tensor_single_scalar(out=adj[:], in_=turns[:], scalar=-0.5, op=ALU.is_lt), reads=[turns],
           writes=[adj])
        op("dve", lambda e: e.tensor_tensor(out=turns[:], in0=turns[:], in1=adj[:], op=ALU.add), reads=[turns, adj],
           writes=[turns])
        op("dve", lambda e: e.tensor_scalar(out=turns[:], in0=turns[:], scalar1=0.4999999, scalar2=-0.4999999,
                                            op0=ALU.min, op1=ALU.max), reads=[turns], writes=[turns])
        op("act", lambda e: e.activation(out=rope_cs[:], in_=turns[:], func=AF.Sin, scale=float(2.0 * np.pi)),
           reads=[turns], writes=[rope_cs])
        K.barrier()

    zt = sb(root, "zt", [128, D], BF16)
    op("pool", lambda e: e.memset(zt[:, :], 0.0), writes=[zt])
    xs_v = xs_d.rearrange("(a p) d -> p a d", p=128)
    NA = NSLOT // 128
    for a0 in range(0, NA, 16):
        a1 = min(NA, a0 + 16)
        dma("sp", xs_v[:, a0:a1, :], zt[:, :].unsqueeze(1).broadcast_to([128, a1 - a0, D]), reads=[zt],
            writes=[K.db("xs")], partial=True)

    stg_i = [0]
    with ExitStack() as st:
        xt = [sb(st, "xt%d" % i, [128, D], F32) for i in range(2)]
        sq = sb(st, "sq", [128, D], BF16)
        ssq = sb(st, "ssq", [128, 4], F32)
        rt = sb(st, "rt", [128, 16], F32)
        rs = sb(st, "rs", [128, 4], F32)
        hn = [sb(st, "hn%d" % i, [128, D], BF16) for i in range(2)]
        hT4 = [sb(st, "hT4_%d" % i, [128, 8, 512], BF16) for i in range(2)]
        stg = [sb(st, "stg%d" % i, [128, 512], F32) for i in range(6)]
        stgb = [sb(st, "stgb%d" % i, [128, 512], BF16) for i in range(6)]
        cz = [sb(st, "cz%d" % i, [128, 416], F32) for i in range(2)]
        czs = sb(st, "czs", [128, 384], BF16)
        cn = sb(st, "cn", [128, 384], BF16)
        cT = sb(st, "cT", [128, 3, 128], BF16)
        qk = sb(st, "qk", [128, 2, 8, 96], F32)
        qk2 = sb(st, "qk2", [128, 2, 8, 96], BF16)
        qss = sb(st, "qss", [128, 16], F32)
        qrs = sb(st, "qrs", [128, 16], F32)
        qkn = sb(st, "qkn", [128, 2, 8, 96], F32)
        qkb = sb(st, "qkb", [128, 2, 8, 96], BF16)
        rtmp = [sb(st, "rtmp%d" % i, [128, 2, 8, 16], F32) for i in range(4)]
        qkT = [sb(st, "qkT%d" % i, [96, 8, 128], BF16) for i in range(2)]
        vmb = sb(st, "vmb", [128, 8, 64], BF16)

        def next_stg(bf):
            i = stg_i[0]
            stg_i[0] += 1
            return (stgb if bf else stg)[i % 6]

        NG = TT // 512
        for g in range(NG):
            seq = (g * 512) // S
            t0 = (g * 512) % S
            h4 = hT4[g % 2]
            for j in range(4):
                i = g * 4 + j
                x_ = xt[i % 2]
                h_ = hn[i % 2]
                dma("sp", x_[:, :], x_d[seq, t0 + j * 128:t0 + (j + 1) * 128, :], writes=[x_])
                op("act", lambda e: e.activation(out=sq[:, :], in_=x_[:, :], func=AF.Square), reads=[x_], writes=[sq])
                op("dve", lambda e: e.tensor_reduce(out=ssq[:, 0:1], in_=sq[:, :], axis=AX.X, op=ALU.add), reads=[sq],
                   writes=[ssq])
                rstd_from_ssq(ssq[:, 0:1], rt, rs[:, 0:1], D, [ssq, rs])
                op("dve", lambda e: e.scalar_tensor_tensor(out=h_[:, :], in0=x_[:, :], scalar=rs[:, 0:1], in1=g_mix[:, :],
                                                           op0=ALU.mult, op1=ALU.mult), reads=[x_, rs, g_mix], writes=[h_])
                pa, pb = bank()
                pbf = pa.bitcast(BF16)
                for kc in range(8):
                    op("pe", lambda e, kc=kc: e.transpose(out=pbf[:, kc * 128:(kc + 1) * 128],
                                                          in_=h_[:, kc * 128:(kc + 1) * 128], identity=ident_b[:, :]),
                       reads=[h_, ident_b], writes=[pb], signal=(kc == 7))
                op("act", lambda e: e.copy(out=h4[:, :, j * 128:(j + 1) * 128],
                                           in_=pbf.rearrange("p (k t) -> p k t", k=8)), reads=[pb], writes=[h4],
                   partial=True)

            for c in range(12):
                pa, pb = bank()
                for kc in range(8):
                    op("pe", lambda e, kc=kc: e.matmul(pa, lhsT=Win[:, kc, c * 128:(c + 1) * 128], rhs=h4[:, kc, :],
                                                       start=(kc == 0), stop=(kc == 7)),
                       reads=[Win, h4], writes=[pb], signal=(kc == 7))
                if c < 4:
                    s_ = next_stg(True)
                    op("act", lambda e: e.activation(out=s_[:, :], in_=pa, func=AF.Silu), reads=[pb], writes=[s_])
                    dma("act", zq_d[seq, c, :, t0:t0 + 512], s_[:, :], reads=[s_], writes=[K.db("zq", seq, c, g)])
                else:
                    d_ = (c - 4) // 4
                    h_i = (c - 4) % 4
                    s_ = next_stg(False)
                    op("dve", lambda e: e.tensor_copy(out=s_[:, :], in_=pa), reads=[pb], writes=[s_])
                    dma("sp", zf_d[seq, d_, h_i, :, t0:t0 + 512], s_[:, :], reads=[s_],
                        writes=[K.db("zf", seq, d_, h_i, g)])

            for j in range(4):
                i = g * 4 + j
                tok0 = i * 128
                lts = h4[:, :, j * 128:(j + 1) * 128]
                groups = [(1536, 2048, "v"), (2048, 2560, "og"), (2560, 2976, "c"), (2976, 3488, "g0"),
                          (3488, 4000, "g1"), (4000, 4512, "g2"), (4512, 5024, "g3")]
                for (c0, c1, kind) in groups:
                    pa, pb = bank()
                    n = c1 - c0
                    for kc in range(8):
                        op("pe", lambda e, kc=kc: e.matmul(pa[:, 0:n], lhsT=lts[:, kc, :], rhs=Win[:, kc, c0:c1],
                                                           start=(kc == 0), stop=(kc == 7)),
                           reads=[Win, h4], writes=[pb], signal=(kc == 7))
                    if kind == "v":
                        s_ = next_stg(True)
                        op("dve", lambda e: e.tensor_copy(out=s_[:, :], in_=pa), reads=[pb], writes=[s_])
                        dma("sp", zv_d[tok0:tok0 + 128, :], s_[:, :], reads=[s_], writes=[K.db("zv", i)])
                    elif kind == "og":
                        s_ = next_stg(True)
                        op("act", lambda e: e.activation(out=s_[:, :], in_=pa, func=AF.Silu), reads=[pb], writes=[s_])
                        dma("act", zog_d[tok0:tok0 + 128, :], s_[:, :], reads=[s_], writes=[K.db("zog", i)])
                    elif kind[0] == "g":
                        gi = int(kind[1])
                        s_ = next_stg(True)
                        op("act", lambda e: e.activation(out=s_[:, :], in_=pa, func=AF.Sigmoid), reads=[pb], writes=[s_])
                        dma("act", zg_d[tok0:tok0 + 128, gi * 512:(gi + 1) * 512], s_[:, :], reads=[s_],
                            writes=[K.db("zg", i, gi)])
                    else:
                        c_ = cz[i % 2]
                        op("dve", lambda e: e.tensor_copy(out=c_[:, :], in_=pa[:, 0:416]), reads=[pb], writes=[c_])
                        op("act", lambda e: e.activation(out=czs[:, :], in_=c_[:, 0:384], func=AF.Square), reads=[c_],
                           writes=[czs])
                        op("dve", lambda e: e.tensor_reduce(out=ssq[:, 1:2], in_=czs[:, 0:256], axis=AX.X, op=ALU.add),
                           reads=[czs], writes=[ssq])
                        op("dve", lambda e: e.tensor_reduce(out=ssq[:, 2:3], in_=czs[:, 256:384], axis=AX.X, op=ALU.add),
                           reads=[czs], writes=[ssq])
                        rstd_from_ssq(ssq[:, 1:2], rt, rs[:, 1:2], 256, [ssq, rs])
                        rstd_from_ssq(ssq[:, 2:3], rt, rs[:, 2:3], 128, [ssq, rs])
                        op("dve", lambda e: e.scalar_tensor_tensor(out=cn[:, 0:256], in0=c_[:, 0:256], scalar=rs[:, 1:2],
                                                                   in1=g_qa[:, :], op0=ALU.mult, op1=ALU.mult),
                           reads=[c_, rs, g_qa], writes=[cn])
                        op("dve", lambda e: e.scalar_tensor_tensor(out=cn[:, 256:384], in0=c_[:, 256:384], scalar=rs[:, 2:3],
                                                                   in1=g_kva[:, :], op0=ALU.mult, op1=ALU.mult),
                           reads=[c_, rs, g_kva], writes=[cn])
                        pa2, pb2 = bank()
                        pbf2 = pa2.bitcast(BF16)
                        for kc in range(3):
                            op("pe", lambda e, kc=kc: e.transpose(out=pbf2[:, kc * 128:(kc + 1) * 128],
                                                                  in_=cn[:, kc * 128:(kc + 1) * 128], identity=ident_b[:, :]),
                               reads=[cn, ident_b], writes=[pb2], signal=(kc == 2))
                        op("act", lambda e: e.copy(out=cT[:, :, :], in_=pbf2[:, 0:384].rearrange("p (k t) -> p k t", k=3)),
                           reads=[pb2], writes=[cT])
                        pq0, pqb0 = bank()
                        pq1, pqb1 = bank()
                        for kc in range(2):
                            op("pe", lambda e, kc=kc: e.matmul(pq0, lhsT=cT[:, kc, :], rhs=w_uq[:, kc, 0:512],
                                                               start=(kc == 0), stop=(kc == 1)),
                               reads=[cT, w_uq], writes=[pqb0], signal=(kc == 1))
                        for kc in range(2):
                            op("pe", lambda e, kc=kc: e.matmul(pq1[:, 0:256], lhsT=cT[:, kc, :], rhs=w_uq[:, kc, 512:768],
                                                               start=(kc == 0), stop=(kc == 1)),
                               reads=[cT, w_uq], writes=[pqb1], signal=(kc == 1))
                        qflat = qk[:, 0, :, :].rearrange("p h d -> p (h d)")
                        op("act", lambda e: e.copy(out=qflat[:, 0:512], in_=pq0), reads=[pqb0], writes=[qk], partial=True)
                        op("act", lambda e: e.copy(out=qflat[:, 512:768], in_=pq1[:, 0:256]), reads=[pqb1], writes=[qk],
                           partial=True)
                        pk0, pkb0 = bank()
                        pk1, pkb1 = bank()
                        op("pe", lambda e: e.matmul(pk0, lhsT=cT[:, 2, :], rhs=w_ukv[:, 0:512], start=True, stop=True),
                           reads=[cT, w_ukv], writes=[pkb0])
                        op("pe", lambda e: e.matmul(pk1, lhsT=cT[:, 2, :], rhs=w_ukv[:, 512:1024], start=True, stop=True),
                           reads=[cT, w_ukv], writes=[pkb1])
                        for hh, (pk, pkb) in enumerate(((pk0, pkb0), (pk1, pkb1))):
                            pkv = pk.rearrange("p (h d) -> p h d", h=4)
                            op("dve", lambda e: e.tensor_copy(out=qk[:, 1, hh * 4:(hh + 1) * 4, 0:64], in_=pkv[:, :, 0:64]),
                               reads=[pkb], writes=[qk], partial=True)
                            op("act", lambda e: e.copy(out=vmb[:, hh * 4:(hh + 1) * 4, :], in_=pkv[:, :, 64:128]),
                               reads=[pkb], writes=[vmb], partial=True)
                        op("dve", lambda e: e.tensor_copy(out=qk[:, 1, :, 64:96],
                                                          in_=c_[:, 384:416].unsqueeze(1).broadcast_to([128, 8, 32])),
                           reads=[c_], writes=[qk], partial=True)
                        dma("act", vm_d[tok0:tok0 + 128, :, :], vmb[:, :, :], reads=[vmb], writes=[K.db("vm", i)])
                        op("act", lambda e: e.activation(out=qk2[:], in_=qk[:], func=AF.Square), reads=[qk], writes=[qk2])
                        op("dve", lambda e: e.tensor_reduce(out=qss[:, :], in_=qk2[:].rearrange("p a h d -> p (a h) d"),
                                                            axis=AX.X, op=ALU.add), reads=[qk2], writes=[qss])
                        rstd_from_ssq(qss[:, :], rt, qrs[:, :], 96, [qss, qrs])
                        op("dve", lambda e: e.tensor_tensor(out=qkn[:].rearrange("p a h d -> p (a h) d"),
                                                            in0=qk[:].rearrange("p a h d -> p (a h) d"),
                                                            in1=qrs[:, :].unsqueeze(2).broadcast_to([128, 16, 96]),
                                                            op=ALU.mult), reads=[qk, qrs], writes=[qkn])
                        for a_, gg in ((0, g_qn), (1, g_kn)):
                            op("dve", lambda e, a_=a_, gg=gg: e.tensor_tensor(
                                out=qkn[:, a_, :, :], in0=qkn[:, a_, :, :],
                                in1=gg[:, :].unsqueeze(1).broadcast_to([128, 8, 96]), op=ALU.mult),
                               reads=[qkn, gg], writes=[qkn])
                        cosb = rope_cs[:, i, 0:16].unsqueeze(1).unsqueeze(1).broadcast_to([128, 2, 8, 16])
                        sinb = rope_cs[:, i, 16:32].unsqueeze(1).unsqueeze(1).broadcast_to([128, 2, 8, 16])
                        x1_ = qkn[:, :, :, 64:80]
                        x2_ = qkn[:, :, :, 80:96]
                        op("dve", lambda e: e.tensor_tensor(out=rtmp[0][:], in0=x1_, in1=cosb, op=ALU.mult),
                           reads=[qkn, rope_cs], writes=[rtmp[0]])
                        op("pool", lambda e: e.tensor_tensor(out=rtmp[1][:], in0=x2_, in1=sinb, op=ALU.mult),
                           reads=[qkn, rope_cs], writes=[rtmp[1]])
                        op("dve", lambda e: e.tensor_tensor(out=rtmp[2][:], in0=x2_, in1=cosb, op=ALU.mult),
                           reads=[qkn, rope_cs], writes=[rtmp[2]])
                        op("pool", lambda e: e.tensor_tensor(out=rtmp[3][:], in0=x1_, in1=sinb, op=ALU.mult),
                           reads=[qkn, rope_cs], writes=[rtmp[3]])
                        op("act", lambda e: e.copy(out=qkb[:, :, :, 0:64], in_=qkn[:, :, :, 0:64]), reads=[qkn],
                           writes=[qkb], partial=True)
                        op("dve", lambda e: e.tensor_tensor(out=qkb[:, :, :, 64:80], in0=rtmp[0][:], in1=rtmp[1][:],
                                                            op=ALU.subtract), reads=[rtmp[0], rtmp[1]], writes=[qkb],
                           partial=True)
                        op("dve", lambda e: e.tensor_tensor(out=qkb[:, :, :, 80:96], in0=rtmp[2][:], in1=rtmp[3][:],
                                                            op=ALU.add), reads=[rtmp[2], rtmp[3]], writes=[qkb],
                           partial=True)
                        for a_, dst in ((0, qT_d), (1, kT_d)):
                            pa3, pb3 = bank()
                            pbf3 = pa3.bitcast(BF16)
                            for h in range(8):
                                op("pe", lambda e, h=h, a_=a_: e.transpose(out=pbf3[0:96, h * 128:(h + 1) * 128],
                                                                           in_=qkb[:, a_, h, :], identity=ident_b[:, :]),
                                   reads=[qkb, ident_b], writes=[pb3], signal=(h == 7))
                            qt_ = qkT[a_]
                            op("act" if a_ == 0 else "dve",
                               (lambda e: e.copy(out=qt_[:, :, :], in_=pbf3[0:96, :].rearrange("p (h t) -> p h t", h=8)))
                               if a_ == 0 else
                               (lambda e: e.tensor_copy(out=qt_[:, :, :], in_=pbf3[0:96, :].rearrange("p (h t) -> p h t", h=8))),
                               reads=[pb3], writes=[qt_])
                            tloc = t0 + j * 128
                            dma("sp", dst[seq, :, :, tloc:tloc + 128], qt_[:, :, :], reads=[qt_],
                                writes=[K.db("qkT", a_, i)])
        K.barrier()
    phA.close()
    if upto == "A":
        root.close()
        return nc

    moeR = root
    Moh = sb(moeR, "Moh", [128, NT, 2, 64], BF16)
    wts = sb(moeR, "wts", [128, NT, 2], F32)
    dest_i = sb(root, "dest_i", [128, NT, 2], I32)

    for seq in range(NS):
        seqst = ExitStack()
        oaT = sb(seqst, "oaT", [128, 4, S], BF16)
        obT = sb(seqst, "obT", [64, 8, S], BF16)

        with ExitStack() as st:
            qTs = sb(st, "qTs", [128, S], BF16)
            vtk = sb(st, "vtk", [CH, NCH, 128], BF16)
            ogt = sb(st, "ogt", [CH, NCH, 128], BF16)
            qt = [sb(st, "qt%d" % d, [128, S], BF16) for d in range(2)]
            kt = [sb(st, "kt%d" % d, [128, S], BF16) for d in range(2)]
            kdt = [sb(st, "kdt%d" % d, [CH, NCH, 128], BF16) for d in range(2)]
            dec = [sb(st, "dec%d" % d, [128, NCH], F32) for d in range(2)]
            S32 = [sb(st, "S32_%d" % d, [128, 128], F32) for d in range(2)]
            Sbf = [sb(st, "Sbf_%d" % d, [128, 128], BF16) for d in range(2)]
            PT = [[sb(st, "PT%d_%d" % (d, i), [CH, CH], BF16) for i in range(2)] for d in range(2)]
            for h in range(4):
                pst = ExitStack()
                SH = S // 2
                NCH2 = NCH // 2
                smask = sb(pst, "smask", [128, SH], F32)
                op("dve", lambda e: e.memset(smask[:, :], 1.0), writes=[smask])
                op("dve", lambda e: e.memset(smask[:, :].rearrange("p (c j) -> p c j", j=CH)[:, :, 0:1], 0.0), writes=[smask])
                lgH = [sb(pst, "lg%d" % i, [128, SH], F32) for i in range(2)]
                fAH = [sb(pst, "fA%d" % i, [128, SH], F32) for i in range(2)]
                lfH = [sb(pst, "lf%d" % i, [128, SH], F32) for i in range(2)]
                kkH = [sb(pst, "kk%d" % i, [128, SH], F32) for i in range(2)]
                gAH = [sb(pst, "gA%d" % i, [128, SH], F32) for i in range(2)]
                eAH = [sb(pst, "eA%d" % i, [128, SH], F32) for i in range(2)]
                kdT = sb(pst, "kdT", [128, S], BF16)
                for g in range(S // 512):
                    dma("sp", qTs[:, g * 512:(g + 1) * 512], zq_d[seq, h, :, g * 512:(g + 1) * 512],
                        reads=[K.db("zq", seq, h, seq * (S // 512) + g)], writes=[qTs], partial=True)
                dma("sp", vtk[:, :, :], zv_d[seq * S:(seq + 1) * S, h * 128:(h + 1) * 128].rearrange("(c p) v -> p c v", p=CH),
                    reads=[K.db("zv", i) for i in range(seq * NTS, (seq + 1) * NTS)], writes=[vtk])
                dma("sp", ogt[:, :, :], zog_d[seq * S:(seq + 1) * S, h * 128:(h + 1) * 128].rearrange("(c p) v -> p c v", p=CH),
                    reads=[K.db("zog", i) for i in range(seq * NTS, (seq + 1) * NTS)], writes=[ogt])
                for d in range(2):
                    col = d * 4 + h
                    for hf in range(2):
                        for g in range(SH // 512):
                            gg = hf * (SH // 512) + g
                            dma("sp", lgH[hf][:, g * 512:(g + 1) * 512], zf_d[seq, d, h, :, gg * 512:(gg + 1) * 512],
                                reads=[K.db("zf", seq, d, h, seq * (S // 512) + gg)], writes=[lgH[hf]], partial=True)
                    HF = (0, 1)

                    def v3(t_):
                        return t_[:, :].rearrange("p (c j) -> p c j", j=CH)
                    for hf in HF:
                        op("act", lambda e: e.activation(out=fAH[hf][:, :], in_=lgH[hf][:, :], func=AF.Sigmoid), reads=[lgH[hf]],
                           writes=[fAH[hf]])
                    for hf in HF:
                        op("dve", lambda e: e.tensor_scalar(out=fAH[hf][:, :], in0=fAH[hf][:, :], scalar1=oml[:, col:col + 1],
                                                            scalar2=lb[:, col:col + 1], op0=ALU.mult, op1=ALU.add),
                           reads=[fAH[hf], oml, lb], writes=[fAH[hf]])
                    for hf in HF:
                        op("act", lambda e: e.activation(out=lfH[hf][:, :], in_=fAH[hf][:, :], func=AF.Ln), reads=[fAH[hf]],
                           writes=[lfH[hf]])
                        op("pool", lambda e: e.tensor_scalar(out=kkH[hf][:, :], in0=fAH[hf][:, :], scalar1=-1.0, scalar2=1.0,
                                                             op0=ALU.mult, op1=ALU.add), reads=[fAH[hf]], writes=[kkH[hf]])
                    for hf in HF:
                        op("dve", lambda e: e.tensor_tensor_scan(out=gAH[hf][:, :], data0=smask[:, :], data1=lfH[hf][:, :],
                                                                 initial=0.0, op0=ALU.mult, op1=ALU.add),
                           reads=[smask, lfH[hf]], writes=[gAH[hf]])
                    GH = []
                    for hf in HF:
                        if d == 0:
                            GH.append(gAH[hf])
                        else:
                            op("dve", lambda e: e.tensor_tensor(out=lgH[hf][:, :], in0=lfH[hf][:, :], in1=gAH[hf][:, :],
                                                                op=ALU.subtract), reads=[lfH[hf], gAH[hf]], writes=[lgH[hf]])
                            op("dve", lambda e: e.tensor_tensor(out=v3(lgH[hf]), in0=v3(lgH[hf]),
                                                                in1=v3(gAH[hf])[:, :, CH - 1:CH].broadcast_to([128, NCH2, CH]),
                                                                op=ALU.add), reads=[lgH[hf], gAH[hf]], writes=[lgH[hf]])
                            GH.append(lgH[hf])
                    for hf in HF:
                        glast = v3(gAH[hf])[:, :, CH - 1:CH]
                        op("act", lambda e: e.activation(out=dec[d][:, hf * NCH2:(hf + 1) * NCH2],
                                                         in_=glast.rearrange("p c o -> p (c o)"), func=AF.Exp),
                           reads=[gAH[hf]], writes=[dec[d]], partial=True)
                        op("act", lambda e: e.activation(out=eAH[hf][:, :], in_=GH[hf][:, :], func=AF.Exp), reads=[GH[hf]],
                           writes=[eAH[hf]])
                    for hf in HF:
                        op("dve", lambda e: e.tensor_tensor(out=qt[d][:, hf * SH:(hf + 1) * SH], in0=qTs[:, hf * SH:(hf + 1) * SH],
                                                            in1=eAH[hf][:, :], op=ALU.mult), reads=[qTs, eAH[hf]], writes=[qt[d]],
                           partial=True)
                    for hf in HF:
                        op("act", lambda e: e.activation(out=eAH[hf][:, :], in_=GH[hf][:, :], func=AF.Exp, scale=-1.0),
                           reads=[GH[hf]], writes=[eAH[hf]])
                    for hf in HF:
                        op("pool", lambda e: e.tensor_tensor(out=kt[d][:, hf * SH:(hf + 1) * SH], in0=kkH[hf][:, :],
                                                             in1=eAH[hf][:, :], op=ALU.mult), reads=[kkH[hf], eAH[hf]],
                           writes=[kt[d]], partial=True)
                        glast = v3(gAH[hf])[:, :, CH - 1:CH]
                        op("dve", lambda e: e.tensor_tensor(out=v3(fAH[hf]), in0=glast.broadcast_to([128, NCH2, CH]),
                                                            in1=v3(GH[hf]), op=ALU.subtract), reads=[gAH[hf], GH[hf]],
                           writes=[fAH[hf]])
                    for hf in HF:
                        op("act", lambda e: e.activation(out=fAH[hf][:, :], in_=fAH[hf][:, :], func=AF.Exp), reads=[fAH[hf]],
                           writes=[fAH[hf]])
                    for hf in HF:
                        op("dve", lambda e: e.tensor_tensor(out=kdT[:, hf * SH:(hf + 1) * SH], in0=kkH[hf][:, :], in1=fAH[hf][:, :],
                                                            op=ALU.mult), reads=[kkH[hf], fAH[hf]], writes=[kdT], partial=True)
                    for c8 in range(0, NCH, 8):
                        pa, pb = bank()
                        pbf = pa.bitcast(BF16)
                        for cc in range(8):
                            c = c8 + cc
                            op("pe", lambda e, c=c, cc=cc: e.transpose(out=pbf[0:CH, cc * 128:(cc + 1) * 128],
                                                                       in_=kdT[:, c * CH:(c + 1) * CH], identity=ident_b[:, :]),
                               reads=[kdT, ident_b], writes=[pb], signal=(cc == 7))
                        op("act", lambda e: e.copy(out=kdt[d][:, c8:c8 + 8, :],
                                                   in_=pbf[0:CH, :].rearrange("p (c k) -> p c k", c=8)),
                           reads=[pb], writes=[kdt[d]], partial=True)
                K.barrier()
                pst.close()
                rst = ExitStack()
                oacc = [sb(rst, "oacc%d" % d, [CH, NCH, 128], F32) for d in range(2)]
                osq = sb(rst, "osq", [CH, NCH, 128], BF16)
                oss = sb(rst, "oss", [CH, NCH], F32)
                ors = sb(rst, "ors", [CH, NCH], F32)
                ort = sb(rst, "ort", [CH, NCH], F32)
                ohg = sb(rst, "ohg", [CH, NCH, 128], BF16)
                for d in range(2):
                    op("dve", lambda e, d=d: e.memset(S32[d][:, :], 0.0), writes=[S32[d]])
                    op("pool", lambda e, d=d: e.memset(Sbf[d][:, :], 0.0), writes=[Sbf[d]])
                for step in range(NCH):
                    for d in range(2):
                        c = step if d == 0 else NCH - 1 - step
                        cs_ = slice(c * CH, (c + 1) * CH)
                        pt_ = PT[d][step % 2]
                        pa, pb = bank()
                        op("pe", lambda e: e.matmul(pa[0:CH, 0:CH], lhsT=kt[d][:, cs_], rhs=qt[d][:, cs_], start=True, stop=True),
                           reads=[kt[d], qt[d]], writes=[pb])
                        mk = maskf if d == 0 else maskb
                        op("dve", lambda e: e.tensor_tensor(out=pt_[:, :], in0=pa[0:CH, 0:CH], in1=mk[0:CH, 0:CH], op=ALU.mult),
                           reads=[pb, mk], writes=[pt_])
                        pa2, pb2 = bank()
                        op("pe", lambda e: e.matmul(pa2[0:CH, 0:128], lhsT=qt[d][:, cs_], rhs=Sbf[d][:, :], start=True, stop=False),
                           reads=[qt[d], Sbf[d]], writes=[pb2], signal=False)
                        op("pe", lambda e: e.matmul(pa2[0:CH, 0:128], lhsT=pt_[:, :], rhs=vtk[:, c, :], start=False, stop=True),
                           reads=[pt_, vtk], writes=[pb2])
                        op("act", lambda e: e.copy(out=oacc[d][:, c, :], in_=pa2[0:CH, 0:128]), reads=[pb2], writes=[oacc[d]],
                           partial=True)
                        pa3, pb3 = bank()
                        op("pe", lambda e: e.matmul(pa3[:, 0:128], lhsT=kdt[d][:, c, :], rhs=vtk[:, c, :], start=True, stop=True),
                           reads=[kdt[d], vtk], writes=[pb3])
                        op("dve", lambda e: e.scalar_tensor_tensor(out=S32[d][:, :], in0=S32[d][:, :], scalar=dec[d][:, c:c + 1],
                                                                   in1=pa3[:, 0:128], op0=ALU.mult, op1=ALU.add),
                           reads=[S32[d], dec[d], pb3], writes=[S32[d]])
                        op("pool", lambda e: e.tensor_copy(out=Sbf[d][:, :], in_=S32[d][:, :]), reads=[S32[d]], writes=[Sbf[d]])
                op("dve", lambda e: e.tensor_tensor(out=oacc[0][:], in0=oacc[0][:], in1=oacc[1][:], op=ALU.add),
                   reads=[oacc[0], oacc[1]], writes=[oacc[0]])
                op("act", lambda e: e.activation(out=osq[:], in_=oacc[0][:], func=AF.Square), reads=[oacc[0]], writes=[osq])
                op("dve", lambda e: e.tensor_reduce(out=oss[:, :], in_=osq[:], axis=AX.X, op=ALU.add), reads=[osq], writes=[oss])
                rstd_from_ssq(oss[:, :], ort, ors[:, :], 128, [oss, ors])
                op("dve", lambda e: e.tensor_tensor(out=oacc[0][:], in0=oacc[0][:],
                                                    in1=ors[:, :].unsqueeze(2).broadcast_to([CH, NCH, 128]), op=ALU.mult),
                   reads=[oacc[0], ors], writes=[oacc[0]])
                op("pool", lambda e: e.tensor_tensor(out=oacc[0][:], in0=oacc[0][:],
                                                     in1=g_on[0:CH, :].unsqueeze(1).broadcast_to([CH, NCH, 128]), op=ALU.mult),
                   reads=[oacc[0], g_on], writes=[oacc[0]])
                op("dve", lambda e: e.tensor_tensor(out=ohg[:], in0=oacc[0][:], in1=ogt[:], op=ALU.mult),
                   reads=[oacc[0], ogt], writes=[ohg])
                for c8 in range(0, NCH, 8):
                    pa, pb = bank()
                    pbf = pa.bitcast(BF16)
                    for cc in range(8):
                        c = c8 + cc
                        op("pe", lambda e, c=c, cc=cc: e.transpose(out=pbf[:, cc * CH:(cc + 1) * CH], in_=ohg[:, c, :],
                                                                   identity=ident_b[0:CH, 0:CH]),
                           reads=[ohg, ident_b], writes=[pb], signal=(cc == 7))
                    op("act", lambda e: e.copy(out=oaT[:, h, c8 * CH:(c8 + 8) * CH], in_=pbf[:, 0:8 * CH]), reads=[pb],
                       writes=[oaT], partial=True)
                K.barrier()
                rst.close()

        if upto == "B":
            seqst.close()
            root.close()
            return nc
        with ExitStack() as st:
            QT = [sb(st, "QT%d" % i, [96, S], BF16) for i in range(2)]
            KT = [sb(st, "KT%d" % i, [96, S], BF16) for i in range(2)]
            VV = [sb(st, "VV%d" % i, [128, NTS, 65], BF16) for i in range(2)]
            for i in range(2):
                op("dve", lambda e, i=i: e.memset(VV[i][:, :, 64:65], 1.0), writes=[VV[i]], partial=True)
            G_ = 4
            NPT = 2 * G_ + 1
            PTs = [sb(st, "PTs%d" % i, [128, 512], BF16) for i in range(NPT)]
            Osb = [sb(st, "Osb%d" % i, [65, 512], F32) for i in range(2)]
            rden = [sb(st, "rden%d" % i, [64, 512], F32) for i in range(2)]
            scale = float(96 ** -0.5)
            reserved.update((0, 1))
            tiles = list(range(seq * NTS, (seq + 1) * NTS))
            NQG = S // 512
            items = [(h, qg, kt_) for h in range(8) for qg in range(NQG) for kt_ in range(NTS)]
            LA = G_

            def c_load(h):
                Q_, K_, V_ = QT[h % 2], KT[h % 2], VV[h % 2]
                dma("sp", Q_[:, :], qT_d[seq, :, h, :], reads=[K.db("qkT", 0, i) for i in tiles], writes=[Q_])
                dma("sp", K_[:, :], kT_d[seq, :, h, :], reads=[K.db("qkT", 1, i) for i in tiles], writes=[K_])
                dma("sp", V_[:, :, 0:64], vm_d[seq * S:(seq + 1) * S, h, :].rearrange("(n p) d -> p n d", p=128),
                    reads=[K.db("vm", i) for i in tiles], writes=[V_], partial=True)

            def c_qk(ii):
                h, qg, kt_ = items[ii]
                Q_, K_ = QT[h % 2], KT[h % 2]
                pa, pb = bank()
                op("pe", lambda e: e.matmul(pa, lhsT=K_[:, kt_ * 128:(kt_ + 1) * 128], rhs=Q_[:, qg * 512:(qg + 1) * 512],
                                            start=True, stop=True), reads=[K_, Q_], writes=[pb])
                p_ = PTs[ii % NPT]
                op("act", lambda e: e.activation(out=p_[:, :], in_=pa, func=AF.Exp, scale=scale), reads=[pb], writes=[p_])

            epi = []

            def c_pv(ii):
                h, qg, kt_ = items[ii]
                V_ = VV[h % 2]
                gi = h * NQG + qg
                po, pob = fixed_bank(gi % 2)
                p_ = PTs[ii % NPT]
                op("pe", lambda e: e.matmul(po[0:65, :], lhsT=V_[:, kt_, :], rhs=p_[:, :], start=(kt_ == 0),
                                            stop=(kt_ == NTS - 1)), reads=[V_, p_], writes=[pob], signal=(kt_ == NTS - 1))
                if kt_ == NTS - 1:
                    o_ = Osb[gi % 2]
                    op("dve", lambda e: e.tensor_copy(out=o_[:, :], in_=po[0:65, :]), reads=[pob], writes=[o_])
                    epi.append((ii, h, qg, gi))

            def c_epi(h, qg, gi):
                o_ = Osb[gi % 2]
                r_ = rden[gi % 2]
                pd, pdb = bank()
                op("pe", lambda e: e.matmul(pd[0:64, :], lhsT=sel[0:65, :], rhs=o_[:, :], start=True, stop=True),
                   reads=[sel, o_], writes=[pdb])
                op("dve", lambda e: e.reciprocal(out=r_[:, :], in_=pd[0:64, :]), reads=[pdb], writes=[r_])
                op("dve", lambda e: e.tensor_tensor(out=obT[:, h, qg * 512:(qg + 1) * 512], in0=o_[0:64, :], in1=r_[:, :],
                                                    op=ALU.mult), reads=[o_, r_], writes=[obT], partial=True)

            n_it = len(items)
            assert n_it % G_ == 0
            c_load(0)
            c_load(1)
            ngrp = n_it // G_
            for g in range(ngrp + 2):
                if g < ngrp:
                    for ii in range(g * G_, (g + 1) * G_):
                        c_qk(ii)
                while epi and epi[0][0] < (g - 1) * G_:
                    _, h_, qg_, gi_ = epi.pop(0)
                    c_epi(h_, qg_, gi_)
                if 1 <= g <= ngrp:
                    for ii in range((g - 1) * G_, g * G_):
                        c_pv(ii)
                    hl, qgl, ktl = items[g * G_ - 1]
                    if qgl == NQG - 1 and ktl == NTS - 1 and hl + 2 < 8:
                        c_load(hl + 2)
            assert not epi
            reserved.clear()
            K.barrier()
        if upto == "C":
            seqst.close()
            root.close()
            return nc

        with ExitStack() as st:
            w_oA = sb(st, "w_oA", [128, 4, D], BF16)
            w_oB = sb(st, "w_oB", [64, 8, D], BF16)
            w_out = sb(st, "w_out", [128, 8, D], BF16)
            w_rt = sb(st, "w_rt", [128, 8, 72], F32)
            wst_cur[0] = [sb(st, "wstD%d" % i, [128, 1024], F32) for i in range(3)]
            for kc in range(4):
                load_cast(w_oA[:, kc, :], w_oA, w_oA_d[kc * 128:(kc + 1) * 128, :], 1024)
            for h in range(8):
                load_cast(w_oB[:, h, :], w_oB, w_oB_d[h * 64:(h + 1) * 64, :], 1024, npart=64)
            for kc in range(8):
                load_cast(w_out[:, kc, :], w_out, w_out_d[kc * 128:(kc + 1) * 128, :], 1024)
            dma("sp", w_rt[:, :, 0:8], w_rg_d.rearrange("(k p) g -> p k g", p=128), writes=[w_rt], partial=True)
            dma("sp", w_rt[:, :, 8:72], w_re_d.rearrange("(k p) g -> p k g", p=128), writes=[w_rt], partial=True)
            g_moe = bc_load(st, "g_moe", ln_moe_d, D)
            b_rt = sb(st, "b_rt", [128, 72], F32)
            dma("sp", b_rt[:, 0:8], b_rg_d.partition_broadcast(128), writes=[b_rt], partial=True)
            dma("sp", b_rt[:, 8:72], b_re_d.partition_broadcast(128), writes=[b_rt], partial=True)
            hmbt = [sb(st, "hmbt%d" % i, [128, D], BF16) for i in range(2)]
            sg = [sb(st, "sg%d" % i, [128, 2048], BF16) for i in range(2)]
            xin = [sb(st, "xin%d" % i, [128, D], F32) for i in range(2)]
            ta_l = [sb(st, "ta%d" % _i, [128, D], F32) for _i in range(2)]
            tb_l = [sb(st, "tb%d" % _i, [128, D], F32) for _i in range(2)]
            mg_l = [sb(st, "mg%d" % _i, [128, D], BF16) for _i in range(2)]
            mT_l = [sb(st, "mT%d" % _i, [128, 8, 128], BF16) for _i in range(2)]
            x1t = [sb(st, "x1t%d" % i, [128, D], F32) for i in range(2)]
            sq_l = [sb(st, "sqD%d" % _i, [128, D], BF16) for _i in range(2)]
            ssq_l = [sb(st, "ssqD%d" % _i, [128, 4], F32) for _i in range(2)]
            rt_l = [sb(st, "rtD%d" % _i, [128, 4], F32) for _i in range(2)]
            rs_l = [sb(st, "rsD%d" % _i, [128, 4], F32) for _i in range(2)]
            hmf_l = [sb(st, "hmf%d" % _i, [128, D], F32) for _i in range(2)]
            hmT_l = [sb(st, "hmT%d" % _i, [128, 8, 128], F32) for _i in range(2)]
            lgt_l = [sb(st, "lgt%d" % _i, [128, 72], F32) for _i in range(2)]
            r8_l = [sb(st, "r8%d" % _i, [128, 8], F32) for _i in range(2)]
            gmx_l = [sb(st, "gmx%d" % _i, [128, 8], F32) for _i in range(2)]
            goh_l = [sb(st, "goh%d" % _i, [128, 8], F32) for _i in range(2)]
            gex_l = [sb(st, "gex%d" % _i, [128, 8], F32) for _i in range(2)]
            gsum_l = [sb(st, "gsum%d" % _i, [128, 2], F32) for _i in range(2)]
            pgrp_l = [sb(st, "pgrp%d" % _i, [128, 2], F32) for _i in range(2)]
            eml_l = [sb(st, "eml%d" % _i, [128, 64], F32) for _i in range(2)]
            pen_l = [sb(st, "pen%d" % _i, [128, 8], F32) for _i in range(2)]
            top8_l = [sb(st, "top8%d" % _i, [128, 8], F32) for _i in range(2)]
            dv_l = [sb(st, "dv%d" % _i, [128, 2], F32) for _i in range(2)]
            def d_tile(jt):
                ta = ta_l[(seq * NTS + jt) % 2]
                tb = tb_l[(seq * NTS + jt) % 2]
                mg = mg_l[(seq * NTS + jt) % 2]
                mT = mT_l[(seq * NTS + jt) % 2]
                sq = sq_l[(seq * NTS + jt) % 2]
                ssq = ssq_l[(seq * NTS + jt) % 2]
                rt = rt_l[(seq * NTS + jt) % 2]
                rs = rs_l[(seq * NTS + jt) % 2]
                hmf = hmf_l[(seq * NTS + jt) % 2]
                hmT = hmT_l[(seq * NTS + jt) % 2]
                lgt = lgt_l[(seq * NTS + jt) % 2]
                r8 = r8_l[(seq * NTS + jt) % 2]
                gmx = gmx_l[(seq * NTS + jt) % 2]
                goh = goh_l[(seq * NTS + jt) % 2]
                gex = gex_l[(seq * NTS + jt) % 2]
                gsum = gsum_l[(seq * NTS + jt) % 2]
                pgrp = pgrp_l[(seq * NTS + jt) % 2]
                eml = eml_l[(seq * NTS + jt) % 2]
                pen = pen_l[(seq * NTS + jt) % 2]
                top8 = top8_l[(seq * NTS + jt) % 2]
                dv = dv_l[(seq * NTS + jt) % 2]
                i = seq * NTS + jt
                tsl = slice(jt * 128, (jt + 1) * 128)
                s_ = sg[i % 2]
                x_ = xin[i % 2]
                x1_ = x1t[i % 2]
                dma("sp", s_[:, :], zg_d[i * 128:(i + 1) * 128, :], reads=[K.db("zg", i, gi) for gi in range(4)], writes=[s_])
                dma("sp", x_[:, :], x_d[seq, tsl, :], writes=[x_])
                yield
                ya = [bank(), bank()]
                for hf in range(2):
                    for hh in range(4):
                        op("pe", lambda e: e.matmul(ya[hf][0], lhsT=oaT[:, hh, tsl], rhs=w_oA[:, hh, hf * 512:(hf + 1) * 512],
                                                    start=(hh == 0), stop=(hh == 3)), reads=[oaT, w_oA], writes=[ya[hf][1]],
                           signal=(hh == 3))
                    op("dve", lambda e: e.tensor_tensor(out=ta[:, hf * 512:(hf + 1) * 512], in0=ya[hf][0],
                                                        in1=s_[:, hf * 512:(hf + 1) * 512], op=ALU.mult),
                       reads=[ya[hf][1], s_], writes=[ta], partial=True)
                yb = [bank(), bank()]
                for hf in range(2):
                    for hh in range(8):
                        op("pe", lambda e: e.matmul(yb[hf][0], lhsT=obT[:, hh, tsl], rhs=w_oB[:, hh, hf * 512:(hf + 1) * 512],
                                                    start=(hh == 0), stop=(hh == 7)), reads=[obT, w_oB], writes=[yb[hf][1]],
                           signal=(hh == 7))
                    op("dve", lambda e: e.tensor_tensor(out=tb[:, hf * 512:(hf + 1) * 512], in0=yb[hf][0],
                                                        in1=s_[:, 1024 + hf * 512:1024 + (hf + 1) * 512], op=ALU.mult),
                       reads=[yb[hf][1], s_], writes=[tb], partial=True)
                yield
                op("pool", lambda e: e.tensor_tensor(out=mg[:, :], in0=ta[:, :], in1=tb[:, :], op=ALU.add), reads=[ta, tb],
                   writes=[mg])
                pa, pb = bank()
                pbf = pa.bitcast(BF16)
                for kc in range(8):
                    op("pe", lambda e, kc=kc: e.transpose(out=pbf[:, kc * 128:(kc + 1) * 128], in_=mg[:, kc * 128:(kc + 1) * 128],
                                                          identity=ident_b[:, :]), reads=[mg, ident_b], writes=[pb],
                       signal=(kc == 7))
                op("act", lambda e: e.copy(out=mT[:, :, :], in_=pbf.rearrange("p (k t) -> p k t", k=8)), reads=[pb], writes=[mT])
                yield
                for hf in range(2):
                    pa, pb = bank()
                    for kc in range(8):
                        op("pe", lambda e, kc=kc: e.matmul(pa, lhsT=mT[:, kc, :], rhs=w_out[:, kc, hf * 512:(hf + 1) * 512],
                                                           start=(kc == 0), stop=(kc == 7)), reads=[mT, w_out], writes=[pb],
                           signal=(kc == 7))
                    op("dve", lambda e: e.tensor_tensor(out=x1_[:, hf * 512:(hf + 1) * 512], in0=pa,
                                                        in1=x_[:, hf * 512:(hf + 1) * 512], op=ALU.add), reads=[pb, x_],
                       writes=[x1_], partial=True)
                dma("sp", x1_d[i * 128:(i + 1) * 128, :], x1_[:, :], reads=[x1_], writes=[K.db("x1", i)])
                yield
                op("act", lambda e: e.activation(out=sq[:, :], in_=x1_[:, :], func=AF.Square), reads=[x1_], writes=[sq])
                op("dve", lambda e: e.tensor_reduce(out=ssq[:, 0:1], in_=sq[:, :], axis=AX.X, op=ALU.add), reads=[sq], writes=[ssq])
                rstd_from_ssq(ssq[:, 0:1], rt, rs[:, 0:1], D, [ssq, rs])
                op("dve", lambda e: e.scalar_tensor_tensor(out=hmf[:, :], in0=x1_[:, :], scalar=rs[:, 0:1], in1=g_moe[:, :],
                                                           op0=ALU.mult, op1=ALU.mult), reads=[x1_, rs, g_moe], writes=[hmf])
                hb_ = hmbt[i % 2]
                op("pool", lambda e: e.tensor_copy(out=hb_[:, :], in_=hmf[:, :]), reads=[hmf], writes=[hb_])
                dma("sp", hmb_d[i * 128:(i + 1) * 128, :], hb_[:, :], reads=[hb_], writes=[K.db("hmb", i)])
                yield
                for half in range(2):
                    pa, pb = bank()
                    for kc4 in range(4):
                        kc = half * 4 + kc4
                        op("pe", lambda e, kc=kc, kc4=kc4: e.transpose(out=pa[:, kc4 * 128:(kc4 + 1) * 128],
                                                                       in_=hmf[:, kc * 128:(kc + 1) * 128], identity=ident_f[:, :]),
                           reads=[hmf, ident_f], writes=[pb], signal=(kc4 == 3))
                    op("act", lambda e: e.copy(out=hmT[:, half * 4:(half + 1) * 4, :],
                                               in_=pa.rearrange("p (k t) -> p k t", k=4)), reads=[pb], writes=[hmT], partial=True)
                yield
                pa, pb = bank()
                for kc in range(8):
                    op("pe", lambda e, kc=kc: e.matmul(pa[:, 0:72], lhsT=hmT[:, kc, :], rhs=w_rt[:, kc, :], start=(kc == 0),
                                                       stop=(kc == 7)), reads=[hmT, w_rt], writes=[pb], signal=(kc == 7))
                op("dve", lambda e: e.tensor_tensor(out=lgt[:, :], in0=pa[:, 0:72], in1=b_rt[:, :], op=ALU.add), reads=[pb, b_rt],
                   writes=[lgt])
                op("dve", lambda e: e.max(out=gmx[:, :], in_=lgt[:, 0:8]), reads=[lgt], writes=[gmx])
                op("dve", lambda e: e.tensor_scalar(out=goh[:, :], in0=lgt[:, 0:8], scalar1=gmx[:, 0:1], scalar2=None,
                                                    op0=ALU.is_equal), reads=[lgt, gmx], writes=[goh])
                op("dve", lambda e: e.tensor_scalar(out=gex[:, :], in0=lgt[:, 0:8], scalar1=gmx[:, 0:1], scalar2=None,
                                                    op0=ALU.subtract), reads=[lgt, gmx], writes=[gex])
                op("act", lambda e: e.activation(out=gex[:, :], in_=gex[:, :], func=AF.Exp), reads=[gex], writes=[gex])
                op("dve", lambda e: e.tensor_reduce(out=gsum[:, 0:1], in_=gex[:, :], axis=AX.X, op=ALU.add), reads=[gex],
                   writes=[gsum])
                op("dve", lambda e: e.reciprocal(out=pgrp[:, 0:1], in_=gsum[:, 0:1]), reads=[gsum], writes=[pgrp])
                yield
                op("dve", lambda e: e.tensor_scalar(out=pen[:, :], in0=goh[:, :], scalar1=1.0e30, scalar2=-1.0e30, op0=ALU.mult,
                                                    op1=ALU.add), reads=[goh], writes=[pen])
                op("dve", lambda e: e.tensor_tensor(out=eml[:, :].rearrange("p (g j) -> p g j", g=8),
                                                    in0=lgt[:, 8:72].rearrange("p (g j) -> p g j", g=8),
                                                    in1=pen[:, :].unsqueeze(2).broadcast_to([128, 8, 8]), op=ALU.add),
                   reads=[lgt, pen], writes=[eml])
                op("dve", lambda e: e.max(out=top8[:, :], in_=eml[:, :]), reads=[eml], writes=[top8])
                for j2 in range(2):
                    op("dve", lambda e, j2=j2: e.tensor_scalar(out=Moh[:, i, j2, :], in0=eml[:, :], scalar1=top8[:, j2:j2 + 1],
                                                               scalar2=None, op0=ALU.is_equal), reads=[eml, top8],
                       writes=[Moh], partial=True)
                op("dve", lambda e: e.tensor_tensor(out=dv[:, 0:1], in0=top8[:, 0:1], in1=top8[:, 1:2], op=ALU.subtract),
                   reads=[top8], writes=[dv])
                op("dve", lambda e: e.tensor_tensor(out=dv[:, 1:2], in0=top8[:, 1:2], in1=top8[:, 0:1], op=ALU.subtract),
                   reads=[top8], writes=[dv])
                op("act", lambda e: e.activation(out=dv[:, :], in_=dv[:, :], func=AF.Sigmoid), reads=[dv], writes=[dv])
                op("dve", lambda e: e.tensor_scalar(out=wts[:, i, :], in0=dv[:, :], scalar1=pgrp[:, 0:1], scalar2=None,
                                                    op0=ALU.mult), reads=[dv, pgrp], writes=[wts], partial=True)
                yield
            pipeline([(lambda jt=jt: d_tile(jt)) for jt in range(NTS)], 2)
            K.barrier()
        seqst.close()
    if upto == "D":
        root.close()
        return nc

    with ExitStack() as st:
        Msum = sb(st, "Msum", [128, NT, 64], BF16)
        eoff_i = sb(st, "eoff_i", [128, 64], I32)
        eoff = sb(st, "eoff", [128, 64], F32)
        crk = sb(st, "crk", [128, 64], F32)
        junk = sb(st, "junk", [128, 64], F32)
        dest_f = sb(st, "dest_f", [128, NT, 2], F32)
        op("pool", lambda e: e.iota(eoff_i[:], pattern=[[CAP, 64]], base=0, channel_multiplier=0), writes=[eoff_i])
        op("dve", lambda e: e.tensor_copy(out=eoff[:], in_=eoff_i[:]), reads=[eoff_i], writes=[eoff])
        op("dve", lambda e: e.tensor_tensor(out=Msum[:], in0=Moh[:, :, 0, :], in1=Moh[:, :, 1, :], op=ALU.add), reads=[Moh],
           writes=[Msum])
        for i in range(NT):
            pa, pb = bank()
            for i2 in range(i + 1):
                lt = lstrict if i2 == i else ones_b
                op("pe", lambda e, i2=i2, lt=lt: e.matmul(pa[:, 0:64], lhsT=lt[:, :], rhs=Msum[:, i2, :], start=(i2 == 0),
                                                          stop=(i2 == i)), reads=[lt, Msum], writes=[pb], signal=(i2 == i))
            op("dve", lambda e: e.tensor_scalar(out=crk[:, :], in0=pa[:, 0:64], scalar1=float(CAP - 1), scalar2=None,
                                                op0=ALU.min), reads=[pb], writes=[crk])
            op("dve", lambda e: e.tensor_tensor(out=crk[:, :], in0=crk[:, :], in1=eoff[:, :], op=ALU.add), reads=[crk, eoff],
               writes=[crk])
            for j2 in range(2):
                op("dve", lambda e, j2=j2: e.tensor_tensor(out=junk[:, :], in0=crk[:, :], in1=Moh[:, i, j2, :], op=ALU.mult),
                   reads=[crk, Moh], writes=[junk])
                op("dve", lambda e, j2=j2: e.tensor_reduce(out=dest_f[:, i, j2:j2 + 1], in_=junk[:, :], axis=AX.X, op=ALU.add),
                   reads=[junk], writes=[dest_f], partial=True)
        op("dve", lambda e: e.tensor_copy(out=dest_i[:], in_=dest_f[:]), reads=[dest_f], writes=[dest_i])
        hst = [sb(st, "hst%d" % i, [128, D], BF16) for i in range(3)]
        for i in range(NT):
            hmb = hst[i % 3]
            dma("sp", hmb[:, :], hmb_d[i * 128:(i + 1) * 128, :], reads=[K.db("hmb", i)], writes=[hmb])
            for j2 in range(2):
                dma("pool", xs_d[:, :], hmb[:, :], reads=[hmb, dest_i], writes=[K.db("xs")], partial=True,
                    indirect=dict(out_offset=bass.IndirectOffsetOnAxis(ap=dest_i[:, i, j2:j2 + 1], axis=0), in_offset=None))
        K.barrier()

    with ExitStack() as st:
        NB = CAP // 128
        ws13 = [sb(st, "ws13_%d" % i, [128, 8, 512], F32) for i in range(3)]
        ws2 = [sb(st, "ws2_%d" % i, [128, 2, D], F32) for i in range(3)]
        wb13 = [sb(st, "wb13_%d" % i, [128, 8, 512], BF16) for i in range(2)]
        wb2 = [sb(st, "wb2_%d" % i, [128, 2, D], BF16) for i in range(2)]
        xsb = [sb(st, "xsb%d" % i, [128, NB, D], BF16) for i in range(3)]
        xsT = [sb(st, "xsT%d" % i, [128, 8, CAP], BF16) for i in range(2)]
        sl = [sb(st, "sl%d" % i, [128, 2, CAP], F32) for i in range(2)]
        hh_ = [sb(st, "hh%d" % i, [128, 2, CAP], BF16) for i in range(2)]
        ysb = [sb(st, "ysb%d" % i, [128, D], F32) for i in range(4)]
        yi = [0]

        def e_load(ex):
            a13, a2 = ws13[ex % 3], ws2[ex % 3]
            dma("sp", a13[:, :, 0:256], w1_d[ex].rearrange("(p k) f -> p k f", k=8), writes=[a13], partial=True)
            dma("sp", a13[:, :, 256:512], w3_d[ex].rearrange("(p k) f -> p k f", k=8), writes=[a13], partial=True)
            dma("act", a2[:, :, :], w2_d[ex].rearrange("(c p) d -> p c d", p=128), writes=[a2])
            xb = xsb[ex % 3]
            dma("act", xb[:, :, :], xs_d[ex * CAP:(ex + 1) * CAP, :].rearrange("(b p) d -> p b d", p=128),
                reads=[K.db("xs")], writes=[xb])

        def e_cast(ex):
            a13, a2, b13, b2 = ws13[ex % 3], ws2[ex % 3], wb13[ex % 2], wb2[ex % 2]
            op("dve", lambda e: e.tensor_copy(out=b13[:, 0:3, :], in_=a13[:, 0:3, :]), reads=[a13], writes=[b13], partial=True)
            op("act", lambda e: e.copy(out=b13[:, 3:6, :], in_=a13[:, 3:6, :]), reads=[a13], writes=[b13], partial=True)
            op("pool", lambda e: e.tensor_copy(out=b13[:, 6:8, :], in_=a13[:, 6:8, :]), reads=[a13], writes=[b13], partial=True)
            op("dve", lambda e: e.tensor_copy(out=b2[:, 0:1, :], in_=a2[:, 0:1, :]), reads=[a2], writes=[b2], partial=True)
            op("act", lambda e: e.copy(out=b2[:, 1:2, :], in_=a2[:, 1:2, :]), reads=[a2], writes=[b2], partial=True)

        def e_compute(ex):
            b13, b2 = wb13[ex % 2], wb2[ex % 2]
            xb, xT, hb, sl_ = xsb[ex % 3], xsT[ex % 2], hh_[ex % 2], sl[ex % 2]
            for b_ in range(NB):
                pa, pb = bank()
                pbf = pa.bitcast(BF16)
                xv = xb[:, b_, :].rearrange("p (q k) -> p k q", k=8)
                for kc in range(8):
                    op("pe", lambda e, kc=kc: e.transpose(out=pbf[:, kc * 128:(kc + 1) * 128], in_=xv[:, kc, :],
                                                          identity=ident_b[:, :]),
                       reads=[xb, ident_b], writes=[pb], signal=(kc == 7))
                if b_ % 2 == 0:
                    op("act", lambda e: e.copy(out=xT[:, :, b_ * 128:(b_ + 1) * 128], in_=pbf.rearrange("p (k t) -> p k t", k=8)),
                       reads=[pb], writes=[xT], partial=True)
                else:
                    op("dve", lambda e: e.tensor_copy(out=xT[:, :, b_ * 128:(b_ + 1) * 128],
                                                      in_=pbf.rearrange("p (k t) -> p k t", k=8)),
                       reads=[pb], writes=[xT], partial=True)
            ups = []
            for u in range(4):
                pa, pb = bank()
                for kc in range(8):
                    op("pe", lambda e, kc=kc: e.matmul(pa[:, 0:CAP], lhsT=b13[:, kc, u * 128:(u + 1) * 128], rhs=xT[:, kc, :],
                                                       start=(kc == 0), stop=(kc == 7)), reads=[b13, xT], writes=[pb],
                       signal=(kc == 7))
                ups.append((pa, pb))
            for c in range(2):
                op("act", lambda e: e.activation(out=sl_[:, c, :], in_=ups[c][0][:, 0:CAP], func=AF.Silu), reads=[ups[c][1]],
                   writes=[sl_], partial=True)
                op("dve", lambda e: e.tensor_tensor(out=hb[:, c, :], in0=ups[2 + c][0][:, 0:CAP], in1=sl_[:, c, :], op=ALU.mult),
                   reads=[ups[2 + c][1], sl_], writes=[hb], partial=True)
            for b_ in range(NB):
                y_ = ysb[yi[0] % 4]
                yi[0] += 1
                for hf in range(2):
                    pa, pb = bank()
                    for c in range(2):
                        op("pe", lambda e, c=c: e.matmul(pa, lhsT=hb[:, c, b_ * 128:(b_ + 1) * 128],
                                                         rhs=b2[:, c, hf * 512:(hf + 1) * 512], start=(c == 0), stop=(c == 1)),
                           reads=[hb, b2], writes=[pb], signal=(c == 1))
                    if hf == 0:
                        op("act", lambda e: e.copy(out=y_[:, 0:512], in_=pa), reads=[pb], writes=[y_], partial=True)
                    else:
                        op("dve", lambda e: e.tensor_copy(out=y_[:, 512:1024], in_=pa), reads=[pb], writes=[y_], partial=True)
                s0 = ex * CAP + b_ * 128
                dma("pool", ys_d[s0:s0 + 128, :], y_[:, :], reads=[y_], writes=[K.db("ys")], partial=True)

        e_load(0)
        e_load(1)
        e_cast(0)
        for ex in range(NEXP):
            if ex + 2 < NEXP:
                e_load(ex + 2)
            if ex + 1 < NEXP:
                e_cast(ex + 1)
            e_compute(ex)
        K.barrier()
    if upto == "E":
        root.close()
        return nc
    with ExitStack() as st:
        wstage2 = [sb(st, "wstF%d" % i, [128, 1024], F32) for i in range(3)]
        w_pg = sb(st, "w_pg", [128, 8, D], BF16)
        w_pp = sb(st, "w_pp", [128, 2, D], BF16)
        g_ple = bc_load(st, "g_ple", ln_ple_d, D)
        for kc in range(10):
            stg = wstage2[kc % 2]
            src = w_pg_d[kc * 128:(kc + 1) * 128, :] if kc < 8 else w_pp_d[(kc - 8) * 128:(kc - 7) * 128, :]
            dstT = w_pg if kc < 8 else w_pp
            dst = w_pg[:, kc, :] if kc < 8 else w_pp[:, kc - 8, :]
            dma("sp", stg[:, :], src, writes=[stg])
            op("dve" if kc % 2 == 0 else "pool", lambda e, dst=dst, stg=stg: e.tensor_copy(out=dst, in_=stg[:, :]), reads=[stg],
               writes=[dstT], partial=True)
        x1t = [sb(st, "x1F%d" % i, [128, D], F32) for i in range(3)]
        y1 = [sb(st, "y1F%d" % i, [128, D], F32) for i in range(3)]
        y2 = [sb(st, "y2F%d" % i, [128, D], F32) for i in range(3)]
        pt_ = [sb(st, "ptF%d" % i, [128, 256], F32) for i in range(3)]
        ptb_l = [sb(st, "ptb%d" % _i, [128, 256], BF16) for _i in range(3)]
        pT_l = [sb(st, "pT%d" % _i, [128, 2, 128], BF16) for _i in range(3)]
        sq_l = [sb(st, "sqF%d" % _i, [128, D], BF16) for _i in range(3)]
        ssq_l = [sb(st, "ssqF%d" % _i, [128, 4], F32) for _i in range(3)]
        rt_l = [sb(st, "rtF%d" % _i, [128, 4], F32) for _i in range(3)]
        rs_l = [sb(st, "rsF%d" % _i, [128, 4], F32) for _i in range(3)]
        hnb_l = [sb(st, "hnb%d" % _i, [128, D], BF16) for _i in range(3)]
        hnT_l = [sb(st, "hnT%d" % _i, [128, 8, 128], BF16) for _i in range(3)]
        gsb_l = [sb(st, "gsb%d" % _i, [128, D], F32) for _i in range(3)]
        ot = [sb(st, "otF%d" % i, [128, D], F32) for i in range(3)]
        def f_tile(i):
            ptb = ptb_l[i % 3]
            pT = pT_l[i % 3]
            sq = sq_l[i % 3]
            ssq = ssq_l[i % 3]
            rt = rt_l[i % 3]
            rs = rs_l[i % 3]
            hnb = hnb_l[i % 3]
            hnT = hnT_l[i % 3]
            gsb = gsb_l[i % 3]
            seq = i // NTS
            jt = i % NTS
            x_ = x1t[i % 3]
            o_ = ot[i % 3]
            dma("sp", x_[:, :], x1_d[i * 128:(i + 1) * 128, :], reads=[K.db("x1", i)], writes=[x_])
            dma("sp", pt_[i % 3][:, :], p_d[seq, jt * 128:(jt + 1) * 128, :], writes=[pt_[i % 3]])
            ys_ = [y1[i % 3], y2[i % 3]]
            for j2 in range(2):
                dma("pool", ys_[j2][:, :], ys_d[:, :], reads=[K.db("ys"), dest_i], writes=[ys_[j2]],
                    indirect=dict(out_offset=None, in_offset=bass.IndirectOffsetOnAxis(ap=dest_i[:, i, j2:j2 + 1], axis=0)))
            yield
            for j2 in range(2):
                op("dve", lambda e, j2=j2: e.scalar_tensor_tensor(out=x_[:, :], in0=ys_[j2][:, :], scalar=wts[:, i, j2:j2 + 1],
                                                                  in1=x_[:, :], op0=ALU.mult, op1=ALU.add),
                   reads=[ys_[j2], wts, x_], writes=[x_])
            yield
            op("act", lambda e: e.activation(out=sq[:, :], in_=x_[:, :], func=AF.Square), reads=[x_], writes=[sq])
            op("dve", lambda e: e.tensor_reduce(out=ssq[:, 0:1], in_=sq[:, :], axis=AX.X, op=ALU.add), reads=[sq], writes=[ssq])
            rstd_from_ssq(ssq[:, 0:1], rt, rs[:, 0:1], D, [ssq, rs])
            op("dve", lambda e: e.scalar_tensor_tensor(out=hnb[:, :], in0=x_[:, :], scalar=rs[:, 0:1], in1=g_ple[:, :],
                                                       op0=ALU.mult, op1=ALU.mult), reads=[x_, rs, g_ple], writes=[hnb])
            yield
            pa, pb = bank()
            pbf = pa.bitcast(BF16)
            for kc in range(8):
                op("pe", lambda e, kc=kc: e.transpose(out=pbf[:, kc * 128:(kc + 1) * 128], in_=hnb[:, kc * 128:(kc + 1) * 128],
                                                      identity=ident_b[:, :]), reads=[hnb, ident_b], writes=[pb], signal=(kc == 7))
            op("act", lambda e: e.copy(out=hnT[:, :, :], in_=pbf.rearrange("p (k t) -> p k t", k=8)), reads=[pb], writes=[hnT])
            yield
            op("pool", lambda e: e.tensor_copy(out=ptb[:, :], in_=pt_[i % 3][:, :]), reads=[pt_[i % 3]], writes=[ptb])
            pa, pb = bank()
            pbf = pa.bitcast(BF16)
            for kc in range(2):
                op("pe", lambda e, kc=kc: e.transpose(out=pbf[:, kc * 128:(kc + 1) * 128], in_=ptb[:, kc * 128:(kc + 1) * 128],
                                                      identity=ident_b[:, :]), reads=[ptb, ident_b], writes=[pb], signal=(kc == 1))
            op("act", lambda e: e.copy(out=pT[:, :, :], in_=pbf[:, 0:256].rearrange("p (k t) -> p k t", k=2)), reads=[pb],
               writes=[pT])
            yield
            for hf in range(2):
                pg, pgb = bank()
                for kc in range(8):
                    op("pe", lambda e, kc=kc: e.matmul(pg, lhsT=hnT[:, kc, :], rhs=w_pg[:, kc, hf * 512:(hf + 1) * 512],
                                                       start=(kc == 0), stop=(kc == 7)), reads=[hnT, w_pg], writes=[pgb],
                       signal=(kc == 7))
                op("act", lambda e: e.activation(out=gsb[:, hf * 512:(hf + 1) * 512], in_=pg, func=AF.Sigmoid), reads=[pgb],
                   writes=[gsb], partial=True)
                pp, ppb = bank()
                for kc in range(2):
                    op("pe", lambda e, kc=kc: e.matmul(pp, lhsT=pT[:, kc, :], rhs=w_pp[:, kc, hf * 512:(hf + 1) * 512],
                                                       start=(kc == 0), stop=(kc == 1)), reads=[pT, w_pp], writes=[ppb],
                       signal=(kc == 1))
                op("dve", lambda e: e.tensor_tensor(out=o_[:, hf * 512:(hf + 1) * 512], in0=pp,
                                                    in1=gsb[:, hf * 512:(hf + 1) * 512], op=ALU.mult), reads=[ppb, gsb],
                   writes=[o_], partial=True)
            yield
            op("pool", lambda e: e.tensor_tensor(out=o_[:, :], in0=o_[:, :], in1=x_[:, :], op=ALU.add), reads=[o_, x_],
               writes=[o_])
            dma("sp", out_d[seq, jt * 128:(jt + 1) * 128, :], o_[:, :], reads=[o_], writes=[K.db("out", i)])
            yield
        pipeline([(lambda i=i: f_tile(i)) for i in range(NT)], 3)
        K.barrier()
    root.close()
    return nc


WNAMES = ["ln_mix", "w_in", "hg_lb", "hg_onorm", "w_oA", "mla_qa_norm", "mla_kva_norm", "w_uq", "w_ukv", "q_norm",
          "k_norm", "w_oB", "w_out", "ln_moe", "w_rg", "b_rg", "w_re", "b_re", "w1", "w3", "w2", "ln_ple",
          "w_ple_gate", "w_ple_proj"]


def make_in_maps(inputs, n_cores, NS):
    shared = {}
    for n in WNAMES:
        a = np.ascontiguousarray(np.asarray(inputs[n], dtype=np.float32))
        if n == "hg_lb":
            shared[n] = a
        else:
            shared[n] = a.reshape(a.shape[1:]) if a.shape[0] == 1 else a
    for n in ("ln_mix", "hg_onorm", "mla_qa_norm", "mla_kva_norm", "q_norm", "k_norm", "ln_moe", "b_rg", "b_re", "ln_ple"):
        shared[n] = shared[n].reshape(-1)
    x = np.asarray(inputs["x"], dtype=np.float32)
    p = np.asarray(inputs["p"], dtype=np.float32)[0]
    pos = np.asarray(inputs["positions"], dtype=np.int32)
    maps = []
    for c in range(n_cores):
        m = dict(shared)
        m["x"] = np.ascontiguousarray(x[c * NS:(c + 1) * NS])
        m["p"] = np.ascontiguousarray(p[c * NS:(c + 1) * NS])
        m["positions"] = np.ascontiguousarray(pos[c * NS:(c + 1) * NS])
        maps.append(m)
    return maps


def kernel(**inputs):
    n = 8
    NS = 2
    nc = build(NS=NS, S=2048, CAP=256, debug=True)
    maps = make_in_maps(inputs, n, NS)
    res = run_bass_kernel_spmd(nc, maps, core_ids=list(range(n)))
    return np.concatenate([np.asarray(r["out"]) for r in res.results], axis=0).astype(np.float32)
```
